# Optimizing a Trainium2 kernel written in Bass

```python
import jax
import jax.numpy as jnp
from jax import lax
import numpy as np

D_MODEL = 1024
BATCH = 4
SEQ = 8192
DEPTH = 4

HEAD_DIM = 64
N_HEADS = 4
BRANCH_W = N_HEADS * HEAD_DIM
N_BRANCH = 4
Q_BLOCK = 128
CMP_BLOCK = 32
SEL_BLOCK = 64
SEL_TOPK = 16
NSA_WINDOW = 512
SWA_WINDOW = 128
SWA_KV_HEADS = 2
RET_CHUNK = 128
RMS_EPS = 1e-6
LN_EPS = 1e-5
NEG_INF = -1e30
TINY = 1e-30
FORCED_SCORE = 1e4

IN_WIDTHS = (
    BRANCH_W, 6 * HEAD_DIM, 3 * N_HEADS, BRANCH_W,
    BRANCH_W, SWA_KV_HEADS * HEAD_DIM, SWA_KV_HEADS * HEAD_DIM, BRANCH_W,
    BRANCH_W, BRANCH_W, BRANCH_W, BRANCH_W,
    BRANCH_W, BRANCH_W, BRANCH_W, BRANCH_W,
)
IN_W = sum(IN_WIDTHS)
IN_OFFSETS = tuple(int(v) for v in np.cumsum(IN_WIDTHS)[:-1])

kernel_name = "hybrid_nsa_swa_stickbreak_retention"


def alibi_slopes(n):
    return jnp.asarray(2.0 ** (-8.0 * (np.arange(n) + 1) / n), dtype=jnp.float32)


def rms_norm(x, g):
    xf = x.astype(jnp.float32)
    y = xf * lax.rsqrt(jnp.mean(xf * xf, axis=-1, keepdims=True) + RMS_EPS)
    return (y * g.astype(jnp.float32)).astype(x.dtype)


def masked_softmax(s, mask, sink=None):
    s = jnp.where(mask, s, NEG_INF)
    m = jnp.max(s, axis=-1, keepdims=True)
    if sink is not None:
        m = jnp.maximum(m, sink)
    e = jnp.where(mask, jnp.exp(s - m), 0.0)
    den = jnp.sum(e, axis=-1, keepdims=True)
    if sink is not None:
        den = den + jnp.exp(sink - m)
    return e / jnp.maximum(den, TINY)


def band_windows(k, n_prev):
    B, S = k.shape[:2]
    nblk = S // Q_BLOCK
    pad = [(0, 0), (n_prev * Q_BLOCK, 0)] + [(0, 0)] * (k.ndim - 2)
    kp = jnp.pad(k, pad).reshape(B, nblk + n_prev, Q_BLOCK, *k.shape[2:])
    return jnp.concatenate([kp[:, j:j + nblk] for j in range(n_prev + 1)], axis=2)


def banded_attention(q, k, v, window, slopes, sink=None):
    B, S, H, Dh = q.shape
    Hkv = k.shape[2]
    G = H // Hkv
    nblk = S // Q_BLOCK
    n_prev = -(-(window - 1) // Q_BLOCK)
    K = (n_prev + 1) * Q_BLOCK
    kw = band_windows(k, n_prev)
    vw = band_windows(v, n_prev)
    qb = q.reshape(B, nblk, Q_BLOCK, Hkv, G, Dh)
    s = jnp.einsum("bnqhgd,bnkhd->bnhgqk", qb, kw).astype(jnp.float32) * Dh ** -0.5
    rel = jnp.arange(Q_BLOCK)[:, None] + n_prev * Q_BLOCK - jnp.arange(K)[None, :]
    kpos = jnp.arange(nblk)[:, None] * Q_BLOCK - n_prev * Q_BLOCK + jnp.arange(K)[None, :]
    mask = ((rel >= 0) & (rel < window))[None] & (kpos >= 0)[:, None, :]
    s = s - slopes.reshape(Hkv, G, 1, 1) * rel.astype(jnp.float32)
    sk = None if sink is None else sink.astype(jnp.float32).reshape(1, 1, Hkv, G, 1, 1)
    p = masked_softmax(s, mask[None, :, None, None], sk)
    o = jnp.einsum("bnhgqk,bnkhd->bnqhgd", p.astype(v.dtype), vw)
    return o.reshape(B, S, H, Dh)


def nsa_attention(q, kv, gate_logits, pos_emb, w1, w2):
    B, S, H, Dh = q.shape
    k_cmp, v_cmp, k_sel, v_sel, k_win, v_win = jnp.split(kv, 6, axis=-1)
    slopes = alibi_slopes(H)
    scale = Dh ** -0.5
    t = jnp.arange(S)

    nc = S // CMP_BLOCK
    def compress(z, j):
        zb = z.reshape(B, nc, CMP_BLOCK, Dh) + pos_emb[j]
        return jax.nn.silu(zb.reshape(B, nc, CMP_BLOCK * Dh) @ w1[j]) @ w2[j]
    kc = compress(k_cmp, 0)
    vc = compress(v_cmp, 1)
    blk_end = jnp.arange(nc) * CMP_BLOCK + CMP_BLOCK - 1
    dist = t[:, None] - blk_end[None, :]
    s = jnp.einsum("bshd,bcd->bhsc", q, kc).astype(jnp.float32) * scale
    s = s - slopes[:, None, None] * dist.astype(jnp.float32)
    p_cmp = masked_softmax(s, dist >= 0)
    o_cmp = jnp.einsum("bhsc,bcd->bshd", p_cmp.astype(vc.dtype), vc)

    ns = S // SEL_BLOCK
    ratio = SEL_BLOCK // CMP_BLOCK
    imp = p_cmp.sum(axis=1).reshape(B, S, ns, ratio).sum(axis=-1)
    blk = jnp.arange(ns)[None, :]
    cur = (t // SEL_BLOCK)[:, None]
    future = blk * SEL_BLOCK > t[:, None]
    forced = (blk == 0) | (blk == cur) | (blk == cur - 1)
    imp = jnp.where(forced, FORCED_SCORE, jnp.where(future, -1.0, imp))
    topk = min(SEL_TOPK, ns)
    _, idx = lax.top_k(imp, topk)

    kb = k_sel.reshape(B, ns, SEL_BLOCK, Dh)
    vb = v_sel.reshape(B, ns, SEL_BLOCK, Dh)
    nq = S // Q_BLOCK
    qs = q.reshape(B, nq, Q_BLOCK, H, Dh).swapaxes(0, 1)
    ids = idx.reshape(B, nq, Q_BLOCK, topk).swapaxes(0, 1)
    qpos = t.reshape(nq, Q_BLOCK)
    bidx = jnp.arange(B)[:, None, None]
    n_keys = topk * SEL_BLOCK

    def sel_block(args):
        qi, ii, pi = args
        kg = kb[bidx, ii].reshape(B, Q_BLOCK, n_keys, Dh)
        vg = vb[bidx, ii].reshape(B, Q_BLOCK, n_keys, Dh)
        kpos = (ii[..., None] * SEL_BLOCK + jnp.arange(SEL_BLOCK)).reshape(B, Q_BLOCK, n_keys)
        d = pi[None, :, None] - kpos
        sc = jnp.einsum("bqhd,bqkd->bhqk", qi, kg).astype(jnp.float32) * scale
        sc = sc - slopes[:, None, None] * d[:, None].astype(jnp.float32)
        pr = masked_softmax(sc, (d >= 0)[:, None])
        return jnp.einsum("bhqk,bqkd->bqhd", pr.astype(vg.dtype), vg)

    o_sel = lax.map(sel_block, (qs, ids, qpos)).swapaxes(0, 1).reshape(B, S, H, Dh)

    o_win = banded_attention(q, k_win[:, :, None], v_win[:, :, None], NSA_WINDOW, slopes)

    g = jax.nn.sigmoid(gate_logits.astype(jnp.float32)).reshape(B, S, H, 3).astype(q.dtype)
    return g[..., 0:1] * o_cmp + g[..., 1:2] * o_sel + g[..., 2:3] * o_win


def stick_breaking_attention(q, k, v):
    B, S, H, Dh = q.shape
    nq = S // Q_BLOCK
    qs = q.reshape(B, nq, Q_BLOCK, H, Dh).swapaxes(0, 1)
    qpos = jnp.arange(S).reshape(nq, Q_BLOCK)
    kpos = jnp.arange(S)
    scale = Dh ** -0.5

    def one_block(args):
        qi, pi = args
        z = jnp.einsum("bqhd,bkhd->bhqk", qi, k).astype(jnp.float32) * scale
        mask = kpos[None, :] < pi[:, None]
        log_beta = jax.nn.log_sigmoid(z)
        log_1m = jnp.where(mask, jax.nn.log_sigmoid(-z), 0.0)
        suffix = lax.cumsum(log_1m, axis=3, reverse=True) - log_1m
        a = jnp.where(mask, jnp.exp(log_beta + suffix), 0.0)
        return jnp.einsum("bhqk,bkhd->bqhd", a.astype(v.dtype), v)

    return lax.map(one_block, (qs, qpos)).swapaxes(0, 1).reshape(B, S, H, Dh)


def retention(q, k, v):
    B, S, H, Dh = q.shape
    C = RET_CHUNK
    n = S // C
    log_g = jnp.log(1.0 - jnp.asarray(2.0 ** (-5.0 - np.arange(H)), dtype=jnp.float32))
    qc = (q.astype(jnp.float32) * Dh ** -0.5).reshape(B, n, C, H, Dh)
    kc = k.astype(jnp.float32).reshape(B, n, C, H, Dh)
    vc = v.astype(jnp.float32).reshape(B, n, C, H, Dh)
    i = jnp.arange(C)
    diff = (i[:, None] - i[None, :]).astype(jnp.float32)
    dmat = jnp.where(diff >= 0, jnp.exp(log_g[:, None, None] * jnp.maximum(diff, 0.0)), 0.0)
    s = jnp.einsum("bnihd,bnjhd->bnhij", qc, kc) * dmat
    intra = jnp.einsum("bnhij,bnjhd->bnihd", s, vc)
    zeta = jnp.exp(log_g[:, None] * (C - 1 - i)[None, :].astype(jnp.float32))
    u = jnp.einsum("bnjhd,bnjhe,hj->nbhde", kc, vc, zeta)
    g_chunk = jnp.exp(log_g * C)[None, :, None, None]

    def step(r, u_n):
        return r * g_chunk + u_n, r

    _, r_prev = lax.scan(step, jnp.zeros((B, H, Dh, Dh), jnp.float32), u)
    xi = jnp.exp(log_g[:, None] * (i + 1)[None, :].astype(jnp.float32))
    cross = jnp.einsum("bnihd,nbhde,hi->bnihe", qc, r_prev, xi)
    o = (intra + cross).reshape(B, S, H, Dh)
    mu = jnp.mean(o, axis=-1, keepdims=True)
    var = jnp.mean((o - mu) ** 2, axis=-1, keepdims=True)
    return ((o - mu) * lax.rsqrt(var + LN_EPS)).astype(q.dtype)


def hybrid_layer(x, c, w_ada, b_ada, norm_g, w_in, cmp_pos, cmp_w1, cmp_w2, sink, w_merge, w_br, w_out):
    B, S, _ = x.shape
    mod = (jax.nn.silu(c) @ w_ada + b_ada)[:, None, :]
    shift, scale, gate = jnp.split(mod, 3, axis=-1)
    h = rms_norm(x, norm_g) * (1.0 + scale) + shift
    (a_q, a_kv, a_g, a_z, b_q, b_k, b_v, b_z,
     c_q, c_k, c_v, c_z, d_q, d_k, d_v, d_z) = jnp.split(h @ w_in, IN_OFFSETS, axis=-1)

    def heads(z):
        return z.reshape(B, S, -1, HEAD_DIM)

    y_a = nsa_attention(heads(a_q), a_kv, a_g, cmp_pos, cmp_w1, cmp_w2)
    y_b = banded_attention(heads(b_q), heads(b_k), heads(b_v), SWA_WINDOW, alibi_slopes(N_HEADS), sink)
    y_c = stick_breaking_attention(heads(c_q), heads(c_k), heads(c_v))
    y_d = retention(heads(d_q), heads(d_k), heads(d_v))

    branches = ((y_a, a_z), (y_b, b_z), (y_c, c_z), (y_d, d_z))
    merged = None
    for i, (y, z) in enumerate(branches):
        yi = y.reshape(B, S, BRANCH_W) * jax.nn.silu(z)
        term = jax.nn.sigmoid(h @ w_merge[i]) * (yi @ w_br[i])
        merged = term if merged is None else merged + term
    return x + gate * (merged @ w_out)


def setup_inputs(seed: int = 0) -> dict:
    key = jax.random.key(seed)
    ks = jax.random.split(key, 14)
    D = D_MODEL
    f32 = jnp.float32

    def nrm(k, shape, fan):
        return jax.random.normal(k, shape, f32) * fan ** -0.5

    return {
        "x": jax.random.normal(ks[0], (BATCH, SEQ, D), f32),
        "c": jax.random.normal(ks[1], (BATCH, D), f32),
        "w_ada": nrm(ks[2], (DEPTH, D, 3 * D), D) * 0.5,
        "b_ada": 0.02 * jax.random.normal(ks[3], (DEPTH, 3 * D), f32),
        "norm_g": 1.0 + 0.05 * jax.random.normal(ks[4], (DEPTH, D), f32),
        "w_in": nrm(ks[5], (DEPTH, D, IN_W), D),
        "cmp_pos": 0.1 * jax.random.normal(ks[6], (DEPTH, 2, CMP_BLOCK, HEAD_DIM), f32),
        "cmp_w1": nrm(ks[7], (DEPTH, 2, CMP_BLOCK * HEAD_DIM, HEAD_DIM), CMP_BLOCK * HEAD_DIM),
        "cmp_w2": nrm(ks[8], (DEPTH, 2, HEAD_DIM, HEAD_DIM), HEAD_DIM),
        "sink": 0.5 * jax.random.normal(ks[9], (DEPTH, N_HEADS), f32),
        "w_merge": nrm(ks[10], (DEPTH, N_BRANCH, D, D), D),
        "w_br": nrm(ks[11], (DEPTH, N_BRANCH, BRANCH_W, D), BRANCH_W),
        "w_out": nrm(ks[12], (DEPTH, D, D), D),
        "final_g": 1.0 + 0.05 * jax.random.normal(ks[13], (D,), f32),
    }


def reference(x, c, w_ada, b_ada, norm_g, w_in, cmp_pos, cmp_w1, cmp_w2, sink, w_merge, w_br, w_out, final_g):
    for l in range(DEPTH):
        x = hybrid_layer(x, c, w_ada[l], b_ada[l], norm_g[l], w_in[l], cmp_pos[l], cmp_w1[l], cmp_w2[l],
                         sink[l], w_merge[l], w_br[l], w_out[l])
    return rms_norm(x, final_g)
```

```python
from contextlib import ExitStack
import numpy as np
import ml_dtypes
import concourse.bass as bass
import concourse.mybir as mybir
from concourse.bass_utils import run_bass_kernel_spmd

F32 = mybir.dt.float32
BF16 = mybir.dt.bfloat16
AF = mybir.ActivationFunctionType
ALU = mybir.AluOpType

D = 1024
S = 8192
NT = 16
TT = 512
NB = 64
NEG = -30000.0
RMS_EPS = 1e-6
LN_EPS = 1e-5

FM_AQ01, FM_AQ23, FM_KVC, FM_KSW, FM_G0, FM_G1, FM_G2, FM_AZ, FM_BQ, FM_BK, FM_BZ, \
    FM_CQ, FM_CK, FM_CZ, FM_DQ, FM_DK, FM_DZ = range(17)
NFM = 17
Q_TILES = (FM_AQ01, FM_AQ23, FM_BQ, FM_CQ, FM_DQ)
TM_VSEL, TM_VWIN, TM_BV, TM_CV0, TM_CV1, TM_DV0, TM_DV1, TM_DK0, TM_DK1 = [64 * i for i in range(9)]
NTM = 576
NCOL = NFM * 128 + NTM

MK_STRICT = 0
MK_NONSTRICT = 4
MK_WINA = 8
MK_WINB = 12
MK_CMP = 17
NMASK = 25


class Prog:
    def __init__(self, nc, ctx, n_dma_sems=24):
        self.nc = nc
        self.eng = {"pe": nc.tensor, "act": nc.scalar, "dve": nc.vector, "pool": nc.gpsimd, "sp": nc.sync}
        self.sems = []
        self.semid = {}
        for e in self.eng:
            self.semid[e] = len(self.sems)
            self.sems.append(ctx.enter_context(nc.semaphore("p_" + e)))
        self.cnt = {e: 0 for e in self.eng}
        self.dsem = []
        for i in range(n_dma_sems):
            self.dsem.append(len(self.sems))
            self.sems.append(ctx.enter_context(nc.semaphore("d%d" % i)))
        self.duse = [0] * n_dma_sems
        self.dnext = 0
        self.known = {e: {} for e in self.eng}
        self.res = {}
        self.out_tokens = []
        self.ninstr = 0

    def _st(self, k):
        st = self.res.get(k)
        if st is None:
            st = [None, {}]
            self.res[k] = st
        return st

    def _deps(self, reads, writes):
        need = {}

        def add(tok):
            s, v = tok
            if need.get(s, 0) < v:
                need[s] = v
        for k in reads:
            st = self._st(k)
            if st[0] is not None:
                add(st[0])
        for k in writes:
            st = self._st(k)
            if st[0] is not None:
                add(st[0])
            for s, v in st[1].items():
                add((s, v))
        return need

    def _wait(self, e, need):
        kn = self.known[e]
        for s, v in need.items():
            if e == "pe" and s == self.semid["pe"]:
                continue
            if kn.get(s, 0) >= v:
                continue
            self.eng[e].wait_ge(self.sems[s], v)
            kn[s] = v
            self.ninstr += 1

    def _record(self, tok, reads, writes):
        for k in reads:
            st = self._st(k)
            if st[1].get(tok[0], 0) < tok[1]:
                st[1][tok[0]] = tok[1]
        for k in writes:
            st = self._st(k)
            st[0] = tok
            st[1] = {}

    def op(self, e, fn, reads=(), writes=()):
        self._wait(e, self._deps(reads, writes))
        ins = fn(self.eng[e])
        self.cnt[e] += 1
        tok = (self.semid[e], self.cnt[e])
        ins.then_inc(self.sems[tok[0]], 1)
        self._record(tok, reads, writes)
        self.ninstr += 1
        return tok

    def dma(self, q, out, in_, reads=(), writes=(), is_output=False):
        k = self.dnext
        self.dnext = (k + 1) % len(self.dsem)
        need = self._deps(reads, writes)
        if self.duse[k] > 0:
            s = self.dsem[k]
            if need.get(s, 0) < 16 * self.duse[k]:
                need[s] = 16 * self.duse[k]
        self._wait(q, need)
        ins = self.eng[q].dma_start(out=out, in_=in_)
        self.duse[k] += 1
        tok = (self.dsem[k], 16 * self.duse[k])
        ins.then_inc(self.sems[tok[0]], 16)
        self._record(tok, reads, writes)
        if is_output:
            self.out_tokens.append(tok)
        self.ninstr += 1
        return tok

    def barrier(self):
        need = {}
        for e in self.eng:
            if self.cnt[e] > 0:
                need[self.semid[e]] = self.cnt[e]
        for k, s in enumerate(self.dsem):
            if self.duse[k] > 0:
                need[s] = 16 * self.duse[k]
        for e in self.eng:
            n2 = {s: v for s, v in need.items() if s != self.semid[e] or e != "pe"}
            self._wait(e, n2)
        self.res = {}

    def finish(self):
        need = {}
        for s, v in self.out_tokens:
            if need.get(s, 0) < v:
                need[s] = v
        self._wait("sp", need)


def _bc(ap, shape, axis):
    return ap.unsqueeze(axis).broadcast_to(shape)


def build_program(first, last_only, dbg=None, stop_after=99, mixers="ABCD"):
    dbg = dbg or set()
    nc = bass.Bass("TRN2", target_bir_lowering=False)

    def din(name, shape, dt=F32):
        return nc.dram_tensor(name, list(shape), dt, kind="ExternalInput").ap()

    def dout(name, shape, dt=F32):
        return nc.dram_tensor(name, list(shape), dt, kind="ExternalOutput").ap()

    def dscr(name, shape, dt):
        kind = "ExternalOutput" if name in dbg else "Internal"
        return nc.dram_tensor(name, list(shape), dt, kind=kind).ap()

    xT = din("xT", [D, S])
    if not first:
        paT = din("paT", [D, S])
        pbT = din("pbT", [D, S])
    if last_only:
        fgT = din("fgT", [128, 8])
        outT = dout("outT", [D, S])
    else:
        cT = din("cT", [128, 8])
        w_ada = din("w_ada", [D, 3 * D])
        b_adaT = din("b_adaT", [128, 24])
        gT = din("gT", [128, 8])
        w_in = din("w_in", [D, NCOL])
        cmp_posT = din("cmp_posT", [64, 2, 32])
        cmp_w1 = din("cmp_w1", [64, 2, 32, 64])
        cmp_w2 = din("cmp_w2", [64, 2, 64])
        sinkb = din("sinkb", [128, 1])
        w_merge = din("w_merge", [4, D, D])
        w_br = din("w_br", [4, 128, D])
        w_out = din("w_out", [D, D])
        masks = din("masks", [NMASK, 128, TT], BF16)
        qpos = din("qpos", [4, 4, S], BF16)
        kpos_tok = din("kpos_tok", [4, S], BF16)
        kpos_cmp = din("kpos_cmp", [4, 256], BF16)
        ewide = din("ewide", [128, S], BF16)
        ident = din("ident", [128, 128], BF16)
        ntri = din("ntri", [128, 128], BF16)
        pairsum = din("pairsum", [2, 128, 128], BF16)
        selkeep = din("selkeep", [128, 256])
        seladd = din("seladd", [128, 256])
        dtab = din("dtab", [128, 2, 128])
        zeta = din("zeta", [128, 2])
        xi_bc = din("xi_bc", [64, 2, 128])
        gchunk = din("gchunk", [64, 2])
        partT = dout("partT", [D, S])
        if not first:
            xcT = dout("xcT", [D, S])
        hT = dscr("hT", [D, S], BF16)
        fm = dscr("fm", [NFM * 128, S], BF16)
        tm = dscr("tm", [S, NTM], BF16)
        yzd = dscr("yzd", [512, S], BF16)

    ctx = ExitStack()
    with ctx:
        P = Prog(nc, ctx)
        banks = [ctx.enter_context(nc.psum_tensor("bank%d" % i, [128, 512], F32)) for i in range(8)]

        def sb(c, name, shape, dt):
            return c.enter_context(nc.sbuf_tensor(name, list(shape), dt))

        ones_f = sb(ctx, "ones_f", [128, 128], F32)
        P.op("dve", lambda e: e.memset(ones_f[:], 1.0), writes=["ones_f"])
        xview = xT.rearrange("(k p) t -> p k t", p=128)

        if last_only:
            with ExitStack() as c:
                fg = sb(c, "fg", [128, 8], F32)
                P.dma("sp", fg[:], fgT[:, :], writes=["fg"])
                pav = paT.rearrange("(k p) t -> p k t", p=128)
                pbv = pbT.rearrange("(k p) t -> p k t", p=128)
                ov = outT.rearrange("(k p) t -> p k t", p=128)
                xt = [sb(c, "xt%d" % i, [128, 8, TT], F32) for i in range(2)]
                pt = [sb(c, "pt%d" % i, [128, 8, TT], F32) for i in range(2)]
                qt = [sb(c, "qt%d" % i, [128, 8, TT], F32) for i in range(2)]
                sq = sb(c, "sq", [128, 8, TT], F32)
                rt = sb(c, "rt", [128, TT], F32)
                rstd = sb(c, "rstd", [128, TT], F32)
                for tt in range(NT):
                    i = tt % 2
                    ts_ = slice(tt * TT, (tt + 1) * TT)
                    P.dma("sp", xt[i][:], xview[:, :, ts_], writes=[("xt", i)])
                    P.dma("sp", pt[i][:], pav[:, :, ts_], writes=[("pt", i)])
                    P.dma("sp", qt[i][:], pbv[:, :, ts_], writes=[("qt", i)])
                    P.op("dve", lambda e: e.tensor_tensor(out=xt[i][:], in0=xt[i][:], in1=pt[i][:], op=ALU.add),
                         reads=[("xt", i), ("pt", i)], writes=[("xt", i)])
                    P.op("dve", lambda e: e.tensor_tensor(out=xt[i][:], in0=xt[i][:], in1=qt[i][:], op=ALU.add),
                         reads=[("xt", i), ("qt", i)], writes=[("xt", i)])
                    P.op("act", lambda e: e.activation(out=sq[:], in_=xt[i][:], func=AF.Square),
                         reads=[("xt", i)], writes=["sq"])
                    bk = banks[tt % 2]
                    for kc in range(8):
                        P.op("pe", lambda e: e.matmul(bk[:], ones_f[:], sq[:, kc, :], start=(kc == 0), stop=(kc == 7)),
                             reads=["sq", "ones_f"], writes=[("bank", tt % 2)])
                    P.op("act", lambda e: e.activation(out=rt[:], in_=bk[:], func=AF.Sqrt, scale=1.0 / D, bias=RMS_EPS),
                         reads=[("bank", tt % 2)], writes=["rt"])
                    P.op("dve", lambda e: e.reciprocal(out=rstd[:], in_=rt[:]), reads=["rt"], writes=["rstd"])
                    P.op("dve", lambda e: e.tensor_tensor(out=xt[i][:], in0=xt[i][:],
                                                          in1=_bc(rstd[:], [128, 8, TT], 1), op=ALU.mult),
                         reads=[("xt", i), "rstd"], writes=[("xt", i)])
                    for kc in range(8):
                        P.op("act", lambda e: e.activation(out=xt[i][:, kc, :], in_=xt[i][:, kc, :], func=AF.Copy,
                                                           scale=fg[:, kc:kc + 1]),
                             reads=[("xt", i), "fg"], writes=[("xt", i)])
                    P.dma("pool", ov[:, :, ts_], xt[i][:], reads=[("xt", i)], is_output=True)
            P.finish()
            return nc


        ident_b = sb(ctx, "ident_b", [128, 128], BF16)
        P.dma("sp", ident_b[:], ident[:, :], writes=["ident_b"])
        ones_b = sb(ctx, "ones_b", [128, 128], BF16)
        P.op("dve", lambda e: e.memset(ones_b[:], 1.0), writes=["ones_b"])
        mod = sb(ctx, "mod", [128, 24], F32)
        gs = sb(ctx, "gs", [128, 8], F32)
        with ExitStack() as c:
            cs = sb(c, "cs", [128, 8], F32)
            csil = sb(c, "csil", [128, 8, 2], F32)
            bada = sb(c, "bada", [128, 24], F32)
            gt_ = sb(c, "gt_", [128, 8], F32)
            wa = [sb(c, "wa%d" % i, [128, 3 * D], F32) for i in range(2)]
            P.dma("sp", cs[:], cT[:, :], writes=["cs"])
            P.dma("sp", bada[:], b_adaT[:, :], writes=["bada"])
            P.dma("sp", gt_[:], gT[:, :], writes=["gt_"])
            for j in range(2):
                P.op("act", lambda e: e.activation(out=csil[:, :, j], in_=cs[:], func=AF.Silu),
                     reads=["cs"], writes=["csil"])
            for kc in range(8):
                i = kc % 2
                P.dma("sp", wa[i][:], w_ada[kc * 128:(kc + 1) * 128, :], writes=[("wa", i)])
                for oc in range(24):
                    P.op("pe", lambda e: e.matmul(banks[0][:, 2 * oc:2 * oc + 2], wa[i][:, oc * 128:(oc + 1) * 128],
                                                  csil[:, kc, :], start=(kc == 0 and oc == 0), stop=(kc == 7 and oc == 23)),
                         reads=[("wa", i), "csil"], writes=[("bank", 0)])
            P.op("dve", lambda e: e.tensor_tensor(out=mod[:], in0=banks[0][:, 0:48:2], in1=bada[:], op=ALU.add),
                 reads=[("bank", 0), "bada"], writes=["mod"])
            P.op("dve", lambda e: e.scalar_tensor_tensor(out=gs[:], in0=mod[:, 8:16], scalar=1.0, in1=gt_[:],
                                                         op0=ALU.add, op1=ALU.mult),
                 reads=["mod", "gt_"], writes=["gs"])
            P.barrier()

        def load_cast(c_stage, stg, dst_ap, src_ap, n, key_dst, idx):
            i = idx % 2
            P.dma("sp", stg[i][:, 0:n], src_ap, writes=[("stg", i)])
            eng = "dve" if idx % 2 == 0 else "pool"
            P.op(eng, lambda e: e.tensor_copy(dst_ap, stg[i][:, 0:n]), reads=[("stg", i)], writes=[key_dst])

        with ExitStack() as c:
            wb = sb(c, "wb", [128, 8, NCOL], BF16)
            stg = [sb(c, "stg%d" % i, [128, NCOL], F32) for i in range(2)]
            for kc in range(8):
                load_cast(c, stg, wb[:, kc, :], w_in[kc * 128:(kc + 1) * 128, :], NCOL, ("wb", kc), kc)
            xt = [sb(c, "xt%d" % i, [128, 8, TT], F32) for i in range(2)]
            if not first:
                pt = sb(c, "pt", [128, 8, TT], F32)
                pav = paT.rearrange("(k p) t -> p k t", p=128)
                pbv = pbT.rearrange("(k p) t -> p k t", p=128)
                xcv = xcT.rearrange("(k p) t -> p k t", p=128)
            sq = sb(c, "sq", [128, 8, TT], F32)
            rt = sb(c, "rt", [128, TT], F32)
            rstd = sb(c, "rstd", [128, TT], F32)
            hb = [sb(c, "hb%d" % i, [128, 8, TT], BF16) for i in range(2)]
            ev = [sb(c, "ev%d" % i, [128, TT], BF16) for i in range(4)]
            evt = [sb(c, "evt%d" % i, [128, NTM], BF16) for i in range(2)]
            hview = hT.rearrange("(k p) t -> p k t", p=128)
            nbank = 0
            nev = 0
            for tt in range(NT):
                i = tt % 2
                ts_ = slice(tt * TT, (tt + 1) * TT)
                P.dma("sp", xt[i][:], xview[:, :, ts_], writes=[("xt", i)])
                if not first:
                    for pv in (pav, pbv):
                        P.dma("sp", pt[:], pv[:, :, ts_], writes=["pt"])
                        P.op("dve", lambda e: e.tensor_tensor(out=xt[i][:], in0=xt[i][:], in1=pt[:], op=ALU.add),
                             reads=[("xt", i), "pt"], writes=[("xt", i)])
                    P.dma("pool", xcv[:, :, ts_], xt[i][:], reads=[("xt", i)], is_output=True)
                P.op("act", lambda e: e.activation(out=sq[:], in_=xt[i][:], func=AF.Square),
                     reads=[("xt", i)], writes=["sq"])
                bi = nbank % 8
                nbank += 1
                for kc in range(8):
                    P.op("pe", lambda e: e.matmul(banks[bi][:], ones_f[:], sq[:, kc, :], start=(kc == 0), stop=(kc == 7)),
                         reads=["sq", "ones_f"], writes=[("bank", bi)])
                P.op("act", lambda e: e.activation(out=rt[:], in_=banks[bi][:], func=AF.Sqrt, scale=1.0 / D, bias=RMS_EPS),
                     reads=[("bank", bi)], writes=["rt"])
                P.op("dve", lambda e: e.reciprocal(out=rstd[:], in_=rt[:]), reads=["rt"], writes=["rstd"])
                P.op("dve", lambda e: e.tensor_tensor(out=sq[:], in0=xt[i][:], in1=_bc(rstd[:], [128, 8, TT], 1), op=ALU.mult),
                     reads=[("xt", i), "rstd", "sq"], writes=["sq"])
                for kc in range(8):
                    P.op("act", lambda e: e.activation(out=hb[i][:, kc, :], in_=sq[:, kc, :], func=AF.Identity,
                                                       scale=gs[:, kc:kc + 1], bias=mod[:, kc:kc + 1]),
                         reads=["sq", "gs", "mod"], writes=[("hb", i)])
                P.dma("pool", hview[:, :, ts_], hb[i][:], reads=[("hb", i)], writes=[("hT", tt)])
                for ct in range(NFM):
                    bi = nbank % 8
                    nbank += 1
                    for kc in range(8):
                        P.op("pe", lambda e: e.matmul(banks[bi][:], wb[:, kc, ct * 128:(ct + 1) * 128], hb[i][:, kc, :],
                                                      start=(kc == 0), stop=(kc == 7)),
                             reads=[("wb", kc), ("hb", i)], writes=[("bank", bi)])
                    ei = nev % 4
                    nev += 1
                    if ct in Q_TILES:
                        P.op("act", lambda e: e.activation(out=ev[ei][:], in_=banks[bi][:], func=AF.Copy, scale=0.125),
                             reads=[("bank", bi)], writes=[("ev", ei)])
                    elif ct % 2 == 0:
                        P.op("act", lambda e: e.activation(out=ev[ei][:], in_=banks[bi][:], func=AF.Copy),
                             reads=[("bank", bi)], writes=[("ev", ei)])
                    else:
                        P.op("dve", lambda e: e.tensor_copy(ev[ei][:], banks[bi][:]),
                             reads=[("bank", bi)], writes=[("ev", ei)])
                    P.dma("pool", fm[ct * 128:(ct + 1) * 128, ts_], ev[ei][:], reads=[("ev", ei)], writes=[("fm", ct, tt)])
                for sub in range(4):
                    ei = (tt * 4 + sub) % 2
                    for g, (c0, c1) in enumerate(((0, 320), (320, 576))):
                        bi = nbank % 8
                        nbank += 1
                        for kc in range(8):
                            P.op("pe", lambda e: e.matmul(banks[bi][:, 0:c1 - c0], hb[i][:, kc, sub * 128:(sub + 1) * 128],
                                                          wb[:, kc, NFM * 128 + c0:NFM * 128 + c1],
                                                          start=(kc == 0), stop=(kc == 7)),
                                 reads=[("wb", kc), ("hb", i)], writes=[("bank", bi)])
                        P.op("dve" if g == 0 else "act",
                             (lambda e: e.tensor_copy(evt[ei][:, c0:c1], banks[bi][:, 0:c1 - c0])) if g == 0 else
                             (lambda e: e.activation(out=evt[ei][:, c0:c1], in_=banks[bi][:, 0:c1 - c0], func=AF.Copy)),
                             reads=[("bank", bi)], writes=[("evt", ei)])
                    r0 = tt * TT + sub * 128
                    P.dma("pool", tm[r0:r0 + 128, :], evt[ei][:], reads=[("evt", ei)], writes=[("tm", tt)])
            P.barrier()

        if stop_after <= 1:
            P.finish()
            return nc
        hh_ = 0
        qpos_own = [qpos[0], qpos[1]]
        fmv = fm
        tmv = tm.rearrange("(n p) c -> p n c", p=128)
        stage2 = ExitStack()
        masks_sb = sb(stage2, "masks_sb", [128, NMASK, TT], BF16)
        P.dma("sp", masks_sb[:], masks.rearrange("m p t -> p m t"), writes=["masks_sb"])
        zrot = [0]
        arot = [0]
        a_sb = [sb(stage2, "a_sb%d" % i, [128, TT], BF16) for i in range(3)]
        o_sb = sb(stage2, "o_sb", [64, TT], F32)
        dn_sb = sb(stage2, "dn_sb", [64, TT], F32)
        ACC = 3

        def softmax_tile(q_ap, qkey, ka, kakey, vo, vokey, chunks, selmask=None, sink_ap=None):
            n = len(chunks)
            for idx, (kc, mk) in enumerate(chunks):
                zi = zrot[0] % 3
                zrot[0] += 1
                ai = arot[0] % 3
                arot[0] += 1
                mms = [(ka[0:68, kc * 128:(kc + 1) * 128], q_ap, [kakey, qkey])]
                if selmask is not None:
                    mms.append((ewide_sb[:, kc * 128:(kc + 1) * 128], selmask[:], ["ewide_sb", "mneg"]))
                if mk is not None:
                    mms.append((ident_b[:], masks_sb[:, mk, :], ["ident_b", "masks_sb"]))
                for j, (l_, r_, rd) in enumerate(mms):
                    P.op("pe", lambda e: e.matmul(banks[zi][:], l_, r_, start=(j == 0), stop=(j == len(mms) - 1)),
                         reads=rd, writes=[("bank", zi)])
                P.op("act", lambda e: e.activation(out=a_sb[ai][:], in_=banks[zi][:], func=AF.Exp),
                     reads=[("bank", zi)], writes=[("a_sb", ai)])
                P.op("pe", lambda e: e.matmul(banks[ACC][:], vo[:, kc, :], a_sb[ai][:], start=(idx == 0), stop=(idx == n - 1)),
                     reads=[vokey, ("a_sb", ai)], writes=[("bank", ACC)])
            if sink_ap is not None:
                P.op("dve", lambda e: e.tensor_scalar(out=dn_sb[:], in0=banks[ACC][64:128, :], scalar1=sink_ap, scalar2=1e-30,
                                                      op0=ALU.add, op1=ALU.max),
                     reads=[("bank", ACC), "esink"], writes=["dn_sb"])
            else:
                P.op("dve", lambda e: e.tensor_scalar_max(out=dn_sb[:], in0=banks[ACC][64:128, :], scalar1=1e-30),
                     reads=[("bank", ACC)], writes=["dn_sb"])
            P.op("dve", lambda e: e.reciprocal(out=dn_sb[:], in_=dn_sb[:]), reads=["dn_sb"], writes=["dn_sb"])
            P.op("dve", lambda e: e.tensor_tensor(out=o_sb[:], in0=banks[ACC][0:64, :], in1=dn_sb[:], op=ALU.mult),
                 reads=[("bank", ACC), "dn_sb"], writes=["o_sb"])

        def build_vo(c, name, col):
            vo = sb(c, name, [128, NB, 128], BF16)
            P.dma("sp", vo[:, :, 0:64], tmv[:, :, col:col + 64], writes=[name])
            P.op("pool", lambda e: e.memset(vo[:, :, 64:128], 1.0), writes=[name])
            return vo

        def build_ka(c, name, ct, row0, kp):
            ka = sb(c, name, [68, S], BF16)
            P.dma("sp", ka[0:64, :], fmv[ct * 128 + row0:ct * 128 + row0 + 64, :], writes=[name])
            P.dma("sp", ka[64:68, :], kp, writes=[name])
            return ka

        if "B" in mixers or "A" in mixers:
          with ExitStack() as c:
            qa = [sb(c, "qa%d" % h, [68, TT], BF16) for h in range(4)]
            gl = [sb(c, "gl%d" % i, [64, TT], F32) for i in range(6)]
            zt_sb = sb(c, "zt_sb", [128, TT], BF16)
            zs_sb = sb(c, "zs_sb", [128, TT], F32)
            ya = sb(c, "ya", [128, TT], F32)
            yo = sb(c, "yo", [128, TT], BF16)
            tmp64 = sb(c, "tmp64", [64, TT], F32)
            if "B" in mixers:
                cB = ExitStack()
                kb = build_ka(cB, "kb", FM_BK, 0, kpos_tok[:, :])
                vb = build_vo(cB, "vb", TM_BV)
                esink = sb(cB, "esink", [128, 1], F32)
                P.dma("sp", esink[:], sinkb[:, :], writes=["esink"])
                P.op("act", lambda e: e.activation(out=esink[:], in_=esink[:], func=AF.Exp), reads=["esink"], writes=["esink"])
                es2 = sb(cB, "es2", [64, 2], F32)
                P.op("dve", lambda e: e.tensor_copy(es2[:, 0:1], esink[0:64, :]), reads=["esink"], writes=["esink2"])
                P.op("dve", lambda e: e.tensor_copy(es2[:, 1:2], esink[64:128, :]), reads=["esink"], writes=["esink2"])
                for tt in range(NT):
                    ts_ = slice(tt * TT, (tt + 1) * TT)
                    P.dma("sp", zt_sb[:], fmv[FM_BZ * 128:(FM_BZ + 1) * 128, ts_], writes=["zt_sb"])
                    P.op("act", lambda e: e.activation(out=zs_sb[:], in_=zt_sb[:], func=AF.Silu), reads=["zt_sb"], writes=["zs_sb"])
                    for hl in range(2):
                        h = 2 * hh_ + hl
                        P.dma("sp", qa[hl][0:64, :], fmv[FM_BQ * 128 + hl * 64:FM_BQ * 128 + hl * 64 + 64, ts_], writes=[("qa", hl)])
                        P.dma("sp", qa[hl][64:68, :], qpos_own[hl][:, ts_], writes=[("qa", hl)])
                        chunks = [(kc, MK_WINB + (kc - 4 * tt + 1)) for kc in range(max(0, 4 * tt - 1), 4 * tt + 4)]
                        softmax_tile(qa[hl][:], ("qa", hl), kb, "kb", vb, "vb", chunks, sink_ap=es2[:, hl:hl + 1])
                        P.op("dve", lambda e: e.tensor_copy(ya[hl * 64:hl * 64 + 64, :], o_sb[:]),
                             reads=["o_sb"], writes=["ya"])
                    P.op("dve", lambda e: e.tensor_tensor(out=yo[:], in0=ya[:], in1=zs_sb[:], op=ALU.mult),
                         reads=["ya", "zs_sb"], writes=["yo"])
                    P.dma("pool", yzd[128:256, ts_], yo[:], reads=["yo"], writes=[("yzd", 1, tt)])
                P.barrier()
                cB.close()
            if "A" in mixers:
             with ExitStack() as c2:
              ksel = build_ka(c2, "ksel", FM_KSW, 0, kpos_tok[:, :])
              kwin = build_ka(c2, "kwin", FM_KSW, 64, kpos_tok[:, :])
              vsel = build_vo(c2, "vsel", TM_VSEL)
              vwin = build_vo(c2, "vwin", TM_VWIN)
              ewide_sb = sb(c2, "ewide_sb", [128, S], BF16)
              P.dma("sp", ewide_sb[:], ewide[:, :], writes=["ewide_sb"])
              keep_sb = sb(c2, "keep_sb", [128, 256], F32)
              add_sb = sb(c2, "add_sb", [128, 256], F32)
              P.dma("sp", keep_sb[:], selkeep[:, :], writes=["keep_sb"])
              P.dma("sp", add_sb[:], seladd[:, :], writes=["add_sb"])
              ps_sb = sb(c2, "ps_sb", [128, 2, 128], BF16)
              P.dma("sp", ps_sb[:], pairsum.rearrange("c p b -> p c b"), writes=["ps_sb"])
              kca = sb(c2, "kca", [68, 256], BF16)
              vcs = sb(c2, "vcs", [128, 2, 64], BF16)
              P.dma("sp", kca[64:68, :], kpos_cmp[:, :], writes=["kca"])
              with ExitStack() as c3:
                  kv = sb(c3, "kv", [128, S], BF16)
                  P.dma("sp", kv[:], fmv[FM_KVC * 128:(FM_KVC + 1) * 128, :], writes=["kv"])
                  posf = sb(c3, "posf", [128, 32], F32)
                  P.dma("sp", posf[0:64, :], cmp_posT[:, 0, :], writes=["posf"])
                  P.dma("sp", posf[64:128, :], cmp_posT[:, 1, :], writes=["posf"])
                  kvp = sb(c3, "kvp", [128, 256, 32], BF16)
                  P.op("dve", lambda e: e.tensor_tensor(out=kvp[:], in0=kv[:].rearrange("p (c j) -> p c j", j=32),
                                                        in1=_bc(posf[:], [128, 256, 32], 1), op=ALU.add),
                       reads=["kv", "posf"], writes=["kvp"])
                  w1f = sb(c3, "w1f", [128, 32, 64], F32)
                  w1b = sb(c3, "w1b", [128, 32, 64], BF16)
                  P.dma("sp", w1f[0:64], cmp_w1[:, 0], writes=["w1f"])
                  P.dma("sp", w1f[64:128], cmp_w1[:, 1], writes=["w1f"])
                  P.op("dve", lambda e: e.tensor_copy(w1b[:], w1f[:]), reads=["w1f"], writes=["w1b"])
                  w2f = sb(c3, "w2f", [128, 64], F32)
                  w2b = sb(c3, "w2b", [128, 64], BF16)
                  P.dma("sp", w2f[0:64], cmp_w2[:, 0], writes=["w2f"])
                  P.dma("sp", w2f[64:128], cmp_w2[:, 1], writes=["w2f"])
                  P.op("dve", lambda e: e.tensor_copy(w2b[:], w2f[:]), reads=["w2f"], writes=["w2b"])
                  hk = sb(c3, "hk", [128, 256], BF16)
                  for jj, p0 in enumerate((0, 64)):
                      for j in range(32):
                          P.op("pe", lambda e: e.matmul(banks[jj][p0:p0 + 64, 0:256], w1b[p0:p0 + 64, j, :], kvp[p0:p0 + 64, :, j],
                                                        start=(j == 0), stop=(j == 31)),
                               reads=["w1b", "kvp"], writes=[("bank", jj)])
                      P.op("act", lambda e: e.activation(out=hk[p0:p0 + 64, :], in_=banks[jj][p0:p0 + 64, 0:256], func=AF.Silu),
                           reads=[("bank", jj)], writes=["hk"])
                  P.op("pe", lambda e: e.matmul(banks[2][0:64, 0:256], w2b[0:64, :], hk[0:64, :], start=True, stop=True),
                       reads=["w2b", "hk"], writes=[("bank", 2)])
                  P.op("dve", lambda e: e.tensor_copy(kca[0:64, :], banks[2][0:64, 0:256]), reads=[("bank", 2)], writes=["kca"])
                  for cc in range(2):
                      P.op("pe", lambda e: e.matmul(banks[4 + cc][:, 0:64], hk[64:128, cc * 128:(cc + 1) * 128], w2b[64:128, :],
                                                    start=True, stop=True),
                           reads=["w2b", "hk"], writes=[("bank", 4 + cc)])
                      P.op("dve", lambda e: e.tensor_copy(vcs[:, cc, :], banks[4 + cc][:, 0:64]), reads=[("bank", 4 + cc)], writes=["vcs"])
                  P.barrier()
              e_sb = [sb(c2, "e_sb%d" % i, [128, TT], BF16) for i in range(8)]
              rec = sb(c2, "rec", [128, TT], F32)
              adj = sb(c2, "adj", [128, 128], F32)
              adj2 = sb(c2, "adj2", [128, 128], F32)
              m8a = sb(c2, "m8a", [128, 8], F32)
              m8b = sb(c2, "m8b", [128, 8], F32)
              mt = sb(c2, "mt", [128, 128], BF16)
              mneg = sb(c2, "mneg", [128, TT], BF16)
              ocmp = sb(c2, "ocmp", [64, 2, TT], F32)
              bank4b = banks[4][:].bitcast(BF16)
              for tt in range(NT):
                  ts_ = slice(tt * TT, (tt + 1) * TT)
                  for h in range(4):
                      ct = FM_AQ01 if h < 2 else FM_AQ23
                      r0 = ct * 128 + (h % 2) * 64
                      P.dma("sp", qa[h][0:64, :], fmv[r0:r0 + 64, ts_], writes=[("qa", h)])
                      P.dma("sp", qa[h][64:68, :], qpos[h, :, ts_], writes=[("qa", h)])
                  for j in range(3):
                      for hl in range(2):
                          r0 = (FM_G0 + j) * 128 + hl * 64
                          gi = j * 2 + hl
                          P.dma("sp", zt_sb[0:64, :], fmv[r0:r0 + 64, ts_], writes=["zt_sb"])
                          P.op("act", lambda e: e.activation(out=gl[gi][:], in_=zt_sb[0:64, :], func=AF.Sigmoid),
                               reads=["zt_sb"], writes=[("gl", gi)])
                  ccs = [0] if tt < 8 else [0, 1]
                  for h in range(4):
                      for cc in ccs:
                          zi = zrot[0] % 3
                          zrot[0] += 1
                          m = tt - 8 * cc
                          P.op("pe", lambda e: e.matmul(banks[zi][:], kca[0:68, cc * 128:(cc + 1) * 128], qa[h][:],
                                                        start=True, stop=(m >= 8)),
                               reads=["kca", ("qa", h)], writes=[("bank", zi)])
                          if m < 8:
                              P.op("pe", lambda e: e.matmul(banks[zi][:], ident_b[:], masks_sb[:, MK_CMP + m, :], start=False, stop=True),
                                   reads=["ident_b", "masks_sb"], writes=[("bank", zi)])
                          P.op("act", lambda e: e.activation(out=e_sb[h * 2 + cc][:], in_=banks[zi][:], func=AF.Exp),
                               reads=[("bank", zi)], writes=[("e_sb", h * 2 + cc)])
                      for cc in ccs:
                          P.op("pe", lambda e: e.matmul(banks[ACC][:], ones_b[:], e_sb[h * 2 + cc][:], start=(cc == 0), stop=(cc == ccs[-1])),
                               reads=["ones_b", ("e_sb", h * 2 + cc)], writes=[("bank", ACC)])
                      P.op("dve", lambda e: e.tensor_scalar_max(out=rec[:], in0=banks[ACC][:], scalar1=1e-30),
                           reads=[("bank", ACC)], writes=["rec"])
                      P.op("dve", lambda e: e.reciprocal(out=rec[:], in_=rec[:]), reads=["rec"], writes=["rec"])
                      for cc in ccs:
                          P.op("dve" if cc == 0 else "pool",
                               lambda e: e.tensor_tensor(out=e_sb[h * 2 + cc][:], in0=e_sb[h * 2 + cc][:], in1=rec[:], op=ALU.mult),
                               reads=[("e_sb", h * 2 + cc), "rec"], writes=[("e_sb", h * 2 + cc)])
                      if h // 2 == hh_:
                          hl = h % 2
                          for cc in ccs:
                              P.op("pe", lambda e: e.matmul(banks[5][0:64, :], vcs[:, cc, :], e_sb[h * 2 + cc][:],
                                                            start=(cc == 0), stop=(cc == ccs[-1])),
                                   reads=["vcs", ("e_sb", h * 2 + cc)], writes=[("bank", 5)])
                          P.op("dve", lambda e: e.tensor_tensor(out=ya[hl * 64:hl * 64 + 64, :], in0=banks[5][0:64, :], in1=gl[0 * 2 + hl][:],
                                                                op=ALU.mult),
                               reads=[("bank", 5), ("gl", hl)], writes=["ya"])
                  for sub in range(4):
                      i_blk = tt * 4 + sub
                      first_mm = True
                      nmm = 4 * len(ccs)
                      k_ = 0
                      for h in range(4):
                          for cc in ccs:
                              k_ += 1
                              P.op("pe", lambda e: e.matmul(banks[6][:, 0:128], e_sb[h * 2 + cc][:, sub * 128:(sub + 1) * 128], ps_sb[:, cc, :],
                                                            start=first_mm, stop=(k_ == nmm)),
                                   reads=[("e_sb", h * 2 + cc), "ps_sb"], writes=[("bank", 6)])
                              first_mm = False
                      o0 = 128 - 2 * i_blk
                      P.op("dve", lambda e: e.tensor_tensor(out=adj[:], in0=banks[6][:, 0:128], in1=keep_sb[:, o0:o0 + 128], op=ALU.mult),
                           reads=[("bank", 6), "keep_sb"], writes=["adj"])
                      P.op("dve", lambda e: e.tensor_tensor(out=adj[:], in0=adj[:], in1=add_sb[:, o0:o0 + 128], op=ALU.add),
                           reads=["adj", "add_sb"], writes=["adj"])
                      P.op("dve", lambda e: e.memset(adj[:, 0:1], 1e4), reads=["adj"], writes=["adj"])
                      P.op("dve", lambda e: e.max(out=m8a[:], in_=adj[:]), reads=["adj"], writes=["m8a"])
                      P.op("dve", lambda e: e.match_replace(out=adj2[:], in_to_replace=m8a[:], in_values=adj[:], imm_value=-1e9),
                           reads=["adj", "m8a"], writes=["adj2"])
                      P.op("dve", lambda e: e.max(out=m8b[:], in_=adj2[:]), reads=["adj2"], writes=["m8b"])
                      P.op("dve", lambda e: e.tensor_scalar(out=mt[:], in0=adj[:], scalar1=m8b[:, 7:8], scalar2=-1.0,
                                                            op0=ALU.is_ge, op1=ALU.add),
                           reads=["adj", "m8b"], writes=["mt"])
                      P.op("pe", lambda e: e.transpose(bank4b[:, 0:128], mt[:], ident_b[:]),
                           reads=["mt", "ident_b"], writes=[("bank", 4)])
                      P.op("act", lambda e: e.activation(out=mneg[:, sub * 128:(sub + 1) * 128], in_=bank4b[:, 0:128], func=AF.Copy),
                           reads=[("bank", 4)], writes=["mneg"])
                  for hl in range(2):
                      h = 2 * hh_ + hl
                      chunks = [(kc, (MK_NONSTRICT + kc - 4 * tt) if kc >= 4 * tt else None) for kc in range(0, 4 * tt + 4)]
                      softmax_tile(qa[h][:], ("qa", h), ksel, "ksel", vsel, "vsel", chunks, selmask=mneg)
                      P.op("dve", lambda e: e.tensor_tensor(out=tmp64[:], in0=o_sb[:], in1=gl[1 * 2 + hl][:], op=ALU.mult),
                           reads=["o_sb", ("gl", 2 + hl)], writes=["tmp64"])
                      P.op("dve", lambda e: e.tensor_copy(ocmp[:, hl, :], tmp64[:]), reads=["tmp64"], writes=["ocmp"])
                      chunks = []
                      for kc in range(max(0, 4 * tt - 4), 4 * tt + 4):
                          r = kc - 4 * tt
                          chunks.append((kc, MK_WINA + r + 4 if r < 0 else MK_NONSTRICT + r))
                      softmax_tile(qa[h][:], ("qa", h), kwin, "kwin", vwin, "vwin", chunks)
                      P.op("dve", lambda e: e.tensor_tensor(out=tmp64[:], in0=o_sb[:], in1=gl[2 * 2 + hl][:], op=ALU.mult),
                           reads=["o_sb", ("gl", 4 + hl)], writes=["tmp64"])
                      P.op("dve", lambda e: e.tensor_tensor(out=tmp64[:], in0=tmp64[:], in1=ocmp[:, hl, :], op=ALU.add),
                           reads=["tmp64", "ocmp"], writes=["tmp64"])
                      P.op("dve", lambda e: e.tensor_copy(ocmp[:, hl, :], ya[hl * 64:hl * 64 + 64, :]), reads=["ya"], writes=["ocmp"])
                      P.op("dve", lambda e: e.tensor_tensor(out=ya[hl * 64:hl * 64 + 64, :], in0=tmp64[:], in1=ocmp[:, hl, :], op=ALU.add),
                           reads=["tmp64", "ocmp"], writes=["ya"])
                  P.dma("sp", zt_sb[:], fmv[FM_AZ * 128:(FM_AZ + 1) * 128, ts_], writes=["zt_sb"])
                  P.op("act", lambda e: e.activation(out=zs_sb[:], in_=zt_sb[:], func=AF.Silu), reads=["zt_sb"], writes=["zs_sb"])
                  P.op("dve", lambda e: e.tensor_tensor(out=yo[:], in0=ya[:], in1=zs_sb[:], op=ALU.mult),
                       reads=["ya", "zs_sb"], writes=["yo"])
                  P.dma("pool", yzd[0:128, ts_], yo[:], reads=["yo"], writes=[("yzd", 0, tt)])
              P.barrier()
        if "C" in mixers:
          with ExitStack() as c:
            ntri_sb = sb(c, "ntri_sb", [128, 128], BF16)
            P.dma("sp", ntri_sb[:], ntri[:, :], writes=["ntri_sb"])
            qs = sb(c, "qs", [64, S], BF16)
            ks = sb(c, "ks", [64, S], BF16)
            vs = sb(c, "vs", [128, NB, 64], BF16)
            ee = [sb(c, "ee%d" % i, [128, TT], F32) for i in range(2)]
            sp_ = [sb(c, "sp%d" % i, [128, TT], F32) for i in range(2)]
            hi = [sb(c, "hi%d" % i, [128, TT], BF16) for i in range(3)]
            lo = [sb(c, "lo%d" % i, [128, TT], BF16) for i in range(3)]
            ww = [sb(c, "ww%d" % i, [64, TT], F32) for i in range(3)]
            aa = [sb(c, "aa%d" % i, [128, TT], BF16) for i in range(2)]
            tmpc = sb(c, "tmpc", [64, TT], F32)
            oacc = sb(c, "oacc", [64, TT], F32)
            zt_c = sb(c, "zt_c", [64, TT], BF16)
            zs_c = sb(c, "zs_c", [64, TT], F32)
            yo_c = sb(c, "yo_c", [64, TT], BF16)
            CARRY = 4
            for hl in range(2):
                P.dma("sp", qs[:], fmv[FM_CQ * 128 + hl * 64:FM_CQ * 128 + hl * 64 + 64, :], writes=["qs"])
                P.dma("sp", ks[:], fmv[FM_CK * 128 + hl * 64:FM_CK * 128 + hl * 64 + 64, :], writes=["ks"])
                P.dma("sp", vs[:], tmv[:, :, TM_CV0 + 64 * hl:TM_CV0 + 64 * hl + 64], writes=["vs"])
                recs = []
                for tt in range(NT):
                    kcs = list(range(4 * tt + 3, -1, -1))
                    for j, kc in enumerate(kcs):
                        recs.append(dict(tt=tt, kc=kc, first=(j == 0), last=(j == len(kcs) - 1), i=len(recs)))

                def s1(r):
                    i = r["i"]; zi = i % 4; tt = r["tt"]; kc = r["kc"]
                    diag = kc >= 4 * tt
                    P.op("pe", lambda e: e.matmul(banks[zi][:], ks[:, kc * 128:(kc + 1) * 128], qs[:, tt * TT:(tt + 1) * TT],
                                                  start=True, stop=not diag),
                         reads=["ks", "qs"], writes=[("bank", zi)])
                    if diag:
                        P.op("pe", lambda e: e.matmul(banks[zi][:], ident_b[:], masks_sb[:, MK_STRICT + kc - 4 * tt, :], start=False, stop=True),
                             reads=["ident_b", "masks_sb"], writes=[("bank", zi)])

                def s2(r):
                    i = r["i"]; zi = i % 4
                    P.op("act", lambda e: e.activation(out=ee[i % 2][:], in_=banks[zi][:], func=AF.Exp),
                         reads=[("bank", zi)], writes=[("ee", i % 2)])
                    P.op("act", lambda e: e.activation(out=sp_[i % 2][:], in_=ee[i % 2][:], func=AF.Ln, bias=1.0),
                         reads=[("ee", i % 2)], writes=[("sp", i % 2)])
                    P.op("dve", lambda e: e.tensor_copy(hi[i % 3][:], sp_[i % 2][:]), reads=[("sp", i % 2)], writes=[("hi", i % 3)])
                    P.op("pool", lambda e: e.tensor_tensor(out=lo[i % 3][:], in0=sp_[i % 2][:], in1=hi[i % 3][:], op=ALU.subtract),
                         reads=[("sp", i % 2), ("hi", i % 3)], writes=[("lo", i % 3)])

                def s3(r):
                    i = r["i"]; zi = i % 4
                    if not r["first"]:
                        P.op("act", lambda e: e.activation(out=ww[i % 3][:], in_=banks[CARRY][0:64, :], func=AF.Exp, scale=-1.0),
                             reads=[("bank", CARRY)], writes=[("ww", i % 3)])
                    P.op("pe", lambda e: e.matmul(banks[zi][:], ntri_sb[:], hi[i % 3][:], start=False, stop=False),
                         reads=["ntri_sb", ("hi", i % 3)], writes=[("bank", zi)])
                    P.op("pe", lambda e: e.matmul(banks[zi][:], ntri_sb[:], lo[i % 3][:], start=False, stop=True),
                         reads=["ntri_sb", ("lo", i % 3)], writes=[("bank", zi)])
                    if not r["last"]:
                        P.op("pe", lambda e: e.matmul(banks[CARRY][0:64, :], ones_b[:, 0:64], hi[i % 3][:], start=r["first"], stop=False),
                             reads=["ones_b", ("hi", i % 3)], writes=[("bank", CARRY)])
                        P.op("pe", lambda e: e.matmul(banks[CARRY][0:64, :], ones_b[:, 0:64], lo[i % 3][:], start=False, stop=True),
                             reads=["ones_b", ("lo", i % 3)], writes=[("bank", CARRY)])

                def s4(r):
                    i = r["i"]; zi = i % 4; tt = r["tt"]; kc = r["kc"]
                    pb = 5 + i % 2
                    P.op("act", lambda e: e.activation(out=aa[i % 2][:], in_=banks[zi][:], func=AF.Exp),
                         reads=[("bank", zi)], writes=[("aa", i % 2)])
                    P.op("pe", lambda e: e.matmul(banks[pb][0:64, :], vs[:, kc, :], aa[i % 2][:], start=True, stop=True),
                         reads=["vs", ("aa", i % 2)], writes=[("bank", pb)])
                    if r["first"]:
                        P.op("dve", lambda e: e.tensor_copy(oacc[:], banks[pb][0:64, :]), reads=[("bank", pb)], writes=["oacc"])
                    else:
                        P.op("dve", lambda e: e.tensor_tensor(out=tmpc[:], in0=banks[pb][0:64, :], in1=ww[i % 3][:], op=ALU.mult),
                             reads=[("bank", pb), ("ww", i % 3)], writes=["tmpc"])
                        P.op("pool", lambda e: e.tensor_tensor(out=oacc[:], in0=oacc[:], in1=tmpc[:], op=ALU.add),
                             reads=["oacc", "tmpc"], writes=["oacc"])
                    if r["last"]:
                        ts_ = slice(tt * TT, (tt + 1) * TT)
                        r0 = FM_CZ * 128 + hl * 64
                        P.dma("sp", zt_c[:], fmv[r0:r0 + 64, ts_], writes=["zt_c"])
                        P.op("act", lambda e: e.activation(out=zs_c[:], in_=zt_c[:], func=AF.Silu), reads=["zt_c"], writes=["zs_c"])
                        P.op("dve", lambda e: e.tensor_tensor(out=yo_c[:], in0=oacc[:], in1=zs_c[:], op=ALU.mult),
                             reads=["oacc", "zs_c"], writes=["yo_c"])
                        P.dma("pool", yzd[256 + hl * 64:256 + hl * 64 + 64, ts_], yo_c[:], reads=["yo_c"], writes=[("yzd", 2, hl, tt)])

                n = len(recs)
                for step in range(n + 3):
                    for lag, fn in ((0, s1), (1, s2), (2, s3), (3, s4)):
                        j = step - lag
                        if 0 <= j < n:
                            fn(recs[j])
            P.barrier()

        if "D" in mixers:
          with ExitStack() as c:
            qd = sb(c, "qd", [64, S], BF16)
            kd = sb(c, "kd", [64, S], BF16)
            vd = sb(c, "vd", [128, NB, 64], BF16)
            kt = sb(c, "kt", [128, NB, 64], BF16)
            vz = sb(c, "vz", [128, NB, 64], BF16)
            dt_sb = sb(c, "dt_sb", [128, 2, 128], F32)
            ze_sb = sb(c, "ze_sb", [128, 2], F32)
            xi_sb = sb(c, "xi_sb", [64, 2, 128], F32)
            gc_sb = sb(c, "gc_sb", [64, 2], F32)
            P.dma("sp", dt_sb[:], dtab[:, :, :], writes=["dt_sb"])
            P.dma("sp", ze_sb[:], zeta[:, :], writes=["ze_sb"])
            P.dma("sp", xi_sb[:], xi_bc[:, :, :], writes=["xi_sb"])
            P.dma("sp", gc_sb[:], gchunk[:, :], writes=["gc_sb"])
            uall = sb(c, "uall", [64, NB, 64], F32)
            rall = sb(c, "rall", [64, NB, 64], F32)
            rbf = sb(c, "rbf", [64, NB, 64], BF16)
            sm = [sb(c, "sm%d" % i, [128, TT], BF16) for i in range(2)]
            od = sb(c, "od", [64, TT], F32)
            od2 = sb(c, "od2", [64, TT], F32)
            mean = sb(c, "mean", [64, TT], F32)
            var = sb(c, "var", [64, TT], F32)
            zt_d = sb(c, "zt_d", [64, TT], BF16)
            zs_d = sb(c, "zs_d", [64, TT], F32)
            yo_d = sb(c, "yo_d", [64, TT], BF16)
            for hl in range(2):
                P.dma("sp", qd[:], fmv[FM_DQ * 128 + hl * 64:FM_DQ * 128 + hl * 64 + 64, :], writes=["qd"])
                P.dma("sp", kd[:], fmv[FM_DK * 128 + hl * 64:FM_DK * 128 + hl * 64 + 64, :], writes=["kd"])
                P.dma("sp", vd[:], tmv[:, :, TM_DV0 + 64 * hl:TM_DV0 + 64 * hl + 64], writes=["vd"])
                P.dma("sp", kt[:], tmv[:, :, TM_DK0 + 64 * hl:TM_DK0 + 64 * hl + 64], writes=["kt"])
                P.op("dve", lambda e: e.tensor_scalar(out=vz[:], in0=vd[:], scalar1=ze_sb[:, hl:hl + 1], scalar2=None, op0=ALU.mult),
                     reads=["vd", "ze_sb"], writes=["vz"])
                for g in range(NB // 8):
                    bi = g % 2
                    for j in range(8):
                        n_ = g * 8 + j
                        P.op("pe", lambda e: e.matmul(banks[bi][0:64, j * 64:(j + 1) * 64], kt[:, n_, :], vz[:, n_, :],
                                                      start=(j == 0), stop=(j == 7)),
                             reads=["kt", "vz"], writes=[("bank", bi)])
                    P.op("act", lambda e: e.activation(out=uall[:, g * 8:(g + 1) * 8, :],
                                                       in_=banks[bi][0:64, :].rearrange("p (n e) -> p n e", e=64), func=AF.Copy),
                         reads=[("bank", bi)], writes=["uall"])
                P.op("dve", lambda e: e.memset(rall[:, 0, :], 0.0), writes=["rall"])
                for n_ in range(1, NB):
                    P.op("dve", lambda e: e.scalar_tensor_tensor(out=rall[:, n_, :], in0=rall[:, n_ - 1, :], scalar=gc_sb[:, hl:hl + 1],
                                                                 in1=uall[:, n_ - 1, :], op0=ALU.mult, op1=ALU.add),
                         reads=["rall", "uall", "gc_sb"], writes=["rall"])
                P.op("dve", lambda e: e.tensor_copy(rbf[:], rall[:]), reads=["rall"], writes=["rbf"])
                for tt in range(NT):
                    ts_ = slice(tt * TT, (tt + 1) * TT)
                    si = tt % 2
                    zi = tt % 2
                    for sub in range(4):
                        n_ = tt * 4 + sub
                        cs_ = slice(n_ * 128, (n_ + 1) * 128)
                        P.op("pe", lambda e: e.matmul(banks[zi][:, sub * 128:(sub + 1) * 128], kd[:, cs_], qd[:, cs_],
                                                      start=(sub == 0), stop=(sub == 3)),
                             reads=["kd", "qd"], writes=[("bank", zi)])
                    P.op("dve", lambda e: e.tensor_tensor(out=sm[si][:].rearrange("p (n i) -> p n i", i=128),
                                                          in0=banks[zi][:].rearrange("p (n i) -> p n i", i=128),
                                                          in1=_bc(dt_sb[:, hl, :], [128, 4, 128], 1), op=ALU.mult),
                         reads=[("bank", zi), "dt_sb"], writes=[("sm", si)])
                    for sub in range(4):
                        n_ = tt * 4 + sub
                        cs_ = slice(n_ * 128, (n_ + 1) * 128)
                        P.op("pe", lambda e: e.matmul(banks[2 + zi][0:64, sub * 128:(sub + 1) * 128], vd[:, n_, :], sm[si][:, sub * 128:(sub + 1) * 128],
                                                      start=(sub == 0), stop=(sub == 3)),
                             reads=["vd", ("sm", si)], writes=[("bank", 2 + zi)])
                    for sub in range(4):
                        n_ = tt * 4 + sub
                        cs_ = slice(n_ * 128, (n_ + 1) * 128)
                        P.op("pe", lambda e: e.matmul(banks[4 + zi][0:64, sub * 128:(sub + 1) * 128], rbf[:, n_, :], qd[:, cs_],
                                                      start=(sub == 0), stop=(sub == 3)),
                             reads=["rbf", "qd"], writes=[("bank", 4 + zi)])
                    P.op("dve", lambda e: e.tensor_tensor(out=od[:].rearrange("p (n i) -> p n i", i=128),
                                                          in0=banks[4 + zi][0:64, :].rearrange("p (n i) -> p n i", i=128),
                                                          in1=_bc(xi_sb[:, hl, :], [64, 4, 128], 1), op=ALU.mult),
                         reads=[("bank", 4 + zi), "xi_sb"], writes=["od"])
                    P.op("dve", lambda e: e.tensor_tensor(out=od[:], in0=od[:], in1=banks[2 + zi][0:64, :], op=ALU.add),
                         reads=["od", ("bank", 2 + zi)], writes=["od"])
                    P.op("act", lambda e: e.activation(out=od2[:], in_=od[:], func=AF.Square), reads=["od"], writes=["od2"])
                    P.op("pe", lambda e: e.matmul(banks[6][0:64, :], ones_f[0:64, 0:64], od[:], start=True, stop=True),
                         reads=["ones_f", "od"], writes=[("bank", 6)])
                    P.op("pe", lambda e: e.matmul(banks[7][0:64, :], ones_f[0:64, 0:64], od2[:], start=True, stop=True),
                         reads=["ones_f", "od2"], writes=[("bank", 7)])
                    P.op("act", lambda e: e.activation(out=mean[:], in_=banks[6][0:64, :], func=AF.Copy, scale=1.0 / 64),
                         reads=[("bank", 6)], writes=["mean"])
                    P.op("dve", lambda e: e.tensor_tensor(out=var[:], in0=mean[:], in1=mean[:], op=ALU.mult),
                         reads=["mean"], writes=["var"])
                    P.op("dve", lambda e: e.scalar_tensor_tensor(out=var[:], in0=banks[7][0:64, :], scalar=1.0 / 64, in1=var[:],
                                                                 op0=ALU.mult, op1=ALU.subtract),
                         reads=[("bank", 7), "var"], writes=["var"])
                    P.op("act", lambda e: e.activation(out=var[:], in_=var[:], func=AF.Sqrt, bias=LN_EPS), reads=["var"], writes=["var"])
                    P.op("dve", lambda e: e.reciprocal(out=var[:], in_=var[:]), reads=["var"], writes=["var"])
                    P.op("dve", lambda e: e.tensor_tensor(out=od[:], in0=od[:], in1=mean[:], op=ALU.subtract),
                         reads=["od", "mean"], writes=["od"])
                    P.op("dve", lambda e: e.tensor_tensor(out=od[:], in0=od[:], in1=var[:], op=ALU.mult),
                         reads=["od", "var"], writes=["od"])
                    r0 = FM_DZ * 128 + hl * 64
                    P.dma("sp", zt_d[:], fmv[r0:r0 + 64, ts_], writes=["zt_d"])
                    P.op("act", lambda e: e.activation(out=zs_d[:], in_=zt_d[:], func=AF.Silu), reads=["zt_d"], writes=["zs_d"])
                    P.op("dve", lambda e: e.tensor_tensor(out=yo_d[:], in0=od[:], in1=zs_d[:], op=ALU.mult),
                         reads=["od", "zs_d"], writes=["yo_d"])
                    P.dma("pool", yzd[384 + hl * 64:384 + hl * 64 + 64, ts_], yo_d[:], reads=["yo_d"], writes=[("yzd", 3, hl, tt)])
            P.barrier()
        stage2.close()
        if stop_after <= 2:
            P.finish()
            return nc

        with ExitStack() as c:
            wm = sb(c, "wm", [128, 4, 8, D], BF16)
            wo = sb(c, "wo", [128, 8, D], BF16)
            wbr = sb(c, "wbr", [128, 4, D], BF16)
            stg = [sb(c, "stg3_%d" % i, [128, D], F32) for i in range(2)]
            k_ = 0
            for i in range(4):
                for kc in range(8):
                    load_cast(c, stg, wm[:, i, kc, :], w_merge[i, kc * 128:(kc + 1) * 128, :], D, ("wm", i, kc), k_)
                    k_ += 1
                load_cast(c, stg, wbr[:, i, :], w_br[i, :, :], D, ("wbr", i), k_)
                k_ += 1
            for kc in range(8):
                load_cast(c, stg, wo[:, kc, :], w_out[kc * 128:(kc + 1) * 128, :], D, ("wo", kc), k_)
                k_ += 1
            hb3 = [sb(c, "hb3_%d" % i, [128, 8, TT], BF16) for i in range(2)]
            yz3 = [sb(c, "yz3_%d" % i, [128, 4, TT], BF16) for i in range(2)]
            sg = [sb(c, "sg%d" % i, [128, TT], F32) for i in range(2)]
            term = [sb(c, "term%d" % i, [128, TT], F32) for i in range(2)]
            mrg = sb(c, "mrg", [128, 8, TT], F32)
            mg = sb(c, "mg", [128, 8, TT], BF16)
            po = [sb(c, "po%d" % i, [128, TT], F32) for i in range(2)]
            hview = hT.rearrange("(k p) t -> p k t", p=128)
            yview = yzd.rearrange("(m p) t -> p m t", p=128)
            pview = partT.rearrange("(k p) t -> p k t", p=128)
            nb_ = 0
            ns_ = 0
            for tt in range(NT):
                i2 = tt % 2
                ts_ = slice(tt * TT, (tt + 1) * TT)
                P.dma("sp", hb3[i2][:], hview[:, :, ts_], writes=[("hb3", i2)])
                P.dma("sp", yz3[i2][:], yview[:, :, ts_], writes=[("yz3", i2)])
                for oc in range(8):
                    for i in range(4):
                        bg = nb_ % 6
                        nb_ += 1
                        for kc in range(8):
                            P.op("pe", lambda e: e.matmul(banks[bg][:], wm[:, i, kc, oc * 128:(oc + 1) * 128], hb3[i2][:, kc, :],
                                                          start=(kc == 0), stop=(kc == 7)),
                                 reads=[("wm", i, kc), ("hb3", i2)], writes=[("bank", bg)])
                        bb = 6 + ns_ % 2
                        P.op("pe", lambda e: e.matmul(banks[bb][:], wbr[:, i, oc * 128:(oc + 1) * 128], yz3[i2][:, i, :], start=True, stop=True),
                             reads=[("wbr", i), ("yz3", i2)], writes=[("bank", bb)])
                        si = ns_ % 2
                        ns_ += 1
                        P.op("act", lambda e: e.activation(out=sg[si][:], in_=banks[bg][:], func=AF.Sigmoid),
                             reads=[("bank", bg)], writes=[("sg", si)])
                        if i == 0:
                            P.op("dve", lambda e: e.tensor_tensor(out=mrg[:, oc, :], in0=banks[bb][:], in1=sg[si][:], op=ALU.mult),
                                 reads=[("bank", bb), ("sg", si)], writes=[("mrg", oc)])
                        else:
                            P.op("dve", lambda e: e.tensor_tensor(out=term[si][:], in0=banks[bb][:], in1=sg[si][:], op=ALU.mult),
                                 reads=[("bank", bb), ("sg", si)], writes=[("term", si)])
                            P.op("pool", lambda e: e.tensor_tensor(out=mrg[:, oc, :], in0=mrg[:, oc, :], in1=term[si][:], op=ALU.add),
                                 reads=[("mrg", oc), ("term", si)], writes=[("mrg", oc)])
                    P.op("pool", lambda e: e.tensor_copy(mg[:, oc, :], mrg[:, oc, :]), reads=[("mrg", oc)], writes=[("mg", oc)])
                for oc2 in range(8):
                    bg = nb_ % 6
                    nb_ += 1
                    for kc in range(8):
                        P.op("pe", lambda e: e.matmul(banks[bg][:], wo[:, kc, oc2 * 128:(oc2 + 1) * 128], mg[:, kc, :],
                                                      start=(kc == 0), stop=(kc == 7)),
                             reads=[("wo", kc), ("mg", kc)], writes=[("bank", bg)])
                    pi = oc2 % 2
                    P.op("act", lambda e: e.activation(out=po[pi][:], in_=banks[bg][:], func=AF.Copy, scale=mod[:, 16 + oc2:17 + oc2]),
                         reads=[("bank", bg), "mod"], writes=[("po", pi)])
                    P.dma("pool", pview[:, oc2, ts_], po[pi][:], reads=[("po", pi)], is_output=True)
            P.barrier()
        P.finish()
    return nc


def _bf(a):
    return np.asarray(a, dtype=np.float32).astype(ml_dtypes.bfloat16)


def make_consts():
    sl = np.arange(128)[:, None]
    tl = np.arange(TT)[None, :]
    m = np.zeros((NMASK, 128, TT), np.float32)

    def put(idx, ok):
        m[idx] = np.where(ok, 0.0, NEG)
    for r in range(4):
        put(MK_STRICT + r, sl + 128 * r < tl)
        put(MK_NONSTRICT + r, sl + 128 * r <= tl)
    for j, r in enumerate(range(-4, 0)):
        d = tl - sl - 128 * r
        put(MK_WINA + j, (d >= 0) & (d < 512))
    for j, r in enumerate(range(-1, 4)):
        d = tl - sl - 128 * r
        put(MK_WINB + j, (d >= 0) & (d < 128))
    for mm in range(8):
        put(MK_CMP + mm, tl + 512 * mm - 32 * sl - 31 >= 0)
    t = np.arange(S)
    slopes = 2.0 ** (-8.0 * (np.arange(4) + 1) / 4)
    qpos = np.zeros((4, 4, S), np.float32)
    for h in range(4):
        qpos[h, 0] = -slopes[h] * 64 * (t // 64)
        qpos[h, 1] = -slopes[h] * (t % 64)
        qpos[h, 2] = slopes[h] * 64
        qpos[h, 3] = slopes[h]
    kpos_tok = np.stack([np.ones(S), np.ones(S), t // 64, t % 64]).astype(np.float32)
    pc = 32 * np.arange(256) + 31
    kpos_cmp = np.stack([np.ones(256), np.ones(256), pc // 64, pc % 64]).astype(np.float32)
    ewide = 30000.0 * (np.arange(S)[None, :] // 64 == np.arange(128)[:, None]).astype(np.float32)
    ident = np.eye(128, dtype=np.float32)
    ntri = -(np.arange(128)[:, None] >= np.arange(128)[None, :]).astype(np.float32)
    pairsum = np.zeros((2, 128, 128), np.float32)
    for cc in range(2):
        pairsum[cc, np.arange(128), 64 * cc + np.arange(128) // 2] = 1.0
    u = np.arange(256)[None, :] - 128
    cur = (np.arange(128)[:, None] >= 64).astype(np.int64)
    forced = (u == cur) | (u == cur - 1)
    future = u > cur
    selkeep = (~(forced | future)).astype(np.float32)
    seladd = np.where(forced, 1e4, np.where(future, -1.0, 0.0)).astype(np.float32)
    return dict(masks=_bf(m), qpos_all=qpos, kpos_tok=_bf(kpos_tok), kpos_cmp=_bf(kpos_cmp), ewide=_bf(ewide),
                ident=_bf(ident), ntri=_bf(ntri), pairsum=_bf(pairsum), selkeep=selkeep, seladd=seladd)


def ret_consts(hh):
    out = {}
    i = np.arange(128)
    dt_ = np.zeros((128, 2, 128), np.float32)
    zt = np.zeros((128, 2), np.float32)
    xi = np.zeros((64, 2, 128), np.float32)
    gc = np.zeros((64, 2), np.float32)
    for hl in range(2):
        h = 2 * hh + hl
        log_g = np.log(np.float32(1.0) - np.float32(2.0 ** (-5.0 - h)))
        diff = (i[None, :] - i[:, None]).astype(np.float32)
        dt_[:, hl, :] = np.where(diff >= 0, np.exp(log_g * np.maximum(diff, 0.0)), 0.0)
        zt[:, hl] = np.exp(log_g * (127 - i))
        xi[:, hl, :] = np.exp(log_g * (i + 1))[None, :]
        gc[:, hl] = np.exp(log_g * 128)
    return dict(dtab=dt_, zeta=zt, xi_bc=xi, gchunk=gc)


def arrange_w_in(w, hh):
    o = np.cumsum([0, 256, 384, 12, 256, 256, 128, 128, 256, 256, 256, 256, 256, 256, 256, 256, 256])
    a_q, a_kv, a_g, a_z, b_q, b_k, b_v, b_z, c_q, c_k, c_v, c_z, d_q, d_k, d_v, d_z = [
        w[:, o[i]:o[i + 1]] for i in range(16)]
    own = slice(128 * hh, 128 * hh + 128)
    kvh = slice(64 * hh, 64 * hh + 64)
    k_cmp, v_cmp, k_sel, v_sel, k_win, v_win = [a_kv[:, 64 * i:64 * i + 64] for i in range(6)]
    gcols = []
    for j in range(3):
        g = np.concatenate([np.repeat(a_g[:, 3 * (2 * hh + hl) + j][:, None], 64, axis=1) for hl in range(2)], axis=1)
        gcols.append(g)
    zpad = np.zeros((D, 64), np.float32)
    oth = slice(128 * (1 - hh), 128 * (1 - hh) + 128)
    fmt = [a_q[:, own], a_q[:, oth], np.concatenate([k_cmp, v_cmp], 1), np.concatenate([k_sel, k_win], 1),
           gcols[0], gcols[1], gcols[2], a_z[:, own], b_q[:, own], np.concatenate([b_k[:, kvh], zpad], 1), b_z[:, own],
           c_q[:, own], c_k[:, own], c_z[:, own], d_q[:, own], d_k[:, own], d_z[:, own]]
    tmc = [v_sel, v_win, b_v[:, kvh], c_v[:, own], d_v[:, own], d_k[:, own]]
    return np.ascontiguousarray(np.concatenate(fmt + tmc, axis=1), dtype=np.float32)


def layer_inputs(l, b, hh, c, w_ada, b_ada, norm_g, w_in, cmp_pos, cmp_w1, cmp_w2, sink, w_merge, w_br, w_out, consts):
    d = dict(consts)
    qp = d.pop("qpos_all")
    order = [2 * hh, 2 * hh + 1, 2 * (1 - hh), 2 * (1 - hh) + 1]
    d["qpos"] = _bf(qp[order])
    d.update(ret_consts(hh))
    d["cT"] = np.ascontiguousarray(c[b].reshape(8, 128).T)
    d["w_ada"] = np.ascontiguousarray(w_ada[l])
    d["b_adaT"] = np.ascontiguousarray(b_ada[l].reshape(24, 128).T)
    d["gT"] = np.ascontiguousarray(norm_g[l].reshape(8, 128).T)
    d["w_in"] = arrange_w_in(w_in[l], hh)
    d["cmp_posT"] = np.ascontiguousarray(cmp_pos[l].transpose(2, 0, 1))
    d["cmp_w1"] = np.ascontiguousarray(cmp_w1[l].reshape(2, 32, 64, 64).transpose(2, 0, 1, 3))
    d["cmp_w2"] = np.ascontiguousarray(cmp_w2[l].transpose(1, 0, 2))
    d["sinkb"] = np.ascontiguousarray(np.repeat(sink[l][2 * hh:2 * hh + 2], 64)[:, None].astype(np.float32))
    d["w_merge"] = np.ascontiguousarray(w_merge[l])
    d["w_br"] = np.ascontiguousarray(w_br[l][:, 128 * hh:128 * hh + 128, :])
    d["w_out"] = np.ascontiguousarray(w_out[l])
    return d


_PROG_CACHE = {}


def _prog(first, last_only):
    key = (first, last_only)
    if key not in _PROG_CACHE:
        _PROG_CACHE[key] = build_program(first, last_only)
    return _PROG_CACHE[key]


def kernel(x, c, w_ada, b_ada, norm_g, w_in, cmp_pos, cmp_w1, cmp_w2, sink, w_merge, w_br, w_out, final_g):
    args = [np.asarray(a, dtype=np.float32) for a in
            (x, c, w_ada, b_ada, norm_g, w_in, cmp_pos, cmp_w1, cmp_w2, sink, w_merge, w_br, w_out, final_g)]
    x, c, w_ada, b_ada, norm_g, w_in, cmp_pos, cmp_w1, cmp_w2, sink, w_merge, w_br, w_out, final_g = args
    B = x.shape[0]
    depth = w_ada.shape[0]
    consts = make_consts()
    cores = [(b, hh) for b in range(B) for hh in range(2)]
    xT = [np.ascontiguousarray(x[b].T) for b in range(B)]
    zero = np.zeros((D, S), np.float32)
    parts = [zero] * (2 * B)
    for l in range(depth):
        in_maps = []
        for (b, hh) in cores:
            d = layer_inputs(l, b, hh, c, w_ada, b_ada, norm_g, w_in, cmp_pos, cmp_w1, cmp_w2, sink, w_merge, w_br, w_out, consts)
            d["xT"] = xT[b]
            d["paT"] = parts[2 * b]
            d["pbT"] = parts[2 * b + 1]
            in_maps.append(d)
        res = run_bass_kernel_spmd(_prog(False, False), in_maps, core_ids=list(range(len(cores))))
        xT = [np.asarray(res.results[2 * b]["xcT"]) for b in range(B)]
        parts = [np.asarray(r["partT"]) for r in res.results]
    in_maps = []
    fgT = np.ascontiguousarray(final_g.reshape(8, 128).T)
    for (b, hh) in cores:
        in_maps.append({"xT": xT[b], "paT": parts[2 * b], "pbT": parts[2 * b + 1], "fgT": fgT})
    res = run_bass_kernel_spmd(_prog(False, True), in_maps, core_ids=list(range(len(cores))))
    out = np.stack([np.asarray(res.results[2 * b]["outT"]).T for b in range(B)]).astype(np.float32)
    return np.ascontiguousarray(out)
```

```python
from contextlib import ExitStack
import numpy as np
import ml_dtypes
import concourse.bass as bass
import concourse.mybir as mybir
from concourse.bass_utils import run_bass_kernel_spmd

F32 = mybir.dt.float32
BF16 = mybir.dt.bfloat16
AF = mybir.ActivationFunctionType
ALU = mybir.AluOpType

D = 1024
S = 8192
NT = 16
TT = 512
NB = 64
NEG = -30000.0
RMS_EPS = 1e-6
LN_EPS = 1e-5

FM_AQ01, FM_AQ23, FM_KVC, FM_KSW, FM_G0, FM_G1, FM_G2, FM_AZ, FM_BQ, FM_BK, FM_BZ, \
    FM_CQ, FM_CK, FM_CZ, FM_DQ, FM_DK, FM_DZ = range(17)
NFM = 17
Q_TILES = (FM_AQ01, FM_AQ23, FM_BQ, FM_CQ, FM_DQ)
TM_VSEL, TM_VWIN, TM_BV, TM_CV0, TM_CV1, TM_DV0, TM_DV1, TM_DK0, TM_DK1 = [64 * i for i in range(9)]
NTM = 576
NCOL = NFM * 128 + NTM

MK_STRICT = 0
MK_NONSTRICT = 4
MK_WINA = 8
MK_WINB = 12
MK_CMP = 17
NMASK = 25


class Prog:
    def __init__(self, nc, ctx, n_dma_sems=24):
        self.nc = nc
        self.eng = {"pe": nc.tensor, "act": nc.scalar, "dve": nc.vector, "pool": nc.gpsimd, "sp": nc.sync}
        self.sems = []
        self.semid = {}
        for e in self.eng:
            self.semid[e] = len(self.sems)
            self.sems.append(ctx.enter_context(nc.semaphore("p_" + e)))
        self.cnt = {e: 0 for e in self.eng}
        self.dsem = []
        for i in range(n_dma_sems):
            self.dsem.append(len(self.sems))
            self.sems.append(ctx.enter_context(nc.semaphore("d%d" % i)))
        self.duse = [0] * n_dma_sems
        self.dnext = 0
        self.known = {e: {} for e in self.eng}
        self.res = {}
        self.out_tokens = []
        self.ninstr = 0

    def _st(self, k):
        st = self.res.get(k)
        if st is None:
            st = [None, {}]
            self.res[k] = st
        return st

    def _deps(self, reads, writes):
        need = {}

        def add(tok):
            s, v = tok
            if need.get(s, 0) < v:
                need[s] = v
        for k in reads:
            st = self._st(k)
            if st[0] is not None:
                add(st[0])
        for k in writes:
            st = self._st(k)
            if st[0] is not None:
                add(st[0])
            for s, v in st[1].items():
                add((s, v))
        return need

    def _wait(self, e, need):
        kn = self.known[e]
        for s, v in need.items():
            if e == "pe" and s == self.semid["pe"]:
                continue
            if kn.get(s, 0) >= v:
                continue
            self.eng[e].wait_ge(self.sems[s], v)
            kn[s] = v
            self.ninstr += 1

    def _record(self, tok, reads, writes):
        for k in reads:
            st = self._st(k)
            if st[1].get(tok[0], 0) < tok[1]:
                st[1][tok[0]] = tok[1]
        for k in writes:
            st = self._st(k)
            st[0] = tok
            st[1] = {}

    def op(self, e, fn, reads=(), writes=()):
        self._wait(e, self._deps(reads, writes))
        ins = fn(self.eng[e])
        self.cnt[e] += 1
        tok = (self.semid[e], self.cnt[e])
        ins.then_inc(self.sems[tok[0]], 1)
        self._record(tok, reads, writes)
        self.ninstr += 1
        return tok

    def dma(self, q, out, in_, reads=(), writes=(), is_output=False):
        k = self.dnext
        self.dnext = (k + 1) % len(self.dsem)
        need = self._deps(reads, writes)
        if self.duse[k] > 0:
            s = self.dsem[k]
            if need.get(s, 0) < 16 * self.duse[k]:
                need[s] = 16 * self.duse[k]
        self._wait(q, need)
        ins = self.eng[q].dma_start(out=out, in_=in_)
        self.duse[k] += 1
        tok = (self.dsem[k], 16 * self.duse[k])
        ins.then_inc(self.sems[tok[0]], 16)
        self._record(tok, reads, writes)
        if is_output:
            self.out_tokens.append(tok)
        self.ninstr += 1
        return tok

    def barrier(self):
        need = {}
        for e in self.eng:
            if self.cnt[e] > 0:
                need[self.semid[e]] = self.cnt[e]
        for k, s in enumerate(self.dsem):
            if self.duse[k] > 0:
                need[s] = 16 * self.duse[k]
        for e in self.eng:
            n2 = {s: v for s, v in need.items() if s != self.semid[e] or e != "pe"}
            self._wait(e, n2)
        self.res = {}

    def finish(self):
        need = {}
        for s, v in self.out_tokens:
            if need.get(s, 0) < v:
                need[s] = v
        self._wait("sp", need)


def _bc(ap, shape, axis):
    return ap.unsqueeze(axis).broadcast_to(shape)


def build_program(first, last_only, dbg=None, stop_after=99, mixers="ABCD"):
    dbg = dbg or set()
    nc = bass.Bass("TRN2", target_bir_lowering=False)

    def din(name, shape, dt=F32):
        return nc.dram_tensor(name, list(shape), dt, kind="ExternalInput").ap()

    def dout(name, shape, dt=F32):
        return nc.dram_tensor(name, list(shape), dt, kind="ExternalOutput").ap()

    def dscr(name, shape, dt):
        kind = "ExternalOutput" if name in dbg else "Internal"
        return nc.dram_tensor(name, list(shape), dt, kind=kind).ap()

    xT = din("xT", [D, S])
    if not first:
        paT = din("paT", [D, S])
        pbT = din("pbT", [D, S])
    if last_only:
        fgT = din("fgT", [128, 8])
        outT = dout("outT", [D, S])
    else:
        cT = din("cT", [128, 8])
        w_ada = din("w_ada", [D, 3 * D])
        b_adaT = din("b_adaT", [128, 24])
        gT = din("gT", [128, 8])
        w_in = din("w_in", [D, NCOL])
        cmp_posT = din("cmp_posT", [64, 2, 32])
        cmp_w1 = din("cmp_w1", [64, 2, 32, 64])
        cmp_w2 = din("cmp_w2", [64, 2, 64])
        sinkb = din("sinkb", [128, 1])
        w_merge = din("w_merge", [4, D, D])
        w_br = din("w_br", [4, 128, D])
        w_out = din("w_out", [D, D])
        masks = din("masks", [NMASK, 128, TT], BF16)
        qpos = din("qpos", [4, 4, S], BF16)
        kpos_tok = din("kpos_tok", [4, S], BF16)
        kpos_cmp = din("kpos_cmp", [4, 256], BF16)
        ewide = din("ewide", [128, S], BF16)
        ident = din("ident", [128, 128], BF16)
        ntri = din("ntri", [128, 128], BF16)
        pairsum = din("pairsum", [2, 128, 128], BF16)
        selkeep = din("selkeep", [128, 256])
        seladd = din("seladd", [128, 256])
        dtab = din("dtab", [128, 2, 128])
        zeta = din("zeta", [128, 2])
        xi_bc = din("xi_bc", [64, 2, 128])
        gchunk = din("gchunk", [64, 2])
        partT = dout("partT", [D, S])
        if not first:
            xcT = dout("xcT", [D, S])
        hT = dscr("hT", [D, S], BF16)
        fm = dscr("fm", [NFM * 128, S], BF16)
        tm = dscr("tm", [S, NTM], BF16)
        yzd = dscr("yzd", [512, S], BF16)

    ctx = ExitStack()
    with ctx:
        P = Prog(nc, ctx)
        banks = [ctx.enter_context(nc.psum_tensor("bank%d" % i, [128, 512], F32)) for i in range(8)]

        def sb(c, name, shape, dt):
            return c.enter_context(nc.sbuf_tensor(name, list(shape), dt))

        ones_f = sb(ctx, "ones_f", [128, 128], F32)
        P.op("dve", lambda e: e.memset(ones_f[:], 1.0), writes=["ones_f"])
        xview = xT.rearrange("(k p) t -> p k t", p=128)

        if last_only:
            with ExitStack() as c:
                fg = sb(c, "fg", [128, 8], F32)
                P.dma("sp", fg[:], fgT[:, :], writes=["fg"])
                pav = paT.rearrange("(k p) t -> p k t", p=128)
                pbv = pbT.rearrange("(k p) t -> p k t", p=128)
                ov = outT.rearrange("(k p) t -> p k t", p=128)
                xt = [sb(c, "xt%d" % i, [128, 8, TT], F32) for i in range(2)]
                pt = [sb(c, "pt%d" % i, [128, 8, TT], F32) for i in range(2)]
                qt = [sb(c, "qt%d" % i, [128, 8, TT], F32) for i in range(2)]
                sq = sb(c, "sq", [128, 8, TT], F32)
                rt = sb(c, "rt", [128, TT], F32)
                rstd = sb(c, "rstd", [128, TT], F32)
                for tt in range(NT):
                    i = tt % 2
                    ts_ = slice(tt * TT, (tt + 1) * TT)
                    P.dma("sp", xt[i][:], xview[:, :, ts_], writes=[("xt", i)])
                    P.dma("sp", pt[i][:], pav[:, :, ts_], writes=[("pt", i)])
                    P.dma("sp", qt[i][:], pbv[:, :, ts_], writes=[("qt", i)])
                    P.op("dve", lambda e: e.tensor_tensor(out=xt[i][:], in0=xt[i][:], in1=pt[i][:], op=ALU.add),
                         reads=[("xt", i), ("pt", i)], writes=[("xt", i)])
                    P.op("dve", lambda e: e.tensor_tensor(out=xt[i][:], in0=xt[i][:], in1=qt[i][:], op=ALU.add),
                         reads=[("xt", i), ("qt", i)], writes=[("xt", i)])
                    P.op("act", lambda e: e.activation(out=sq[:], in_=xt[i][:], func=AF.Square),
                         reads=[("xt", i)], writes=["sq"])
                    bk = banks[tt % 2]
                    for kc in range(8):
                        P.op("pe", lambda e: e.matmul(bk[:], ones_f[:], sq[:, kc, :], start=(kc == 0), stop=(kc == 7)),
                             reads=["sq", "ones_f"], writes=[("bank", tt % 2)])
                    P.op("act", lambda e: e.activation(out=rt[:], in_=bk[:], func=AF.Sqrt, scale=1.0 / D, bias=RMS_EPS),
                         reads=[("bank", tt % 2)], writes=["rt"])
                    P.op("dve", lambda e: e.reciprocal(out=rstd[:], in_=rt[:]), reads=["rt"], writes=["rstd"])
                    P.op("dve", lambda e: e.tensor_tensor(out=xt[i][:], in0=xt[i][:],
                                                          in1=_bc(rstd[:], [128, 8, TT], 1), op=ALU.mult),
                         reads=[("xt", i), "rstd"], writes=[("xt", i)])
                    for kc in range(8):
                        P.op("act", lambda e: e.activation(out=xt[i][:, kc, :], in_=xt[i][:, kc, :], func=AF.Copy,
                                                           scale=fg[:, kc:kc + 1]),
                             reads=[("xt", i), "fg"], writes=[("xt", i)])
                    P.dma("pool", ov[:, :, ts_], xt[i][:], reads=[("xt", i)], is_output=True)
            P.finish()
            return nc


        ident_b = sb(ctx, "ident_b", [128, 128], BF16)
        P.dma("sp", ident_b[:], ident[:, :], writes=["ident_b"])
        ones_b = sb(ctx, "ones_b", [128, 128], BF16)
        P.op("dve", lambda e: e.memset(ones_b[:], 1.0), writes=["ones_b"])
        mod = sb(ctx, "mod", [128, 24], F32)
        gs = sb(ctx, "gs", [128, 8], F32)
        with ExitStack() as c:
            cs = sb(c, "cs", [128, 8], F32)
            csil = sb(c, "csil", [128, 8, 2], F32)
            bada = sb(c, "bada", [128, 24], F32)
            gt_ = sb(c, "gt_", [128, 8], F32)
            wa = [sb(c, "wa%d" % i, [128, 3 * D], F32) for i in range(2)]
            P.dma("sp", cs[:], cT[:, :], writes=["cs"])
            P.dma("sp", bada[:], b_adaT[:, :], writes=["bada"])
            P.dma("sp", gt_[:], gT[:, :], writes=["gt_"])
            for j in range(2):
                P.op("act", lambda e: e.activation(out=csil[:, :, j], in_=cs[:], func=AF.Silu),
                     reads=["cs"], writes=["csil"])
            for kc in range(8):
                i = kc % 2
                P.dma("sp", wa[i][:], w_ada[kc * 128:(kc + 1) * 128, :], writes=[("wa", i)])
                for oc in range(24):
                    P.op("pe", lambda e: e.matmul(banks[0][:, 2 * oc:2 * oc + 2], wa[i][:, oc * 128:(oc + 1) * 128],
                                                  csil[:, kc, :], start=(kc == 0 and oc == 0), stop=(kc == 7 and oc == 23)),
                         reads=[("wa", i), "csil"], writes=[("bank", 0)])
            P.op("dve", lambda e: e.tensor_tensor(out=mod[:], in0=banks[0][:, 0:48:2], in1=bada[:], op=ALU.add),
                 reads=[("bank", 0), "bada"], writes=["mod"])
            P.op("dve", lambda e: e.scalar_tensor_tensor(out=gs[:], in0=mod[:, 8:16], scalar=1.0, in1=gt_[:],
                                                         op0=ALU.add, op1=ALU.mult),
                 reads=["mod", "gt_"], writes=["gs"])
            P.barrier()

        def load_cast(c_stage, stg, dst_ap, src_ap, n, key_dst, idx):
            i = idx % 2
            P.dma("sp", stg[i][:, 0:n], src_ap, writes=[("stg", i)])
            eng = "dve" if idx % 2 == 0 else "pool"
            P.op(eng, lambda e: e.tensor_copy(dst_ap, stg[i][:, 0:n]), reads=[("stg", i)], writes=[key_dst])

        with ExitStack() as c:
            wb = sb(c, "wb", [128, 8, NCOL], BF16)
            stg = [sb(c, "stg%d" % i, [128, NCOL], F32) for i in range(2)]
            for kc in range(8):
                load_cast(c, stg, wb[:, kc, :], w_in[kc * 128:(kc + 1) * 128, :], NCOL, ("wb", kc), kc)
            xt = [sb(c, "xt%d" % i, [128, 8, TT], F32) for i in range(2)]
            if not first:
                pt = sb(c, "pt", [128, 8, TT], F32)
                pav = paT.rearrange("(k p) t -> p k t", p=128)
                pbv = pbT.rearrange("(k p) t -> p k t", p=128)
                xcv = xcT.rearrange("(k p) t -> p k t", p=128)
            sq = sb(c, "sq", [128, 8, TT], F32)
            rt = sb(c, "rt", [128, TT], F32)
            rstd = sb(c, "rstd", [128, TT], F32)
            hb = [sb(c, "hb%d" % i, [128, 8, TT], BF16) for i in range(2)]
            ev = [sb(c, "ev%d" % i, [128, TT], BF16) for i in range(4)]
            evt = [sb(c, "evt%d" % i, [128, NTM], BF16) for i in range(2)]
            hview = hT.rearrange("(k p) t -> p k t", p=128)
            ctr = {'nbank': 0, 'nev': 0}

            def s1_norm(tt):
                i = tt % 2
                ts_ = slice(tt * TT, (tt + 1) * TT)
                P.dma("sp", xt[i][:], xview[:, :, ts_], writes=[("xt", i)])
                if not first:
                    for pv in (pav, pbv):
                        P.dma("sp", pt[:], pv[:, :, ts_], writes=["pt"])
                        P.op("dve", lambda e: e.tensor_tensor(out=xt[i][:], in0=xt[i][:], in1=pt[:], op=ALU.add),
                             reads=[("xt", i), "pt"], writes=[("xt", i)])
                    P.dma("pool", xcv[:, :, ts_], xt[i][:], reads=[("xt", i)], is_output=True)
                P.op("act", lambda e: e.activation(out=sq[:], in_=xt[i][:], func=AF.Square),
                     reads=[("xt", i)], writes=["sq"])
                bi = ctr['nbank'] % 8
                ctr['nbank'] += 1
                for kc in range(8):
                    P.op("pe", lambda e: e.matmul(banks[bi][:], ones_f[:], sq[:, kc, :], start=(kc == 0), stop=(kc == 7)),
                         reads=["sq", "ones_f"], writes=[("bank", bi)])
                P.op("act", lambda e: e.activation(out=rt[:], in_=banks[bi][:], func=AF.Sqrt, scale=1.0 / D, bias=RMS_EPS),
                     reads=[("bank", bi)], writes=["rt"])
                P.op("dve", lambda e: e.reciprocal(out=rstd[:], in_=rt[:]), reads=["rt"], writes=["rstd"])
                P.op("dve", lambda e: e.tensor_tensor(out=sq[:], in0=xt[i][:], in1=_bc(rstd[:], [128, 8, TT], 1), op=ALU.mult),
                     reads=[("xt", i), "rstd", "sq"], writes=["sq"])
                for kc in range(8):
                    P.op("act", lambda e: e.activation(out=hb[i][:, kc, :], in_=sq[:, kc, :], func=AF.Identity,
                                                       scale=gs[:, kc:kc + 1], bias=mod[:, kc:kc + 1]),
                         reads=["sq", "gs", "mod"], writes=[("hb", i)])
                P.dma("pool", hview[:, :, ts_], hb[i][:], reads=[("hb", i)], writes=[("hT", tt)])

            def s1_proj(tt):
                i = tt % 2
                ts_ = slice(tt * TT, (tt + 1) * TT)
                for ct in range(NFM):
                    bi = ctr['nbank'] % 8
                    ctr['nbank'] += 1
                    for kc in range(8):
                        P.op("pe", lambda e: e.matmul(banks[bi][:], wb[:, kc, ct * 128:(ct + 1) * 128], hb[i][:, kc, :],
                                                      start=(kc == 0), stop=(kc == 7)),
                             reads=[("wb", kc), ("hb", i)], writes=[("bank", bi)])
                    ei = ctr['nev'] % 4
                    ctr['nev'] += 1
                    if ct in Q_TILES:
                        P.op("act", lambda e: e.activation(out=ev[ei][:], in_=banks[bi][:], func=AF.Copy, scale=0.125),
                             reads=[("bank", bi)], writes=[("ev", ei)])
                    elif ct % 2 == 0:
                        P.op("act", lambda e: e.activation(out=ev[ei][:], in_=banks[bi][:], func=AF.Copy),
                             reads=[("bank", bi)], writes=[("ev", ei)])
                    else:
                        P.op("dve", lambda e: e.tensor_copy(ev[ei][:], banks[bi][:]),
                             reads=[("bank", bi)], writes=[("ev", ei)])
                    P.dma("pool", fm[ct * 128:(ct + 1) * 128, ts_], ev[ei][:], reads=[("ev", ei)], writes=[("fm", ct, tt)])

            def s1_proj_tm(tt):
                i = tt % 2
                ts_ = slice(tt * TT, (tt + 1) * TT)
                for sub in range(4):
                    ei = (tt * 4 + sub) % 2
                    for g, (c0, c1) in enumerate(((0, 320), (320, 576))):
                        bi = ctr['nbank'] % 8
                        ctr['nbank'] += 1
                        for kc in range(8):
                            P.op("pe", lambda e: e.matmul(banks[bi][:, 0:c1 - c0], hb[i][:, kc, sub * 128:(sub + 1) * 128],
                                                          wb[:, kc, NFM * 128 + c0:NFM * 128 + c1],
                                                          start=(kc == 0), stop=(kc == 7)),
                                 reads=[("wb", kc), ("hb", i)], writes=[("bank", bi)])
                        P.op("dve" if g == 0 else "act",
                             (lambda e: e.tensor_copy(evt[ei][:, c0:c1], banks[bi][:, 0:c1 - c0])) if g == 0 else
                             (lambda e: e.activation(out=evt[ei][:, c0:c1], in_=banks[bi][:, 0:c1 - c0], func=AF.Copy)),
                             reads=[("bank", bi)], writes=[("evt", ei)])
                    r0 = tt * TT + sub * 128
                    P.dma("pool", tm[r0:r0 + 128, :], evt[ei][:], reads=[("evt", ei)], writes=[("tm", tt)])

            s1_norm(0)
            for tt in range(NT):
                s1_proj(tt)
                if tt + 1 < NT:
                    s1_norm(tt + 1)
                s1_proj_tm(tt)
            P.barrier()

        if stop_after <= 1:
            P.finish()
            return nc
        hh_ = 0
        qpos_own = [qpos[0], qpos[1]]
        fmv = fm
        tmv = tm.rearrange("(n p) c -> p n c", p=128)
        stage2 = ExitStack()
        masks_sb = sb(stage2, "masks_sb", [128, NMASK, TT], BF16)
        P.dma("sp", masks_sb[:], masks.rearrange("m p t -> p m t"), writes=["masks_sb"])
        zrot = [0]
        arot = [0]
        a_sb = [sb(stage2, "a_sb%d" % i, [128, TT], BF16) for i in range(3)]
        o_sb = sb(stage2, "o_sb", [64, TT], F32)
        dn_sb = sb(stage2, "dn_sb", [64, TT], F32)
        ACC = 3

        def softmax_tile(q_ap, qkey, ka, kakey, vo, vokey, chunks, selmask=None, sink_ap=None):
            n = len(chunks)
            slots = []

            def qk(idx):
                kc, mk = chunks[idx]
                zi = zrot[0] % 3
                zrot[0] += 1
                ai = arot[0] % 3
                arot[0] += 1
                slots.append((zi, ai))
                mms = [(ka[0:68, kc * 128:(kc + 1) * 128], q_ap, [kakey, qkey])]
                if selmask is not None:
                    mms.append((ewide_sb[:, kc * 128:(kc + 1) * 128], selmask[:], ["ewide_sb", "mneg"]))
                if mk is not None:
                    mms.append((ident_b[:], masks_sb[:, mk, :], ["ident_b", "masks_sb"]))
                for j, (l_, r_, rd) in enumerate(mms):
                    P.op("pe", lambda e: e.matmul(banks[zi][:], l_, r_, start=(j == 0), stop=(j == len(mms) - 1)),
                         reads=rd, writes=[("bank", zi)])

            def ex(idx):
                zi, ai = slots[idx]
                P.op("act", lambda e: e.activation(out=a_sb[ai][:], in_=banks[zi][:], func=AF.Exp),
                     reads=[("bank", zi)], writes=[("a_sb", ai)])

            def av(idx):
                kc, mk = chunks[idx]
                zi, ai = slots[idx]
                P.op("pe", lambda e: e.matmul(banks[ACC][:], vo[:, kc, :], a_sb[ai][:], start=(idx == 0), stop=(idx == n - 1)),
                     reads=[vokey, ("a_sb", ai)], writes=[("bank", ACC)])

            for step in range(n + 2):
                if step < n:
                    qk(step)
                if 0 <= step - 1 < n:
                    ex(step - 1)
                if 0 <= step - 2 < n:
                    av(step - 2)
            if sink_ap is not None:
                P.op("dve", lambda e: e.tensor_scalar(out=dn_sb[:], in0=banks[ACC][64:128, :], scalar1=sink_ap, scalar2=1e-30,
                                                      op0=ALU.add, op1=ALU.max),
                     reads=[("bank", ACC), "esink"], writes=["dn_sb"])
            else:
                P.op("dve", lambda e: e.tensor_scalar_max(out=dn_sb[:], in0=banks[ACC][64:128, :], scalar1=1e-30),
                     reads=[("bank", ACC)], writes=["dn_sb"])
            P.op("dve", lambda e: e.reciprocal(out=dn_sb[:], in_=dn_sb[:]), reads=["dn_sb"], writes=["dn_sb"])
            P.op("dve", lambda e: e.tensor_tensor(out=o_sb[:], in0=banks[ACC][0:64, :], in1=dn_sb[:], op=ALU.mult),
                 reads=[("bank", ACC), "dn_sb"], writes=["o_sb"])

        def build_vo(c, name, col):
            vo = sb(c, name, [128, NB, 128], BF16)
            P.dma("sp", vo[:, :, 0:64], tmv[:, :, col:col + 64], writes=[name])
            P.op("pool", lambda e: e.memset(vo[:, :, 64:128], 1.0), writes=[name])
            return vo

        def build_ka(c, name, ct, row0, kp):
            ka = sb(c, name, [68, S], BF16)
            P.dma("sp", ka[0:64, :], fmv[ct * 128 + row0:ct * 128 + row0 + 64, :], writes=[name])
            P.dma("sp", ka[64:68, :], kp, writes=[name])
            return ka

        if "B" in mixers or "A" in mixers:
          with ExitStack() as c:
            qa = [sb(c, "qa%d" % h, [68, TT], BF16) for h in range(4)]
            gl = [sb(c, "gl%d" % i, [64, TT], F32) for i in range(6)]
            zt_sb = sb(c, "zt_sb", [128, TT], BF16)
            zs_sb = sb(c, "zs_sb", [128, TT], F32)
            ya = sb(c, "ya", [128, TT], F32)
            yo = sb(c, "yo", [128, TT], BF16)
            tmp64 = sb(c, "tmp64", [64, TT], F32)
            if "B" in mixers:
                cB = ExitStack()
                kb = build_ka(cB, "kb", FM_BK, 0, kpos_tok[:, :])
                vb = build_vo(cB, "vb", TM_BV)
                esink = sb(cB, "esink", [128, 1], F32)
                P.dma("sp", esink[:], sinkb[:, :], writes=["esink"])
                P.op("act", lambda e: e.activation(out=esink[:], in_=esink[:], func=AF.Exp), reads=["esink"], writes=["esink"])
                es2 = sb(cB, "es2", [64, 2], F32)
                P.op("dve", lambda e: e.tensor_copy(es2[:, 0:1], esink[0:64, :]), reads=["esink"], writes=["esink2"])
                P.op("dve", lambda e: e.tensor_copy(es2[:, 1:2], esink[64:128, :]), reads=["esink"], writes=["esink2"])
                for tt in range(NT):
                    ts_ = slice(tt * TT, (tt + 1) * TT)
                    P.dma("sp", zt_sb[:], fmv[FM_BZ * 128:(FM_BZ + 1) * 128, ts_], writes=["zt_sb"])
                    P.op("act", lambda e: e.activation(out=zs_sb[:], in_=zt_sb[:], func=AF.Silu), reads=["zt_sb"], writes=["zs_sb"])
                    for hl in range(2):
                        h = 2 * hh_ + hl
                        P.dma("sp", qa[hl][0:64, :], fmv[FM_BQ * 128 + hl * 64:FM_BQ * 128 + hl * 64 + 64, ts_], writes=[("qa", hl)])
                        P.dma("sp", qa[hl][64:68, :], qpos_own[hl][:, ts_], writes=[("qa", hl)])
                        chunks = [(kc, MK_WINB + (kc - 4 * tt + 1)) for kc in range(max(0, 4 * tt - 1), 4 * tt + 4)]
                        softmax_tile(qa[hl][:], ("qa", hl), kb, "kb", vb, "vb", chunks, sink_ap=es2[:, hl:hl + 1])
                        P.op("dve", lambda e: e.tensor_copy(ya[hl * 64:hl * 64 + 64, :], o_sb[:]),
                             reads=["o_sb"], writes=["ya"])
                    P.op("dve", lambda e: e.tensor_tensor(out=yo[:], in0=ya[:], in1=zs_sb[:], op=ALU.mult),
                         reads=["ya", "zs_sb"], writes=["yo"])
                    P.dma("pool", yzd[128:256, ts_], yo[:], reads=["yo"], writes=[("yzd", 1, tt)])
                P.barrier()
                cB.close()
            if "A" in mixers:
             with ExitStack() as c2:
              ksel = build_ka(c2, "ksel", FM_KSW, 0, kpos_tok[:, :])
              kwin = build_ka(c2, "kwin", FM_KSW, 64, kpos_tok[:, :])
              vsel = build_vo(c2, "vsel", TM_VSEL)
              vwin = build_vo(c2, "vwin", TM_VWIN)
              ewide_sb = sb(c2, "ewide_sb", [128, S], BF16)
              P.dma("sp", ewide_sb[:], ewide[:, :], writes=["ewide_sb"])
              keep_sb = sb(c2, "keep_sb", [128, 256], F32)
              add_sb = sb(c2, "add_sb", [128, 256], F32)
              P.dma("sp", keep_sb[:], selkeep[:, :], writes=["keep_sb"])
              P.dma("sp", add_sb[:], seladd[:, :], writes=["add_sb"])
              ps_sb = sb(c2, "ps_sb", [128, 2, 128], BF16)
              P.dma("sp", ps_sb[:], pairsum.rearrange("c p b -> p c b"), writes=["ps_sb"])
              kca = sb(c2, "kca", [68, 256], BF16)
              vcs = sb(c2, "vcs", [128, 2, 64], BF16)
              P.dma("sp", kca[64:68, :], kpos_cmp[:, :], writes=["kca"])
              with ExitStack() as c3:
                  kv = sb(c3, "kv", [128, S], BF16)
                  P.dma("sp", kv[:], fmv[FM_KVC * 128:(FM_KVC + 1) * 128, :], writes=["kv"])
                  posf = sb(c3, "posf", [128, 32], F32)
                  P.dma("sp", posf[0:64, :], cmp_posT[:, 0, :], writes=["posf"])
                  P.dma("sp", posf[64:128, :], cmp_posT[:, 1, :], writes=["posf"])
                  kvp = sb(c3, "kvp", [128, 256, 32], BF16)
                  P.op("dve", lambda e: e.tensor_tensor(out=kvp[:], in0=kv[:].rearrange("p (c j) -> p c j", j=32),
                                                        in1=_bc(posf[:], [128, 256, 32], 1), op=ALU.add),
                       reads=["kv", "posf"], writes=["kvp"])
                  w1f = sb(c3, "w1f", [128, 32, 64], F32)
                  w1b = sb(c3, "w1b", [128, 32, 64], BF16)
                  P.dma("sp", w1f[0:64], cmp_w1[:, 0], writes=["w1f"])
                  P.dma("sp", w1f[64:128], cmp_w1[:, 1], writes=["w1f"])
                  P.op("dve", lambda e: e.tensor_copy(w1b[:], w1f[:]), reads=["w1f"], writes=["w1b"])
                  w2f = sb(c3, "w2f", [128, 64], F32)
                  w2b = sb(c3, "w2b", [128, 64], BF16)
                  P.dma("sp", w2f[0:64], cmp_w2[:, 0], writes=["w2f"])
                  P.dma("sp", w2f[64:128], cmp_w2[:, 1], writes=["w2f"])
                  P.op("dve", lambda e: e.tensor_copy(w2b[:], w2f[:]), reads=["w2f"], writes=["w2b"])
                  hk = sb(c3, "hk", [128, 256], BF16)
                  for jj, p0 in enumerate((0, 64)):
                      for j in range(32):
                          P.op("pe", lambda e: e.matmul(banks[jj][p0:p0 + 64, 0:256], w1b[p0:p0 + 64, j, :], kvp[p0:p0 + 64, :, j],
                                                        start=(j == 0), stop=(j == 31)),
                               reads=["w1b", "kvp"], writes=[("bank", jj)])
                      P.op("act", lambda e: e.activation(out=hk[p0:p0 + 64, :], in_=banks[jj][p0:p0 + 64, 0:256], func=AF.Silu),
                           reads=[("bank", jj)], writes=["hk"])
                  P.op("pe", lambda e: e.matmul(banks[2][0:64, 0:256], w2b[0:64, :], hk[0:64, :], start=True, stop=True),
                       reads=["w2b", "hk"], writes=[("bank", 2)])
                  P.op("dve", lambda e: e.tensor_copy(kca[0:64, :], banks[2][0:64, 0:256]), reads=[("bank", 2)], writes=["kca"])
                  for cc in range(2):
                      P.op("pe", lambda e: e.matmul(banks[4 + cc][:, 0:64], hk[64:128, cc * 128:(cc + 1) * 128], w2b[64:128, :],
                                                    start=True, stop=True),
                           reads=["w2b", "hk"], writes=[("bank", 4 + cc)])
                      P.op("dve", lambda e: e.tensor_copy(vcs[:, cc, :], banks[4 + cc][:, 0:64]), reads=[("bank", 4 + cc)], writes=["vcs"])
                  P.barrier()
              e_sb = [sb(c2, "e_sb%d" % i, [128, TT], BF16) for i in range(8)]
              rec = sb(c2, "rec", [128, TT], F32)
              adj = sb(c2, "adj", [128, 128], F32)
              adj2 = sb(c2, "adj2", [128, 128], F32)
              m8a = sb(c2, "m8a", [128, 8], F32)
              m8b = sb(c2, "m8b", [128, 8], F32)
              mt = sb(c2, "mt", [128, 128], BF16)
              mneg = sb(c2, "mneg", [128, TT], BF16)
              ocmp = sb(c2, "ocmp", [64, 2, TT], F32)
              bank4b = banks[4][:].bitcast(BF16)
              for tt in range(NT):
                  ts_ = slice(tt * TT, (tt + 1) * TT)
                  for h in range(4):
                      ct = FM_AQ01 if h < 2 else FM_AQ23
                      r0 = ct * 128 + (h % 2) * 64
                      P.dma("sp", qa[h][0:64, :], fmv[r0:r0 + 64, ts_], writes=[("qa", h)])
                      P.dma("sp", qa[h][64:68, :], qpos[h, :, ts_], writes=[("qa", h)])
                  for j in range(3):
                      for hl in range(2):
                          r0 = (FM_G0 + j) * 128 + hl * 64
                          gi = j * 2 + hl
                          P.dma("sp", zt_sb[0:64, :], fmv[r0:r0 + 64, ts_], writes=["zt_sb"])
                          P.op("act", lambda e: e.activation(out=gl[gi][:], in_=zt_sb[0:64, :], func=AF.Sigmoid),
                               reads=["zt_sb"], writes=[("gl", gi)])
                  ccs = [0] if tt < 8 else [0, 1]
                  for h in range(4):
                      for cc in ccs:
                          zi = zrot[0] % 3
                          zrot[0] += 1
                          m = tt - 8 * cc
                          P.op("pe", lambda e: e.matmul(banks[zi][:], kca[0:68, cc * 128:(cc + 1) * 128], qa[h][:],
                                                        start=True, stop=(m >= 8)),
                               reads=["kca", ("qa", h)], writes=[("bank", zi)])
                          if m < 8:
                              P.op("pe", lambda e: e.matmul(banks[zi][:], ident_b[:], masks_sb[:, MK_CMP + m, :], start=False, stop=True),
                                   reads=["ident_b", "masks_sb"], writes=[("bank", zi)])
                          P.op("act", lambda e: e.activation(out=e_sb[h * 2 + cc][:], in_=banks[zi][:], func=AF.Exp),
                               reads=[("bank", zi)], writes=[("e_sb", h * 2 + cc)])
                      for cc in ccs:
                          P.op("pe", lambda e: e.matmul(banks[ACC][:], ones_b[:], e_sb[h * 2 + cc][:], start=(cc == 0), stop=(cc == ccs[-1])),
                               reads=["ones_b", ("e_sb", h * 2 + cc)], writes=[("bank", ACC)])
                      P.op("dve", lambda e: e.tensor_scalar_max(out=rec[:], in0=banks[ACC][:], scalar1=1e-30),
                           reads=[("bank", ACC)], writes=["rec"])
                      P.op("dve", lambda e: e.reciprocal(out=rec[:], in_=rec[:]), reads=["rec"], writes=["rec"])
                      for cc in ccs:
                          P.op("dve" if cc == 0 else "pool",
                               lambda e: e.tensor_tensor(out=e_sb[h * 2 + cc][:], in0=e_sb[h * 2 + cc][:], in1=rec[:], op=ALU.mult),
                               reads=[("e_sb", h * 2 + cc), "rec"], writes=[("e_sb", h * 2 + cc)])
                      if h // 2 == hh_:
                          hl = h % 2
                          for cc in ccs:
                              P.op("pe", lambda e: e.matmul(banks[5][0:64, :], vcs[:, cc, :], e_sb[h * 2 + cc][:],
                                                            start=(cc == 0), stop=(cc == ccs[-1])),
                                   reads=["vcs", ("e_sb", h * 2 + cc)], writes=[("bank", 5)])
                          P.op("dve", lambda e: e.tensor_tensor(out=ya[hl * 64:hl * 64 + 64, :], in0=banks[5][0:64, :], in1=gl[0 * 2 + hl][:],
                                                                op=ALU.mult),
                               reads=[("bank", 5), ("gl", hl)], writes=["ya"])
                  for sub in range(4):
                      i_blk = tt * 4 + sub
                      first_mm = True
                      nmm = 4 * len(ccs)
                      k_ = 0
                      for h in range(4):
                          for cc in ccs:
                              k_ += 1
                              P.op("pe", lambda e: e.matmul(banks[6][:, 0:128], e_sb[h * 2 + cc][:, sub * 128:(sub + 1) * 128], ps_sb[:, cc, :],
                                                            start=first_mm, stop=(k_ == nmm)),
                                   reads=[("e_sb", h * 2 + cc), "ps_sb"], writes=[("bank", 6)])
                              first_mm = False
                      o0 = 128 - 2 * i_blk
                      P.op("dve", lambda e: e.tensor_tensor(out=adj[:], in0=banks[6][:, 0:128], in1=keep_sb[:, o0:o0 + 128], op=ALU.mult),
                           reads=[("bank", 6), "keep_sb"], writes=["adj"])
                      P.op("dve", lambda e: e.tensor_tensor(out=adj[:], in0=adj[:], in1=add_sb[:, o0:o0 + 128], op=ALU.add),
                           reads=["adj", "add_sb"], writes=["adj"])
                      P.op("dve", lambda e: e.memset(adj[:, 0:1], 1e4), reads=["adj"], writes=["adj"])
                      P.op("dve", lambda e: e.max(out=m8a[:], in_=adj[:]), reads=["adj"], writes=["m8a"])
                      P.op("dve", lambda e: e.match_replace(out=adj2[:], in_to_replace=m8a[:], in_values=adj[:], imm_value=-1e9),
                           reads=["adj", "m8a"], writes=["adj2"])
                      P.op("dve", lambda e: e.max(out=m8b[:], in_=adj2[:]), reads=["adj2"], writes=["m8b"])
                      P.op("dve", lambda e: e.tensor_scalar(out=mt[:], in0=adj[:], scalar1=m8b[:, 7:8], scalar2=-1.0,
                                                            op0=ALU.is_ge, op1=ALU.add),
                           reads=["adj", "m8b"], writes=["mt"])
                      P.op("pe", lambda e: e.transpose(bank4b[:, 0:128], mt[:], ident_b[:]),
                           reads=["mt", "ident_b"], writes=[("bank", 4)])
                      P.op("act", lambda e: e.activation(out=mneg[:, sub * 128:(sub + 1) * 128], in_=bank4b[:, 0:128], func=AF.Copy),
                           reads=[("bank", 4)], writes=["mneg"])
                  for hl in range(2):
                      h = 2 * hh_ + hl
                      chunks = [(kc, (MK_NONSTRICT + kc - 4 * tt) if kc >= 4 * tt else None) for kc in range(0, 4 * tt + 4)]
                      softmax_tile(qa[h][:], ("qa", h), ksel, "ksel", vsel, "vsel", chunks, selmask=mneg)
                      P.op("dve", lambda e: e.tensor_tensor(out=tmp64[:], in0=o_sb[:], in1=gl[1 * 2 + hl][:], op=ALU.mult),
                           reads=["o_sb", ("gl", 2 + hl)], writes=["tmp64"])
                      P.op("dve", lambda e: e.tensor_copy(ocmp[:, hl, :], tmp64[:]), reads=["tmp64"], writes=["ocmp"])
                      chunks = []
                      for kc in range(max(0, 4 * tt - 4), 4 * tt + 4):
                          r = kc - 4 * tt
                          chunks.append((kc, MK_WINA + r + 4 if r < 0 else MK_NONSTRICT + r))
                      softmax_tile(qa[h][:], ("qa", h), kwin, "kwin", vwin, "vwin", chunks)
                      P.op("dve", lambda e: e.tensor_tensor(out=tmp64[:], in0=o_sb[:], in1=gl[2 * 2 + hl][:], op=ALU.mult),
                           reads=["o_sb", ("gl", 4 + hl)], writes=["tmp64"])
                      P.op("dve", lambda e: e.tensor_tensor(out=tmp64[:], in0=tmp64[:], in1=ocmp[:, hl, :], op=ALU.add),
                           reads=["tmp64", "ocmp"], writes=["tmp64"])
                      P.op("dve", lambda e: e.tensor_copy(ocmp[:, hl, :], ya[hl * 64:hl * 64 + 64, :]), reads=["ya"], writes=["ocmp"])
                      P.op("dve", lambda e: e.tensor_tensor(out=ya[hl * 64:hl * 64 + 64, :], in0=tmp64[:], in1=ocmp[:, hl, :], op=ALU.add),
                           reads=["tmp64", "ocmp"], writes=["ya"])
                  P.dma("sp", zt_sb[:], fmv[FM_AZ * 128:(FM_AZ + 1) * 128, ts_], writes=["zt_sb"])
                  P.op("act", lambda e: e.activation(out=zs_sb[:], in_=zt_sb[:], func=AF.Silu), reads=["zt_sb"], writes=["zs_sb"])
                  P.op("dve", lambda e: e.tensor_tensor(out=yo[:], in0=ya[:], in1=zs_sb[:], op=ALU.mult),
                       reads=["ya", "zs_sb"], writes=["yo"])
                  P.dma("pool", yzd[0:128, ts_], yo[:], reads=["yo"], writes=[("yzd", 0, tt)])
              P.barrier()
        if "C" in mixers:
          with ExitStack() as c:
            ntri_sb = sb(c, "ntri_sb", [128, 128], BF16)
            P.dma("sp", ntri_sb[:], ntri[:, :], writes=["ntri_sb"])
            qs = sb(c, "qs", [64, S], BF16)
            ks = sb(c, "ks", [64, S], BF16)
            vs = sb(c, "vs", [128, NB, 64], BF16)
            ee = [sb(c, "ee%d" % i, [128, TT], F32) for i in range(2)]
            sp_ = [sb(c, "sp%d" % i, [128, TT], F32) for i in range(2)]
            hi = [sb(c, "hi%d" % i, [128, TT], BF16) for i in range(3)]
            lo = [sb(c, "lo%d" % i, [128, TT], BF16) for i in range(3)]
            ww = [sb(c, "ww%d" % i, [64, TT], F32) for i in range(3)]
            aa = [sb(c, "aa%d" % i, [128, TT], BF16) for i in range(2)]
            tmpc = sb(c, "tmpc", [64, TT], F32)
            oacc = sb(c, "oacc", [64, TT], F32)
            zt_c = sb(c, "zt_c", [64, TT], BF16)
            zs_c = sb(c, "zs_c", [64, TT], F32)
            yo_c = sb(c, "yo_c", [64, TT], BF16)
            CARRY = 4
            for hl in range(2):
                P.dma("sp", qs[:], fmv[FM_CQ * 128 + hl * 64:FM_CQ * 128 + hl * 64 + 64, :], writes=["qs"])
                P.dma("sp", ks[:], fmv[FM_CK * 128 + hl * 64:FM_CK * 128 + hl * 64 + 64, :], writes=["ks"])
                P.dma("sp", vs[:], tmv[:, :, TM_CV0 + 64 * hl:TM_CV0 + 64 * hl + 64], writes=["vs"])
                recs = []
                for tt in range(NT):
                    kcs = list(range(4 * tt + 3, -1, -1))
                    for j, kc in enumerate(kcs):
                        recs.append(dict(tt=tt, kc=kc, first=(j == 0), last=(j == len(kcs) - 1), i=len(recs)))

                def s1(r):
                    i = r["i"]; zi = i % 4; tt = r["tt"]; kc = r["kc"]
                    diag = kc >= 4 * tt
                    P.op("pe", lambda e: e.matmul(banks[zi][:], ks[:, kc * 128:(kc + 1) * 128], qs[:, tt * TT:(tt + 1) * TT],
                                                  start=True, stop=not diag),
                         reads=["ks", "qs"], writes=[("bank", zi)])
                    if diag:
                        P.op("pe", lambda e: e.matmul(banks[zi][:], ident_b[:], masks_sb[:, MK_STRICT + kc - 4 * tt, :], start=False, stop=True),
                             reads=["ident_b", "masks_sb"], writes=[("bank", zi)])

                def s2(r):
                    i = r["i"]; zi = i % 4
                    P.op("act", lambda e: e.activation(out=ee[i % 2][:], in_=banks[zi][:], func=AF.Exp),
                         reads=[("bank", zi)], writes=[("ee", i % 2)])
                    P.op("act", lambda e: e.activation(out=sp_[i % 2][:], in_=ee[i % 2][:], func=AF.Ln, bias=1.0),
                         reads=[("ee", i % 2)], writes=[("sp", i % 2)])
                    P.op("dve", lambda e: e.tensor_copy(hi[i % 3][:], sp_[i % 2][:]), reads=[("sp", i % 2)], writes=[("hi", i % 3)])
                    P.op("dve", lambda e: e.tensor_tensor(out=lo[i % 3][:], in0=sp_[i % 2][:], in1=hi[i % 3][:], op=ALU.subtract),
                         reads=[("sp", i % 2), ("hi", i % 3)], writes=[("lo", i % 3)])

                def s3(r):
                    i = r["i"]; zi = i % 4
                    if not r["first"]:
                        P.op("act", lambda e: e.activation(out=ww[i % 3][:], in_=banks[CARRY][0:64, :], func=AF.Exp, scale=-1.0),
                             reads=[("bank", CARRY)], writes=[("ww", i % 3)])
                    P.op("pe", lambda e: e.matmul(banks[zi][:], ntri_sb[:], hi[i % 3][:], start=False, stop=False),
                         reads=["ntri_sb", ("hi", i % 3)], writes=[("bank", zi)])
                    P.op("pe", lambda e: e.matmul(banks[zi][:], ntri_sb[:], lo[i % 3][:], start=False, stop=True),
                         reads=["ntri_sb", ("lo", i % 3)], writes=[("bank", zi)])
                    if not r["last"]:
                        P.op("pe", lambda e: e.matmul(banks[CARRY][0:64, :], ones_b[:, 0:64], hi[i % 3][:], start=r["first"], stop=False),
                             reads=["ones_b", ("hi", i % 3)], writes=[("bank", CARRY)])
                        P.op("pe", lambda e: e.matmul(banks[CARRY][0:64, :], ones_b[:, 0:64], lo[i % 3][:], start=False, stop=True),
                             reads=["ones_b", ("lo", i % 3)], writes=[("bank", CARRY)])

                def s4(r):
                    i = r["i"]; zi = i % 4; tt = r["tt"]; kc = r["kc"]
                    pb = 5 + i % 2
                    P.op("act", lambda e: e.activation(out=aa[i % 2][:], in_=banks[zi][:], func=AF.Exp),
                         reads=[("bank", zi)], writes=[("aa", i % 2)])
                    P.op("pe", lambda e: e.matmul(banks[pb][0:64, :], vs[:, kc, :], aa[i % 2][:], start=True, stop=True),
                         reads=["vs", ("aa", i % 2)], writes=[("bank", pb)])
                    if r["first"]:
                        P.op("dve", lambda e: e.tensor_copy(oacc[:], banks[pb][0:64, :]), reads=[("bank", pb)], writes=["oacc"])
                    else:
                        P.op("dve", lambda e: e.tensor_tensor(out=tmpc[:], in0=banks[pb][0:64, :], in1=ww[i % 3][:], op=ALU.mult),
                             reads=[("bank", pb), ("ww", i % 3)], writes=["tmpc"])
                        P.op("pool", lambda e: e.tensor_tensor(out=oacc[:], in0=oacc[:], in1=tmpc[:], op=ALU.add),
                             reads=["oacc", "tmpc"], writes=["oacc"])
                    if r["last"]:
                        ts_ = slice(tt * TT, (tt + 1) * TT)
                        r0 = FM_CZ * 128 + hl * 64
                        P.dma("sp", zt_c[:], fmv[r0:r0 + 64, ts_], writes=["zt_c"])
                        P.op("act", lambda e: e.activation(out=zs_c[:], in_=zt_c[:], func=AF.Silu), reads=["zt_c"], writes=["zs_c"])
                        P.op("dve", lambda e: e.tensor_tensor(out=yo_c[:], in0=oacc[:], in1=zs_c[:], op=ALU.mult),
                             reads=["oacc", "zs_c"], writes=["yo_c"])
                        P.dma("pool", yzd[256 + hl * 64:256 + hl * 64 + 64, ts_], yo_c[:], reads=["yo_c"], writes=[("yzd", 2, hl, tt)])

                n = len(recs)
                for step in range(n + 3):
                    for lag, fn in ((0, s1), (1, s2), (2, s3), (3, s4)):
                        j = step - lag
                        if 0 <= j < n:
                            fn(recs[j])
            P.barrier()

        if "D" in mixers:
          with ExitStack() as c:
            qd = sb(c, "qd", [64, S], BF16)
            kd = sb(c, "kd", [64, S], BF16)
            vd = sb(c, "vd", [128, NB, 64], BF16)
            kt = sb(c, "kt", [128, NB, 64], BF16)
            vz = sb(c, "vz", [128, NB, 64], BF16)
            dt_sb = sb(c, "dt_sb", [128, 2, 128], F32)
            ze_sb = sb(c, "ze_sb", [128, 2], F32)
            xi_sb = sb(c, "xi_sb", [64, 2, 128], F32)
            gc_sb = sb(c, "gc_sb", [64, 2], F32)
            P.dma("sp", dt_sb[:], dtab[:, :, :], writes=["dt_sb"])
            P.dma("sp", ze_sb[:], zeta[:, :], writes=["ze_sb"])
            P.dma("sp", xi_sb[:], xi_bc[:, :, :], writes=["xi_sb"])
            P.dma("sp", gc_sb[:], gchunk[:, :], writes=["gc_sb"])
            uall = sb(c, "uall", [64, NB, 64], F32)
            rall = sb(c, "rall", [64, NB, 64], F32)
            rbf = sb(c, "rbf", [64, NB, 64], BF16)
            sm = [sb(c, "sm%d" % i, [128, TT], BF16) for i in range(2)]
            od = sb(c, "od", [64, TT], F32)
            od2 = sb(c, "od2", [64, TT], F32)
            mean = sb(c, "mean", [64, TT], F32)
            var = sb(c, "var", [64, TT], F32)
            zt_d = sb(c, "zt_d", [64, TT], BF16)
            zs_d = sb(c, "zs_d", [64, TT], F32)
            yo_d = sb(c, "yo_d", [64, TT], BF16)
            for hl in range(2):
                P.dma("sp", qd[:], fmv[FM_DQ * 128 + hl * 64:FM_DQ * 128 + hl * 64 + 64, :], writes=["qd"])
                P.dma("sp", kd[:], fmv[FM_DK * 128 + hl * 64:FM_DK * 128 + hl * 64 + 64, :], writes=["kd"])
                P.dma("sp", vd[:], tmv[:, :, TM_DV0 + 64 * hl:TM_DV0 + 64 * hl + 64], writes=["vd"])
                P.dma("sp", kt[:], tmv[:, :, TM_DK0 + 64 * hl:TM_DK0 + 64 * hl + 64], writes=["kt"])
                P.op("dve", lambda e: e.tensor_scalar(out=vz[:], in0=vd[:], scalar1=ze_sb[:, hl:hl + 1], scalar2=None, op0=ALU.mult),
                     reads=["vd", "ze_sb"], writes=["vz"])
                for g in range(NB // 8):
                    bi = g % 2
                    for j in range(8):
                        n_ = g * 8 + j
                        P.op("pe", lambda e: e.matmul(banks[bi][0:64, j * 64:(j + 1) * 64], kt[:, n_, :], vz[:, n_, :],
                                                      start=(j == 0), stop=(j == 7)),
                             reads=["kt", "vz"], writes=[("bank", bi)])
                    P.op("act", lambda e: e.activation(out=uall[:, g * 8:(g + 1) * 8, :],
                                                       in_=banks[bi][0:64, :].rearrange("p (n e) -> p n e", e=64), func=AF.Copy),
                         reads=[("bank", bi)], writes=["uall"])
                P.op("dve", lambda e: e.memset(rall[:, 0, :], 0.0), writes=["rall"])
                for n_ in range(1, NB):
                    P.op("dve", lambda e: e.scalar_tensor_tensor(out=rall[:, n_, :], in0=rall[:, n_ - 1, :], scalar=gc_sb[:, hl:hl + 1],
                                                                 in1=uall[:, n_ - 1, :], op0=ALU.mult, op1=ALU.add),
                         reads=["rall", "uall", "gc_sb"], writes=["rall"])
                P.op("dve", lambda e: e.tensor_copy(rbf[:], rall[:]), reads=["rall"], writes=["rbf"])
                for tt in range(NT):
                    ts_ = slice(tt * TT, (tt + 1) * TT)
                    si = tt % 2
                    zi = tt % 2
                    for sub in range(4):
                        n_ = tt * 4 + sub
                        cs_ = slice(n_ * 128, (n_ + 1) * 128)
                        P.op("pe", lambda e: e.matmul(banks[zi][:, sub * 128:(sub + 1) * 128], kd[:, cs_], qd[:, cs_],
                                                      start=(sub == 0), stop=(sub == 3)),
                             reads=["kd", "qd"], writes=[("bank", zi)])
                    P.op("dve", lambda e: e.tensor_tensor(out=sm[si][:].rearrange("p (n i) -> p n i", i=128),
                                                          in0=banks[zi][:].rearrange("p (n i) -> p n i", i=128),
                                                          in1=_bc(dt_sb[:, hl, :], [128, 4, 128], 1), op=ALU.mult),
                         reads=[("bank", zi), "dt_sb"], writes=[("sm", si)])
                    for sub in range(4):
                        n_ = tt * 4 + sub
                        cs_ = slice(n_ * 128, (n_ + 1) * 128)
                        P.op("pe", lambda e: e.matmul(banks[2 + zi][0:64, sub * 128:(sub + 1) * 128], vd[:, n_, :], sm[si][:, sub * 128:(sub + 1) * 128],
                                                      start=(sub == 0), stop=(sub == 3)),
                             reads=["vd", ("sm", si)], writes=[("bank", 2 + zi)])
                    for sub in range(4):
                        n_ = tt * 4 + sub
                        cs_ = slice(n_ * 128, (n_ + 1) * 128)
                        P.op("pe", lambda e: e.matmul(banks[4 + zi][0:64, sub * 128:(sub + 1) * 128], rbf[:, n_, :], qd[:, cs_],
                                                      start=(sub == 0), stop=(sub == 3)),
                             reads=["rbf", "qd"], writes=[("bank", 4 + zi)])
                    P.op("dve", lambda e: e.tensor_tensor(out=od[:].rearrange("p (n i) -> p n i", i=128),
                                                          in0=banks[4 + zi][0:64, :].rearrange("p (n i) -> p n i", i=128),
                                                          in1=_bc(xi_sb[:, hl, :], [64, 4, 128], 1), op=ALU.mult),
                         reads=[("bank", 4 + zi), "xi_sb"], writes=["od"])
                    P.op("dve", lambda e: e.tensor_tensor(out=od[:], in0=od[:], in1=banks[2 + zi][0:64, :], op=ALU.add),
                         reads=["od", ("bank", 2 + zi)], writes=["od"])
                    P.op("act", lambda e: e.activation(out=od2[:], in_=od[:], func=AF.Square), reads=["od"], writes=["od2"])
                    P.op("pe", lambda e: e.matmul(banks[6][0:64, :], ones_f[0:64, 0:64], od[:], start=True, stop=True),
                         reads=["ones_f", "od"], writes=[("bank", 6)])
                    P.op("pe", lambda e: e.matmul(banks[7][0:64, :], ones_f[0:64, 0:64], od2[:], start=True, stop=True),
                         reads=["ones_f", "od2"], writes=[("bank", 7)])
                    P.op("act", lambda e: e.activation(out=mean[:], in_=banks[6][0:64, :], func=AF.Copy, scale=1.0 / 64),
                         reads=[("bank", 6)], writes=["mean"])
                    P.op("dve", lambda e: e.tensor_tensor(out=var[:], in0=mean[:], in1=mean[:], op=ALU.mult),
                         reads=["mean"], writes=["var"])
                    P.op("dve", lambda e: e.scalar_tensor_tensor(out=var[:], in0=banks[7][0:64, :], scalar=1.0 / 64, in1=var[:],
                                                                 op0=ALU.mult, op1=ALU.subtract),
                         reads=[("bank", 7), "var"], writes=["var"])
                    P.op("act", lambda e: e.activation(out=var[:], in_=var[:], func=AF.Sqrt, bias=LN_EPS), reads=["var"], writes=["var"])
                    P.op("dve", lambda e: e.reciprocal(out=var[:], in_=var[:]), reads=["var"], writes=["var"])
                    P.op("dve", lambda e: e.tensor_tensor(out=od[:], in0=od[:], in1=mean[:], op=ALU.subtract),
                         reads=["od", "mean"], writes=["od"])
                    P.op("dve", lambda e: e.tensor_tensor(out=od[:], in0=od[:], in1=var[:], op=ALU.mult),
                         reads=["od", "var"], writes=["od"])
                    r0 = FM_DZ * 128 + hl * 64
                    P.dma("sp", zt_d[:], fmv[r0:r0 + 64, ts_], writes=["zt_d"])
                    P.op("act", lambda e: e.activation(out=zs_d[:], in_=zt_d[:], func=AF.Silu), reads=["zt_d"], writes=["zs_d"])
                    P.op("dve", lambda e: e.tensor_tensor(out=yo_d[:], in0=od[:], in1=zs_d[:], op=ALU.mult),
                         reads=["od", "zs_d"], writes=["yo_d"])
                    P.dma("pool", yzd[384 + hl * 64:384 + hl * 64 + 64, ts_], yo_d[:], reads=["yo_d"], writes=[("yzd", 3, hl, tt)])
            P.barrier()
        stage2.close()
        if stop_after <= 2:
            P.finish()
            return nc

        with ExitStack() as c:
            wm = sb(c, "wm", [128, 4, 8, D], BF16)
            wo = sb(c, "wo", [128, 8, D], BF16)
            wbr = sb(c, "wbr", [128, 4, D], BF16)
            stg = [sb(c, "stg3_%d" % i, [128, D], F32) for i in range(2)]
            k_ = 0
            for i in range(4):
                for kc in range(8):
                    load_cast(c, stg, wm[:, i, kc, :], w_merge[i, kc * 128:(kc + 1) * 128, :], D, ("wm", i, kc), k_)
                    k_ += 1
                load_cast(c, stg, wbr[:, i, :], w_br[i, :, :], D, ("wbr", i), k_)
                k_ += 1
            for kc in range(8):
                load_cast(c, stg, wo[:, kc, :], w_out[kc * 128:(kc + 1) * 128, :], D, ("wo", kc), k_)
                k_ += 1
            hb3 = [sb(c, "hb3_%d" % i, [128, 8, TT], BF16) for i in range(2)]
            yz3 = [sb(c, "yz3_%d" % i, [128, 4, TT], BF16) for i in range(2)]
            sg = [sb(c, "sg%d" % i, [128, TT], F32) for i in range(2)]
            term = [sb(c, "term%d" % i, [128, TT], F32) for i in range(2)]
            mrg = sb(c, "mrg", [128, 8, TT], F32)
            mg = sb(c, "mg", [128, 8, TT], BF16)
            po = [sb(c, "po%d" % i, [128, TT], F32) for i in range(2)]
            hview = hT.rearrange("(k p) t -> p k t", p=128)
            yview = yzd.rearrange("(m p) t -> p m t", p=128)
            pview = partT.rearrange("(k p) t -> p k t", p=128)
            nb_ = 0
            ns_ = 0
            for tt in range(NT):
                i2 = tt % 2
                ts_ = slice(tt * TT, (tt + 1) * TT)
                P.dma("sp", hb3[i2][:], hview[:, :, ts_], writes=[("hb3", i2)])
                P.dma("sp", yz3[i2][:], yview[:, :, ts_], writes=[("yz3", i2)])
                for oc in range(8):
                    for i in range(4):
                        bg = nb_ % 6
                        nb_ += 1
                        for kc in range(8):
                            P.op("pe", lambda e: e.matmul(banks[bg][:], wm[:, i, kc, oc * 128:(oc + 1) * 128], hb3[i2][:, kc, :],
                                                          start=(kc == 0), stop=(kc == 7)),
                                 reads=[("wm", i, kc), ("hb3", i2)], writes=[("bank", bg)])
                        bb = 6 + ns_ % 2
                        P.op("pe", lambda e: e.matmul(banks[bb][:], wbr[:, i, oc * 128:(oc + 1) * 128], yz3[i2][:, i, :], start=True, stop=True),
                             reads=[("wbr", i), ("yz3", i2)], writes=[("bank", bb)])
                        si = ns_ % 2
                        ns_ += 1
                        P.op("act", lambda e: e.activation(out=sg[si][:], in_=banks[bg][:], func=AF.Sigmoid),
                             reads=[("bank", bg)], writes=[("sg", si)])
                        if i == 0:
                            P.op("dve", lambda e: e.tensor_tensor(out=mrg[:, oc, :], in0=banks[bb][:], in1=sg[si][:], op=ALU.mult),
                                 reads=[("bank", bb), ("sg", si)], writes=[("mrg", oc)])
                        else:
                            P.op("dve", lambda e: e.tensor_tensor(out=term[si][:], in0=banks[bb][:], in1=sg[si][:], op=ALU.mult),
                                 reads=[("bank", bb), ("sg", si)], writes=[("term", si)])
                            P.op("pool", lambda e: e.tensor_tensor(out=mrg[:, oc, :], in0=mrg[:, oc, :], in1=term[si][:], op=ALU.add),
                                 reads=[("mrg", oc), ("term", si)], writes=[("mrg", oc)])
                    P.op("pool", lambda e: e.tensor_copy(mg[:, oc, :], mrg[:, oc, :]), reads=[("mrg", oc)], writes=[("mg", oc)])
                for oc2 in range(8):
                    bg = nb_ % 6
                    nb_ += 1
                    for kc in range(8):
                        P.op("pe", lambda e: e.matmul(banks[bg][:], wo[:, kc, oc2 * 128:(oc2 + 1) * 128], mg[:, kc, :],
                                                      start=(kc == 0), stop=(kc == 7)),
                             reads=[("wo", kc), ("mg", kc)], writes=[("bank", bg)])
                    pi = oc2 % 2
                    P.op("act", lambda e: e.activation(out=po[pi][:], in_=banks[bg][:], func=AF.Copy, scale=mod[:, 16 + oc2:17 + oc2]),
                         reads=[("bank", bg), "mod"], writes=[("po", pi)])
                    P.dma("pool", pview[:, oc2, ts_], po[pi][:], reads=[("po", pi)], is_output=True)
            P.barrier()
        P.finish()
    return nc


def _bf(a):
    return np.asarray(a, dtype=np.float32).astype(ml_dtypes.bfloat16)


def make_consts():
    sl = np.arange(128)[:, None]
    tl = np.arange(TT)[None, :]
    m = np.zeros((NMASK, 128, TT), np.float32)

    def put(idx, ok):
        m[idx] = np.where(ok, 0.0, NEG)
    for r in range(4):
        put(MK_STRICT + r, sl + 128 * r < tl)
        put(MK_NONSTRICT + r, sl + 128 * r <= tl)
    for j, r in enumerate(range(-4, 0)):
        d = tl - sl - 128 * r
        put(MK_WINA + j, (d >= 0) & (d < 512))
    for j, r in enumerate(range(-1, 4)):
        d = tl - sl - 128 * r
        put(MK_WINB + j, (d >= 0) & (d < 128))
    for mm in range(8):
        put(MK_CMP + mm, tl + 512 * mm - 32 * sl - 31 >= 0)
    t = np.arange(S)
    slopes = 2.0 ** (-8.0 * (np.arange(4) + 1) / 4)
    qpos = np.zeros((4, 4, S), np.float32)
    for h in range(4):
        qpos[h, 0] = -slopes[h] * 64 * (t // 64)
        qpos[h, 1] = -slopes[h] * (t % 64)
        qpos[h, 2] = slopes[h] * 64
        qpos[h, 3] = slopes[h]
    kpos_tok = np.stack([np.ones(S), np.ones(S), t // 64, t % 64]).astype(np.float32)
    pc = 32 * np.arange(256) + 31
    kpos_cmp = np.stack([np.ones(256), np.ones(256), pc // 64, pc % 64]).astype(np.float32)
    ewide = 30000.0 * (np.arange(S)[None, :] // 64 == np.arange(128)[:, None]).astype(np.float32)
    ident = np.eye(128, dtype=np.float32)
    ntri = -(np.arange(128)[:, None] >= np.arange(128)[None, :]).astype(np.float32)
    pairsum = np.zeros((2, 128, 128), np.float32)
    for cc in range(2):
        pairsum[cc, np.arange(128), 64 * cc + np.arange(128) // 2] = 1.0
    u = np.arange(256)[None, :] - 128
    cur = (np.arange(128)[:, None] >= 64).astype(np.int64)
    forced = (u == cur) | (u == cur - 1)
    future = u > cur
    selkeep = (~(forced | future)).astype(np.float32)
    seladd = np.where(forced, 1e4, np.where(future, -1.0, 0.0)).astype(np.float32)
    return dict(masks=_bf(m), qpos_all=qpos, kpos_tok=_bf(kpos_tok), kpos_cmp=_bf(kpos_cmp), ewide=_bf(ewide),
                ident=_bf(ident), ntri=_bf(ntri), pairsum=_bf(pairsum), selkeep=selkeep, seladd=seladd)


def ret_consts(hh):
    out = {}
    i = np.arange(128)
    dt_ = np.zeros((128, 2, 128), np.float32)
    zt = np.zeros((128, 2), np.float32)
    xi = np.zeros((64, 2, 128), np.float32)
    gc = np.zeros((64, 2), np.float32)
    for hl in range(2):
        h = 2 * hh + hl
        log_g = np.log(np.float32(1.0) - np.float32(2.0 ** (-5.0 - h)))
        diff = (i[None, :] - i[:, None]).astype(np.float32)
        dt_[:, hl, :] = np.where(diff >= 0, np.exp(log_g * np.maximum(diff, 0.0)), 0.0)
        zt[:, hl] = np.exp(log_g * (127 - i))
        xi[:, hl, :] = np.exp(log_g * (i + 1))[None, :]
        gc[:, hl] = np.exp(log_g * 128)
    return dict(dtab=dt_, zeta=zt, xi_bc=xi, gchunk=gc)


def arrange_w_in(w, hh):
    o = np.cumsum([0, 256, 384, 12, 256, 256, 128, 128, 256, 256, 256, 256, 256, 256, 256, 256, 256])
    a_q, a_kv, a_g, a_z, b_q, b_k, b_v, b_z, c_q, c_k, c_v, c_z, d_q, d_k, d_v, d_z = [
        w[:, o[i]:o[i + 1]] for i in range(16)]
    own = slice(128 * hh, 128 * hh + 128)
    kvh = slice(64 * hh, 64 * hh + 64)
    k_cmp, v_cmp, k_sel, v_sel, k_win, v_win = [a_kv[:, 64 * i:64 * i + 64] for i in range(6)]
    gcols = []
    for j in range(3):
        g = np.concatenate([np.repeat(a_g[:, 3 * (2 * hh + hl) + j][:, None], 64, axis=1) for hl in range(2)], axis=1)
        gcols.append(g)
    zpad = np.zeros((D, 64), np.float32)
    oth = slice(128 * (1 - hh), 128 * (1 - hh) + 128)
    fmt = [a_q[:, own], a_q[:, oth], np.concatenate([k_cmp, v_cmp], 1), np.concatenate([k_sel, k_win], 1),
           gcols[0], gcols[1], gcols[2], a_z[:, own], b_q[:, own], np.concatenate([b_k[:, kvh], zpad], 1), b_z[:, own],
           c_q[:, own], c_k[:, own], c_z[:, own], d_q[:, own], d_k[:, own], d_z[:, own]]
    tmc = [v_sel, v_win, b_v[:, kvh], c_v[:, own], d_v[:, own], d_k[:, own]]
    return np.ascontiguousarray(np.concatenate(fmt + tmc, axis=1), dtype=np.float32)


def layer_inputs(l, b, hh, c, w_ada, b_ada, norm_g, w_in, cmp_pos, cmp_w1, cmp_w2, sink, w_merge, w_br, w_out, consts):
    d = dict(consts)
    qp = d.pop("qpos_all")
    order = [2 * hh, 2 * hh + 1, 2 * (1 - hh), 2 * (1 - hh) + 1]
    d["qpos"] = _bf(qp[order])
    d.update(ret_consts(hh))
    d["cT"] = np.ascontiguousarray(c[b].reshape(8, 128).T)
    d["w_ada"] = np.ascontiguousarray(w_ada[l])
    d["b_adaT"] = np.ascontiguousarray(b_ada[l].reshape(24, 128).T)
    d["gT"] = np.ascontiguousarray(norm_g[l].reshape(8, 128).T)
    d["w_in"] = arrange_w_in(w_in[l], hh)
    d["cmp_posT"] = np.ascontiguousarray(cmp_pos[l].transpose(2, 0, 1))
    d["cmp_w1"] = np.ascontiguousarray(cmp_w1[l].reshape(2, 32, 64, 64).transpose(2, 0, 1, 3))
    d["cmp_w2"] = np.ascontiguousarray(cmp_w2[l].transpose(1, 0, 2))
    d["sinkb"] = np.ascontiguousarray(np.repeat(sink[l][2 * hh:2 * hh + 2], 64)[:, None].astype(np.float32))
    d["w_merge"] = np.ascontiguousarray(w_merge[l])
    d["w_br"] = np.ascontiguousarray(w_br[l][:, 128 * hh:128 * hh + 128, :])
    d["w_out"] = np.ascontiguousarray(w_out[l])
    return d


_PROG_CACHE = {}


def _prog(first, last_only):
    key = (first, last_only)
    if key not in _PROG_CACHE:
        _PROG_CACHE[key] = build_program(first, last_only)
    return _PROG_CACHE[key]


def kernel(x, c, w_ada, b_ada, norm_g, w_in, cmp_pos, cmp_w1, cmp_w2, sink, w_merge, w_br, w_out, final_g):
    args = [np.asarray(a, dtype=np.float32) for a in
            (x, c, w_ada, b_ada, norm_g, w_in, cmp_pos, cmp_w1, cmp_w2, sink, w_merge, w_br, w_out, final_g)]
    x, c, w_ada, b_ada, norm_g, w_in, cmp_pos, cmp_w1, cmp_w2, sink, w_merge, w_br, w_out, final_g = args
    B = x.shape[0]
    depth = w_ada.shape[0]
    consts = make_consts()
    cores = [(b, hh) for b in range(B) for hh in range(2)]
    xT = [np.ascontiguousarray(x[b].T) for b in range(B)]
    zero = np.zeros((D, S), np.float32)
    parts = [zero] * (2 * B)
    for l in range(depth):
        in_maps = []
        for (b, hh) in cores:
            d = layer_inputs(l, b, hh, c, w_ada, b_ada, norm_g, w_in, cmp_pos, cmp_w1, cmp_w2, sink, w_merge, w_br, w_out, consts)
            d["xT"] = xT[b]
            d["paT"] = parts[2 * b]
            d["pbT"] = parts[2 * b + 1]
            in_maps.append(d)
        res = run_bass_kernel_spmd(_prog(False, False), in_maps, core_ids=list(range(len(cores))))
        xT = [np.asarray(res.results[2 * b]["xcT"]) for b in range(B)]
        parts = [np.asarray(r["partT"]) for r in res.results]
    in_maps = []
    fgT = np.ascontiguousarray(final_g.reshape(8, 128).T)
    for (b, hh) in cores:
        in_maps.append({"xT": xT[b], "paT": parts[2 * b], "pbT": parts[2 * b + 1], "fgT": fgT})
    res = run_bass_kernel_spmd(_prog(False, True), in_maps, core_ids=list(range(len(cores))))
    out = np.stack([np.asarray(res.results[2 * b]["outT"]).T for b in range(B)]).astype(np.float32)
    return np.ascontiguousarray(out)
```

```python
from contextlib import ExitStack
import numpy as np
import ml_dtypes
import concourse.bass as bass
import concourse.mybir as mybir
from concourse.bass_utils import run_bass_kernel_spmd

F32 = mybir.dt.float32
BF16 = mybir.dt.bfloat16
AF = mybir.ActivationFunctionType
ALU = mybir.AluOpType

D = 1024
S = 8192
NT = 16
TT = 512
NB = 64
NEG = -30000.0
RMS_EPS = 1e-6
LN_EPS = 1e-5

FM_AQ01, FM_AQ23, FM_KVC, FM_KSW, FM_G0, FM_G1, FM_G2, FM_AZ, FM_BQ, FM_BK, FM_BZ, \
    FM_CQ, FM_CK, FM_CZ, FM_DQ, FM_DK, FM_DZ = range(17)
NFM = 17
Q_TILES = (FM_AQ01, FM_AQ23, FM_BQ, FM_CQ, FM_DQ)
TM_VSEL, TM_VWIN, TM_BV, TM_CV0, TM_CV1, TM_DV0, TM_DV1, TM_DK0, TM_DK1 = [64 * i for i in range(9)]
NTM = 576
NCOL = NFM * 128 + NTM

MK_STRICT = 0
MK_NONSTRICT = 4
MK_WINA = 8
MK_WINB = 12
MK_CMP = 17
NMASK = 25


class Prog:
    def __init__(self, nc, ctx, n_dma_sems=24):
        self.nc = nc
        self.eng = {"pe": nc.tensor, "act": nc.scalar, "dve": nc.vector, "pool": nc.gpsimd, "sp": nc.sync}
        self.sems = []
        self.semid = {}
        for e in self.eng:
            self.semid[e] = len(self.sems)
            self.sems.append(ctx.enter_context(nc.semaphore("p_" + e)))
        self.cnt = {e: 0 for e in self.eng}
        self.dsem = []
        for i in range(n_dma_sems):
            self.dsem.append(len(self.sems))
            self.sems.append(ctx.enter_context(nc.semaphore("d%d" % i)))
        self.duse = [0] * n_dma_sems
        self.dnext = 0
        self.known = {e: {} for e in self.eng}
        self.res = {}
        self.out_tokens = []
        self.ninstr = 0

    def _st(self, k):
        st = self.res.get(k)
        if st is None:
            st = [None, {}]
            self.res[k] = st
        return st

    def _deps(self, reads, writes):
        need = {}

        def add(tok):
            s, v = tok
            if need.get(s, 0) < v:
                need[s] = v
        for k in reads:
            st = self._st(k)
            if st[0] is not None:
                add(st[0])
        for k in writes:
            st = self._st(k)
            if st[0] is not None:
                add(st[0])
            for s, v in st[1].items():
                add((s, v))
        return need

    def _wait(self, e, need):
        kn = self.known[e]
        for s, v in need.items():
            if e == "pe" and s == self.semid["pe"]:
                continue
            if kn.get(s, 0) >= v:
                continue
            self.eng[e].wait_ge(self.sems[s], v)
            kn[s] = v
            self.ninstr += 1

    def _record(self, tok, reads, writes):
        for k in reads:
            st = self._st(k)
            if st[1].get(tok[0], 0) < tok[1]:
                st[1][tok[0]] = tok[1]
        for k in writes:
            st = self._st(k)
            st[0] = tok
            st[1] = {}

    def op(self, e, fn, reads=(), writes=()):
        self._wait(e, self._deps(reads, writes))
        ins = fn(self.eng[e])
        self.cnt[e] += 1
        tok = (self.semid[e], self.cnt[e])
        ins.then_inc(self.sems[tok[0]], 1)
        self._record(tok, reads, writes)
        self.ninstr += 1
        return tok

    def dma(self, q, out, in_, reads=(), writes=(), is_output=False):
        k = self.dnext
        self.dnext = (k + 1) % len(self.dsem)
        need = self._deps(reads, writes)
        if self.duse[k] > 0:
            s = self.dsem[k]
            if need.get(s, 0) < 16 * self.duse[k]:
                need[s] = 16 * self.duse[k]
        self._wait(q, need)
        ins = self.eng[q].dma_start(out=out, in_=in_)
        self.duse[k] += 1
        tok = (self.dsem[k], 16 * self.duse[k])
        ins.then_inc(self.sems[tok[0]], 16)
        self._record(tok, reads, writes)
        if is_output:
            self.out_tokens.append(tok)
        self.ninstr += 1
        return tok

    def barrier(self):
        need = {}
        for e in self.eng:
            if self.cnt[e] > 0:
                need[self.semid[e]] = self.cnt[e]
        for k, s in enumerate(self.dsem):
            if self.duse[k] > 0:
                need[s] = 16 * self.duse[k]
        for e in self.eng:
            n2 = {s: v for s, v in need.items() if s != self.semid[e] or e != "pe"}
            self._wait(e, n2)
        self.res = {}

    def finish(self):
        need = {}
        for s, v in self.out_tokens:
            if need.get(s, 0) < v:
                need[s] = v
        self._wait("sp", need)


def _bc(ap, shape, axis):
    return ap.unsqueeze(axis).broadcast_to(shape)


def build_program(first, last_only, dbg=None, stop_after=99, mixers="ABCD"):
    dbg = dbg or set()
    nc = bass.Bass("TRN2", target_bir_lowering=False)

    def din(name, shape, dt=F32):
        return nc.dram_tensor(name, list(shape), dt, kind="ExternalInput").ap()

    def dout(name, shape, dt=F32):
        return nc.dram_tensor(name, list(shape), dt, kind="ExternalOutput").ap()

    def dscr(name, shape, dt):
        kind = "ExternalOutput" if name in dbg else "Internal"
        return nc.dram_tensor(name, list(shape), dt, kind=kind).ap()

    xT = din("xT", [D, S])
    if not first:
        paT = din("paT", [D, S])
        pbT = din("pbT", [D, S])
    if last_only:
        fgT = din("fgT", [128, 8])
        outT = dout("outT", [D, S])
    else:
        cT = din("cT", [128, 8])
        w_ada = din("w_ada", [D, 3 * D])
        b_adaT = din("b_adaT", [128, 24])
        gT = din("gT", [128, 8])
        w_in = din("w_in", [D, NCOL])
        cmp_posT = din("cmp_posT", [64, 2, 32])
        cmp_w1 = din("cmp_w1", [64, 2, 32, 64])
        cmp_w2 = din("cmp_w2", [64, 2, 64])
        sinkb = din("sinkb", [128, 1])
        w_merge = din("w_merge", [4, D, D])
        w_br = din("w_br", [4, 128, D])
        w_out = din("w_out", [D, D])
        masks = din("masks", [NMASK, 128, TT], BF16)
        qpos = din("qpos", [4, 4, S], BF16)
        kpos_tok = din("kpos_tok", [4, S], BF16)
        kpos_cmp = din("kpos_cmp", [4, 256], BF16)
        ewide = din("ewide", [128, S], BF16)
        ident = din("ident", [128, 128], BF16)
        ntri = din("ntri", [128, 128], BF16)
        pairsum = din("pairsum", [2, 128, 128], BF16)
        selkeep = din("selkeep", [128, 256])
        seladd = din("seladd", [128, 256])
        dtab = din("dtab", [128, 2, 128])
        zeta = din("zeta", [128, 2])
        xi_bc = din("xi_bc", [64, 2, 128])
        gchunk = din("gchunk", [64, 2])
        partT = dout("partT", [D, S])
        if not first:
            xcT = dout("xcT", [D, S])
        hT = dscr("hT", [D, S], BF16)
        fm = dscr("fm", [NFM * 128, S], BF16)
        tm = dscr("tm", [S, NTM], BF16)
        yzd = dscr("yzd", [512, S], BF16)

    ctx = ExitStack()
    with ctx:
        P = Prog(nc, ctx)
        banks = [ctx.enter_context(nc.psum_tensor("bank%d" % i, [128, 512], F32)) for i in range(8)]

        def sb(c, name, shape, dt):
            return c.enter_context(nc.sbuf_tensor(name, list(shape), dt))

        ones_f = sb(ctx, "ones_f", [128, 128], F32)
        P.op("dve", lambda e: e.memset(ones_f[:], 1.0), writes=["ones_f"])
        xview = xT.rearrange("(k p) t -> p k t", p=128)

        if last_only:
            with ExitStack() as c:
                fg = sb(c, "fg", [128, 8], F32)
                P.dma("sp", fg[:], fgT[:, :], writes=["fg"])
                pav = paT.rearrange("(k p) t -> p k t", p=128)
                pbv = pbT.rearrange("(k p) t -> p k t", p=128)
                ov = outT.rearrange("(k p) t -> p k t", p=128)
                xt = [sb(c, "xt%d" % i, [128, 8, TT], F32) for i in range(2)]
                pt = [sb(c, "pt%d" % i, [128, 8, TT], F32) for i in range(2)]
                qt = [sb(c, "qt%d" % i, [128, 8, TT], F32) for i in range(2)]
                sq = sb(c, "sq", [128, 8, TT], F32)
                rt = sb(c, "rt", [128, TT], F32)
                rstd = sb(c, "rstd", [128, TT], F32)
                for tt in range(NT):
                    i = tt % 2
                    ts_ = slice(tt * TT, (tt + 1) * TT)
                    P.dma("sp", xt[i][:], xview[:, :, ts_], writes=[("xt", i)])
                    P.dma("sp", pt[i][:], pav[:, :, ts_], writes=[("pt", i)])
                    P.dma("sp", qt[i][:], pbv[:, :, ts_], writes=[("qt", i)])
                    P.op("dve", lambda e: e.tensor_tensor(out=xt[i][:], in0=xt[i][:], in1=pt[i][:], op=ALU.add),
                         reads=[("xt", i), ("pt", i)], writes=[("xt", i)])
                    P.op("dve", lambda e: e.tensor_tensor(out=xt[i][:], in0=xt[i][:], in1=qt[i][:], op=ALU.add),
                         reads=[("xt", i), ("qt", i)], writes=[("xt", i)])
                    P.op("act", lambda e: e.activation(out=sq[:], in_=xt[i][:], func=AF.Square),
                         reads=[("xt", i)], writes=["sq"])
                    bk = banks[tt % 2]
                    for kc in range(8):
                        P.op("pe", lambda e: e.matmul(bk[:], ones_f[:], sq[:, kc, :], start=(kc == 0), stop=(kc == 7)),
                             reads=["sq", "ones_f"], writes=[("bank", tt % 2)])
                    P.op("act", lambda e: e.activation(out=rt[:], in_=bk[:], func=AF.Sqrt, scale=1.0 / D, bias=RMS_EPS),
                         reads=[("bank", tt % 2)], writes=["rt"])
                    P.op("dve", lambda e: e.reciprocal(out=rstd[:], in_=rt[:]), reads=["rt"], writes=["rstd"])
                    P.op("dve", lambda e: e.tensor_tensor(out=xt[i][:], in0=xt[i][:],
                                                          in1=_bc(rstd[:], [128, 8, TT], 1), op=ALU.mult),
                         reads=[("xt", i), "rstd"], writes=[("xt", i)])
                    for kc in range(8):
                        P.op("act", lambda e: e.activation(out=xt[i][:, kc, :], in_=xt[i][:, kc, :], func=AF.Copy,
                                                           scale=fg[:, kc:kc + 1]),
                             reads=[("xt", i), "fg"], writes=[("xt", i)])
                    P.dma("pool", ov[:, :, ts_], xt[i][:], reads=[("xt", i)], is_output=True)
            P.finish()
            return nc


        ident_b = sb(ctx, "ident_b", [128, 128], BF16)
        P.dma("sp", ident_b[:], ident[:, :], writes=["ident_b"])
        ones_b = sb(ctx, "ones_b", [128, 128], BF16)
        P.op("dve", lambda e: e.memset(ones_b[:], 1.0), writes=["ones_b"])
        mod = sb(ctx, "mod", [128, 24], F32)
        gs = sb(ctx, "gs", [128, 8], F32)
        with ExitStack() as c:
            cs = sb(c, "cs", [128, 8], F32)
            csil = sb(c, "csil", [128, 8, 2], F32)
            bada = sb(c, "bada", [128, 24], F32)
            gt_ = sb(c, "gt_", [128, 8], F32)
            wa = [sb(c, "wa%d" % i, [128, 3 * D], F32) for i in range(2)]
            P.dma("sp", cs[:], cT[:, :], writes=["cs"])
            P.dma("sp", bada[:], b_adaT[:, :], writes=["bada"])
            P.dma("sp", gt_[:], gT[:, :], writes=["gt_"])
            for j in range(2):
                P.op("act", lambda e: e.activation(out=csil[:, :, j], in_=cs[:], func=AF.Silu),
                     reads=["cs"], writes=["csil"])
            for kc in range(8):
                i = kc % 2
                P.dma("sp", wa[i][:], w_ada[kc * 128:(kc + 1) * 128, :], writes=[("wa", i)])
                for oc in range(24):
                    P.op("pe", lambda e: e.matmul(banks[0][:, 2 * oc:2 * oc + 2], wa[i][:, oc * 128:(oc + 1) * 128],
                                                  csil[:, kc, :], start=(kc == 0 and oc == 0), stop=(kc == 7 and oc == 23)),
                         reads=[("wa", i), "csil"], writes=[("bank", 0)])
            P.op("dve", lambda e: e.tensor_tensor(out=mod[:], in0=banks[0][:, 0:48:2], in1=bada[:], op=ALU.add),
                 reads=[("bank", 0), "bada"], writes=["mod"])
            P.op("dve", lambda e: e.scalar_tensor_tensor(out=gs[:], in0=mod[:, 8:16], scalar=1.0, in1=gt_[:],
                                                         op0=ALU.add, op1=ALU.mult),
                 reads=["mod", "gt_"], writes=["gs"])
            P.barrier()

        def load_cast(c_stage, stg, dst_ap, src_ap, n, key_dst, idx):
            i = idx % 2
            P.dma("sp", stg[i][:, 0:n], src_ap, writes=[("stg", i)])
            eng = "dve" if idx % 2 == 0 else "pool"
            P.op(eng, lambda e: e.tensor_copy(dst_ap, stg[i][:, 0:n]), reads=[("stg", i)], writes=[key_dst])

        with ExitStack() as c:
            wb = sb(c, "wb", [128, 8, NCOL], BF16)
            stg = [sb(c, "stg%d" % i, [128, NCOL], F32) for i in range(2)]
            for kc in range(8):
                load_cast(c, stg, wb[:, kc, :], w_in[kc * 128:(kc + 1) * 128, :], NCOL, ("wb", kc), kc)
            xt = [sb(c, "xt%d" % i, [128, 8, TT], F32) for i in range(2)]
            if not first:
                pt = sb(c, "pt", [128, 8, TT], F32)
                pav = paT.rearrange("(k p) t -> p k t", p=128)
                pbv = pbT.rearrange("(k p) t -> p k t", p=128)
                xcv = xcT.rearrange("(k p) t -> p k t", p=128)
            sq = sb(c, "sq", [128, 8, TT], F32)
            rt = sb(c, "rt", [128, TT], F32)
            rstd = sb(c, "rstd", [128, TT], F32)
            hb = [sb(c, "hb%d" % i, [128, 8, TT], BF16) for i in range(2)]
            ev = [sb(c, "ev%d" % i, [128, TT], BF16) for i in range(4)]
            evt = [sb(c, "evt%d" % i, [128, NTM], BF16) for i in range(2)]
            hview = hT.rearrange("(k p) t -> p k t", p=128)
            ctr = {'nbank': 0, 'nev': 0}

            def s1_norm(tt):
                i = tt % 2
                ts_ = slice(tt * TT, (tt + 1) * TT)
                P.dma("sp", xt[i][:], xview[:, :, ts_], writes=[("xt", i)])
                if not first:
                    for pv in (pav, pbv):
                        P.dma("sp", pt[:], pv[:, :, ts_], writes=["pt"])
                        P.op("dve", lambda e: e.tensor_tensor(out=xt[i][:], in0=xt[i][:], in1=pt[:], op=ALU.add),
                             reads=[("xt", i), "pt"], writes=[("xt", i)])
                    P.dma("pool", xcv[:, :, ts_], xt[i][:], reads=[("xt", i)], is_output=True)
                P.op("act", lambda e: e.activation(out=sq[:], in_=xt[i][:], func=AF.Square),
                     reads=[("xt", i)], writes=["sq"])
                bi = ctr['nbank'] % 8
                ctr['nbank'] += 1
                for kc in range(8):
                    P.op("pe", lambda e: e.matmul(banks[bi][:], ones_f[:], sq[:, kc, :], start=(kc == 0), stop=(kc == 7)),
                         reads=["sq", "ones_f"], writes=[("bank", bi)])
                P.op("act", lambda e: e.activation(out=rt[:], in_=banks[bi][:], func=AF.Sqrt, scale=1.0 / D, bias=RMS_EPS),
                     reads=[("bank", bi)], writes=["rt"])
                P.op("dve", lambda e: e.reciprocal(out=rstd[:], in_=rt[:]), reads=["rt"], writes=["rstd"])
                P.op("dve", lambda e: e.tensor_tensor(out=sq[:], in0=xt[i][:], in1=_bc(rstd[:], [128, 8, TT], 1), op=ALU.mult),
                     reads=[("xt", i), "rstd", "sq"], writes=["sq"])
                for kc in range(8):
                    P.op("act", lambda e: e.activation(out=hb[i][:, kc, :], in_=sq[:, kc, :], func=AF.Identity,
                                                       scale=gs[:, kc:kc + 1], bias=mod[:, kc:kc + 1]),
                         reads=["sq", "gs", "mod"], writes=[("hb", i)])
                P.dma("pool", hview[:, :, ts_], hb[i][:], reads=[("hb", i)], writes=[("hT", tt)])

            def s1_proj(tt):
                i = tt % 2
                ts_ = slice(tt * TT, (tt + 1) * TT)
                for ct in range(NFM):
                    bi = ctr['nbank'] % 8
                    ctr['nbank'] += 1
                    for kc in range(8):
                        P.op("pe", lambda e: e.matmul(banks[bi][:], wb[:, kc, ct * 128:(ct + 1) * 128], hb[i][:, kc, :],
                                                      start=(kc == 0), stop=(kc == 7)),
                             reads=[("wb", kc), ("hb", i)], writes=[("bank", bi)])
                    ei = ctr['nev'] % 4
                    ctr['nev'] += 1
                    if ct in Q_TILES:
                        P.op("act", lambda e: e.activation(out=ev[ei][:], in_=banks[bi][:], func=AF.Copy, scale=0.125),
                             reads=[("bank", bi)], writes=[("ev", ei)])
                    elif ct % 2 == 0:
                        P.op("act", lambda e: e.activation(out=ev[ei][:], in_=banks[bi][:], func=AF.Copy),
                             reads=[("bank", bi)], writes=[("ev", ei)])
                    else:
                        P.op("dve", lambda e: e.tensor_copy(ev[ei][:], banks[bi][:]),
                             reads=[("bank", bi)], writes=[("ev", ei)])
                    P.dma("pool", fm[ct * 128:(ct + 1) * 128, ts_], ev[ei][:], reads=[("ev", ei)], writes=[("fm", ct, tt)])

            def s1_proj_tm(tt):
                i = tt % 2
                ts_ = slice(tt * TT, (tt + 1) * TT)
                for sub in range(4):
                    ei = (tt * 4 + sub) % 2
                    for g, (c0, c1) in enumerate(((0, 320), (320, 576))):
                        bi = ctr['nbank'] % 8
                        ctr['nbank'] += 1
                        for kc in range(8):
                            P.op("pe", lambda e: e.matmul(banks[bi][:, 0:c1 - c0], hb[i][:, kc, sub * 128:(sub + 1) * 128],
                                                          wb[:, kc, NFM * 128 + c0:NFM * 128 + c1],
                                                          start=(kc == 0), stop=(kc == 7)),
                                 reads=[("wb", kc), ("hb", i)], writes=[("bank", bi)])
                        P.op("dve" if g == 0 else "act",
                             (lambda e: e.tensor_copy(evt[ei][:, c0:c1], banks[bi][:, 0:c1 - c0])) if g == 0 else
                             (lambda e: e.activation(out=evt[ei][:, c0:c1], in_=banks[bi][:, 0:c1 - c0], func=AF.Copy)),
                             reads=[("bank", bi)], writes=[("evt", ei)])
                    r0 = tt * TT + sub * 128
                    P.dma("pool", tm[r0:r0 + 128, :], evt[ei][:], reads=[("evt", ei)], writes=[("tm", tt)])

            s1_norm(0)
            for tt in range(NT):
                s1_proj(tt)
                if tt + 1 < NT:
                    s1_norm(tt + 1)
                s1_proj_tm(tt)
            P.barrier()

        if stop_after <= 1:
            P.finish()
            return nc
        hh_ = 0
        qpos_own = [qpos[0], qpos[1]]
        fmv = fm
        tmv = tm.rearrange("(n p) c -> p n c", p=128)
        stage2 = ExitStack()
        masks_sb = sb(stage2, "masks_sb", [128, NMASK, TT], BF16)
        P.dma("sp", masks_sb[:], masks.rearrange("m p t -> p m t"), writes=["masks_sb"])
        zrot = [0]
        arot = [0]
        a_sb = [sb(stage2, "a_sb%d" % i, [128, TT], BF16) for i in range(3)]
        o_sb = sb(stage2, "o_sb", [64, TT], F32)
        dn_sb = sb(stage2, "dn_sb", [64, TT], F32)
        ACC = 3

        def softmax_tile(q_ap, qkey, ka, kakey, vo, vokey, chunks, selmask=None, sink_ap=None, selkey=None, bg=None):
            n = len(chunks)
            slots = []

            def qk(idx):
                kc, mk = chunks[idx]
                zi = zrot[0] % 3
                zrot[0] += 1
                ai = arot[0] % 3
                arot[0] += 1
                slots.append((zi, ai))
                mms = [(ka[0:68, kc * 128:(kc + 1) * 128], q_ap, [kakey, qkey])]
                if selmask is not None:
                    mms.append((ewide_sb[:, kc * 128:(kc + 1) * 128], selmask[:], ["ewide_sb", selkey]))
                if mk is not None:
                    mms.append((ident_b[:], masks_sb[:, mk, :], ["ident_b", "masks_sb"]))
                for j, (l_, r_, rd) in enumerate(mms):
                    P.op("pe", lambda e: e.matmul(banks[zi][:], l_, r_, start=(j == 0), stop=(j == len(mms) - 1)),
                         reads=rd, writes=[("bank", zi)])

            def ex(idx):
                zi, ai = slots[idx]
                P.op("act", lambda e: e.activation(out=a_sb[ai][:], in_=banks[zi][:], func=AF.Exp),
                     reads=[("bank", zi)], writes=[("a_sb", ai)])

            def av(idx):
                kc, mk = chunks[idx]
                zi, ai = slots[idx]
                P.op("pe", lambda e: e.matmul(banks[ACC][:], vo[:, kc, :], a_sb[ai][:], start=(idx == 0), stop=(idx == n - 1)),
                     reads=[vokey, ("a_sb", ai)], writes=[("bank", ACC)])

            for step in range(n + 2):
                if step < n:
                    qk(step)
                if 0 <= step - 1 < n:
                    ex(step - 1)
                if 0 <= step - 2 < n:
                    av(step - 2)
                if bg is not None:
                    next(bg, None)
            if sink_ap is not None:
                P.op("dve", lambda e: e.tensor_scalar(out=dn_sb[:], in0=banks[ACC][64:128, :], scalar1=sink_ap, scalar2=1e-30,
                                                      op0=ALU.add, op1=ALU.max),
                     reads=[("bank", ACC), "esink"], writes=["dn_sb"])
            else:
                P.op("dve", lambda e: e.tensor_scalar_max(out=dn_sb[:], in0=banks[ACC][64:128, :], scalar1=1e-30),
                     reads=[("bank", ACC)], writes=["dn_sb"])
            P.op("dve", lambda e: e.reciprocal(out=dn_sb[:], in_=dn_sb[:]), reads=["dn_sb"], writes=["dn_sb"])
            P.op("dve", lambda e: e.tensor_tensor(out=o_sb[:], in0=banks[ACC][0:64, :], in1=dn_sb[:], op=ALU.mult),
                 reads=[("bank", ACC), "dn_sb"], writes=["o_sb"])

        def build_vo(c, name, col):
            vo = sb(c, name, [128, NB, 128], BF16)
            P.dma("sp", vo[:, :, 0:64], tmv[:, :, col:col + 64], writes=[name])
            P.op("pool", lambda e: e.memset(vo[:, :, 64:128], 1.0), writes=[name])
            return vo

        def build_ka(c, name, ct, row0, kp):
            ka = sb(c, name, [68, S], BF16)
            P.dma("sp", ka[0:64, :], fmv[ct * 128 + row0:ct * 128 + row0 + 64, :], writes=[name])
            P.dma("sp", ka[64:68, :], kp, writes=[name])
            return ka

        if "B" in mixers or "A" in mixers:
          with ExitStack() as c:
            qa = [sb(c, "qa%d" % h, [68, TT], BF16) for h in range(4)]
            gl = [sb(c, "gl%d" % i, [64, TT], F32) for i in range(6)]
            zt_sb = sb(c, "zt_sb", [128, TT], BF16)
            zs_sb = sb(c, "zs_sb", [128, TT], F32)
            ya = sb(c, "ya", [128, TT], F32)
            yo = sb(c, "yo", [128, TT], BF16)
            tmp64 = sb(c, "tmp64", [64, TT], F32)
            if "B" in mixers:
                cB = ExitStack()
                kb = build_ka(cB, "kb", FM_BK, 0, kpos_tok[:, :])
                vb = build_vo(cB, "vb", TM_BV)
                esink = sb(cB, "esink", [128, 1], F32)
                P.dma("sp", esink[:], sinkb[:, :], writes=["esink"])
                P.op("act", lambda e: e.activation(out=esink[:], in_=esink[:], func=AF.Exp), reads=["esink"], writes=["esink"])
                es2 = sb(cB, "es2", [64, 2], F32)
                P.op("dve", lambda e: e.tensor_copy(es2[:, 0:1], esink[0:64, :]), reads=["esink"], writes=["esink2"])
                P.op("dve", lambda e: e.tensor_copy(es2[:, 1:2], esink[64:128, :]), reads=["esink"], writes=["esink2"])
                for tt in range(NT):
                    ts_ = slice(tt * TT, (tt + 1) * TT)
                    P.dma("sp", zt_sb[:], fmv[FM_BZ * 128:(FM_BZ + 1) * 128, ts_], writes=["zt_sb"])
                    P.op("act", lambda e: e.activation(out=zs_sb[:], in_=zt_sb[:], func=AF.Silu), reads=["zt_sb"], writes=["zs_sb"])
                    for hl in range(2):
                        h = 2 * hh_ + hl
                        P.dma("sp", qa[hl][0:64, :], fmv[FM_BQ * 128 + hl * 64:FM_BQ * 128 + hl * 64 + 64, ts_], writes=[("qa", hl)])
                        P.dma("sp", qa[hl][64:68, :], qpos_own[hl][:, ts_], writes=[("qa", hl)])
                        chunks = [(kc, MK_WINB + (kc - 4 * tt + 1)) for kc in range(max(0, 4 * tt - 1), 4 * tt + 4)]
                        softmax_tile(qa[hl][:], ("qa", hl), kb, "kb", vb, "vb", chunks, sink_ap=es2[:, hl:hl + 1])
                        P.op("dve", lambda e: e.tensor_copy(ya[hl * 64:hl * 64 + 64, :], o_sb[:]),
                             reads=["o_sb"], writes=["ya"])
                    P.op("dve", lambda e: e.tensor_tensor(out=yo[:], in0=ya[:], in1=zs_sb[:], op=ALU.mult),
                         reads=["ya", "zs_sb"], writes=["yo"])
                    P.dma("pool", yzd[128:256, ts_], yo[:], reads=["yo"], writes=[("yzd", 1, tt)])
                P.barrier()
                cB.close()
            if "A" in mixers:
             with ExitStack() as c2:
              ksel = build_ka(c2, "ksel", FM_KSW, 0, kpos_tok[:, :])
              kwin = build_ka(c2, "kwin", FM_KSW, 64, kpos_tok[:, :])
              vsel = build_vo(c2, "vsel", TM_VSEL)
              vwin = build_vo(c2, "vwin", TM_VWIN)
              ewide_sb = sb(c2, "ewide_sb", [128, S], BF16)
              P.dma("sp", ewide_sb[:], ewide[:, :], writes=["ewide_sb"])
              keep_sb = sb(c2, "keep_sb", [128, 256], F32)
              add_sb = sb(c2, "add_sb", [128, 256], F32)
              P.dma("sp", keep_sb[:], selkeep[:, :], writes=["keep_sb"])
              P.dma("sp", add_sb[:], seladd[:, :], writes=["add_sb"])
              ps_sb = sb(c2, "ps_sb", [128, 2, 128], BF16)
              P.dma("sp", ps_sb[:], pairsum.rearrange("c p b -> p c b"), writes=["ps_sb"])
              kca = sb(c2, "kca", [68, 256], BF16)
              vcs = sb(c2, "vcs", [128, 2, 64], BF16)
              P.dma("sp", kca[64:68, :], kpos_cmp[:, :], writes=["kca"])
              with ExitStack() as c3:
                  kv = sb(c3, "kv", [128, S], BF16)
                  P.dma("sp", kv[:], fmv[FM_KVC * 128:(FM_KVC + 1) * 128, :], writes=["kv"])
                  posf = sb(c3, "posf", [128, 32], F32)
                  P.dma("sp", posf[0:64, :], cmp_posT[:, 0, :], writes=["posf"])
                  P.dma("sp", posf[64:128, :], cmp_posT[:, 1, :], writes=["posf"])
                  kvp = sb(c3, "kvp", [128, 256, 32], BF16)
                  P.op("dve", lambda e: e.tensor_tensor(out=kvp[:], in0=kv[:].rearrange("p (c j) -> p c j", j=32),
                                                        in1=_bc(posf[:], [128, 256, 32], 1), op=ALU.add),
                       reads=["kv", "posf"], writes=["kvp"])
                  w1f = sb(c3, "w1f", [128, 32, 64], F32)
                  w1b = sb(c3, "w1b", [128, 32, 64], BF16)
                  P.dma("sp", w1f[0:64], cmp_w1[:, 0], writes=["w1f"])
                  P.dma("sp", w1f[64:128], cmp_w1[:, 1], writes=["w1f"])
                  P.op("dve", lambda e: e.tensor_copy(w1b[:], w1f[:]), reads=["w1f"], writes=["w1b"])
                  w2f = sb(c3, "w2f", [128, 64], F32)
                  w2b = sb(c3, "w2b", [128, 64], BF16)
                  P.dma("sp", w2f[0:64], cmp_w2[:, 0], writes=["w2f"])
                  P.dma("sp", w2f[64:128], cmp_w2[:, 1], writes=["w2f"])
                  P.op("dve", lambda e: e.tensor_copy(w2b[:], w2f[:]), reads=["w2f"], writes=["w2b"])
                  hk = sb(c3, "hk", [128, 256], BF16)
                  for jj, p0 in enumerate((0, 64)):
                      for j in range(32):
                          P.op("pe", lambda e: e.matmul(banks[jj][p0:p0 + 64, 0:256], w1b[p0:p0 + 64, j, :], kvp[p0:p0 + 64, :, j],
                                                        start=(j == 0), stop=(j == 31)),
                               reads=["w1b", "kvp"], writes=[("bank", jj)])
                      P.op("act", lambda e: e.activation(out=hk[p0:p0 + 64, :], in_=banks[jj][p0:p0 + 64, 0:256], func=AF.Silu),
                           reads=[("bank", jj)], writes=["hk"])
                  P.op("pe", lambda e: e.matmul(banks[2][0:64, 0:256], w2b[0:64, :], hk[0:64, :], start=True, stop=True),
                       reads=["w2b", "hk"], writes=[("bank", 2)])
                  P.op("dve", lambda e: e.tensor_copy(kca[0:64, :], banks[2][0:64, 0:256]), reads=[("bank", 2)], writes=["kca"])
                  for cc in range(2):
                      P.op("pe", lambda e: e.matmul(banks[4 + cc][:, 0:64], hk[64:128, cc * 128:(cc + 1) * 128], w2b[64:128, :],
                                                    start=True, stop=True),
                           reads=["w2b", "hk"], writes=[("bank", 4 + cc)])
                      P.op("dve", lambda e: e.tensor_copy(vcs[:, cc, :], banks[4 + cc][:, 0:64]), reads=[("bank", 4 + cc)], writes=["vcs"])
                  P.barrier()
              e_sb = [sb(c2, "e_sb%d" % i, [128, TT], BF16) for i in range(8)]
              rec = sb(c2, "rec", [128, TT], F32)
              adj = sb(c2, "adj", [128, 128], F32)
              adj2 = sb(c2, "adj2", [128, 128], F32)
              m8a = sb(c2, "m8a", [128, 8], F32)
              m8b = sb(c2, "m8b", [128, 8], F32)
              mt = sb(c2, "mt", [128, 128], BF16)
              ocmp = sb(c2, "ocmp", [64, 2, TT], F32)
              bank4b = banks[4][:].bitcast(BF16)
              qa2 = [qa, [sb(c2, "qab%d" % h, [68, TT], BF16) for h in range(4)]]
              gl2 = [gl, [sb(c2, "glb%d" % i, [64, TT], F32) for i in range(6)]]
              ya2 = [ya, sb(c2, "yab", [128, TT], F32)]
              mneg2 = [sb(c2, "mneg%d" % i, [128, TT], BF16) for i in range(2)]
              gt_sb = sb(c2, "gt_sb", [64, TT], BF16)
              DEN = 7

              def prep(tt):
                  bf = tt % 2
                  ts_ = slice(tt * TT, (tt + 1) * TT)
                  qa_, gl_, ya_, mneg_ = qa2[bf], gl2[bf], ya2[bf], mneg2[bf]
                  for h in range(4):
                      ct = FM_AQ01 if h < 2 else FM_AQ23
                      r0 = ct * 128 + (h % 2) * 64
                      P.dma("sp", qa_[h][0:64, :], fmv[r0:r0 + 64, ts_], writes=[("qa", bf, h)])
                      P.dma("sp", qa_[h][64:68, :], qpos[h, :, ts_], writes=[("qa", bf, h)])
                  yield
                  for j in range(3):
                      for hl in range(2):
                          r0 = (FM_G0 + j) * 128 + hl * 64
                          gi = j * 2 + hl
                          P.dma("sp", gt_sb[:], fmv[r0:r0 + 64, ts_], writes=["gt_sb"])
                          P.op("act", lambda e: e.activation(out=gl_[gi][:], in_=gt_sb[:], func=AF.Sigmoid),
                               reads=["gt_sb"], writes=[("gl", bf, gi)])
                      yield
                  ccs = [0] if tt < 8 else [0, 1]
                  for h in range(4):
                      for cc in ccs:
                          zi = zrot[0] % 3
                          zrot[0] += 1
                          m = tt - 8 * cc
                          P.op("pe", lambda e: e.matmul(banks[zi][:], kca[0:68, cc * 128:(cc + 1) * 128], qa_[h][:],
                                                        start=True, stop=(m >= 8)),
                               reads=["kca", ("qa", bf, h)], writes=[("bank", zi)])
                          if m < 8:
                              P.op("pe", lambda e: e.matmul(banks[zi][:], ident_b[:], masks_sb[:, MK_CMP + m, :], start=False, stop=True),
                                   reads=["ident_b", "masks_sb"], writes=[("bank", zi)])
                          P.op("act", lambda e: e.activation(out=e_sb[h * 2 + cc][:], in_=banks[zi][:], func=AF.Exp),
                               reads=[("bank", zi)], writes=[("e_sb", h * 2 + cc)])
                          yield
                      for cc in ccs:
                          P.op("pe", lambda e: e.matmul(banks[DEN][:], ones_b[:], e_sb[h * 2 + cc][:], start=(cc == 0), stop=(cc == ccs[-1])),
                               reads=["ones_b", ("e_sb", h * 2 + cc)], writes=[("bank", DEN)])
                      P.op("dve", lambda e: e.tensor_scalar_max(out=rec[:], in0=banks[DEN][:], scalar1=1e-30),
                           reads=[("bank", DEN)], writes=["rec"])
                      P.op("dve", lambda e: e.reciprocal(out=rec[:], in_=rec[:]), reads=["rec"], writes=["rec"])
                      yield
                      for cc in ccs:
                          P.op("dve" if cc == 0 else "pool",
                               lambda e: e.tensor_tensor(out=e_sb[h * 2 + cc][:], in0=e_sb[h * 2 + cc][:], in1=rec[:], op=ALU.mult),
                               reads=[("e_sb", h * 2 + cc), "rec"], writes=[("e_sb", h * 2 + cc)])
                      if h // 2 == hh_:
                          hl = h % 2
                          for cc in ccs:
                              P.op("pe", lambda e: e.matmul(banks[5][0:64, :], vcs[:, cc, :], e_sb[h * 2 + cc][:],
                                                            start=(cc == 0), stop=(cc == ccs[-1])),
                                   reads=["vcs", ("e_sb", h * 2 + cc)], writes=[("bank", 5)])
                          P.op("dve", lambda e: e.tensor_tensor(out=ya_[hl * 64:hl * 64 + 64, :], in0=banks[5][0:64, :], in1=gl_[0 * 2 + hl][:],
                                                                op=ALU.mult),
                               reads=[("bank", 5), ("gl", bf, hl)], writes=[("ya", bf)])
                      yield
                  for sub in range(4):
                      i_blk = tt * 4 + sub
                      first_mm = True
                      nmm = 4 * len(ccs)
                      k_ = 0
                      for h in range(4):
                          for cc in ccs:
                              k_ += 1
                              P.op("pe", lambda e: e.matmul(banks[6][:, 0:128], e_sb[h * 2 + cc][:, sub * 128:(sub + 1) * 128], ps_sb[:, cc, :],
                                                            start=first_mm, stop=(k_ == nmm)),
                                   reads=[("e_sb", h * 2 + cc), "ps_sb"], writes=[("bank", 6)])
                              first_mm = False
                          if h == 1:
                              yield
                      o0 = 128 - 2 * i_blk
                      P.op("dve", lambda e: e.tensor_tensor(out=adj[:], in0=banks[6][:, 0:128], in1=keep_sb[:, o0:o0 + 128], op=ALU.mult),
                           reads=[("bank", 6), "keep_sb"], writes=["adj"])
                      P.op("dve", lambda e: e.tensor_tensor(out=adj[:], in0=adj[:], in1=add_sb[:, o0:o0 + 128], op=ALU.add),
                           reads=["adj", "add_sb"], writes=["adj"])
                      P.op("dve", lambda e: e.memset(adj[:, 0:1], 1e4), reads=["adj"], writes=["adj"])
                      yield
                      P.op("dve", lambda e: e.max(out=m8a[:], in_=adj[:]), reads=["adj"], writes=["m8a"])
                      P.op("dve", lambda e: e.match_replace(out=adj2[:], in_to_replace=m8a[:], in_values=adj[:], imm_value=-1e9),
                           reads=["adj", "m8a"], writes=["adj2"])
                      yield
                      P.op("dve", lambda e: e.max(out=m8b[:], in_=adj2[:]), reads=["adj2"], writes=["m8b"])
                      P.op("dve", lambda e: e.tensor_scalar(out=mt[:], in0=adj[:], scalar1=m8b[:, 7:8], scalar2=-1.0,
                                                            op0=ALU.is_ge, op1=ALU.add),
                           reads=["adj", "m8b"], writes=["mt"])
                      yield
                      P.op("pe", lambda e: e.transpose(bank4b[:, 0:128], mt[:], ident_b[:]),
                           reads=["mt", "ident_b"], writes=[("bank", 4)])
                      P.op("act", lambda e: e.activation(out=mneg_[:, sub * 128:(sub + 1) * 128], in_=bank4b[:, 0:128], func=AF.Copy),
                           reads=[("bank", 4)], writes=[("mneg", bf)])
                      yield

              def selwin(tt, bg):
                  bf = tt % 2
                  ts_ = slice(tt * TT, (tt + 1) * TT)
                  qa_, gl_, ya_, mneg_ = qa2[bf], gl2[bf], ya2[bf], mneg2[bf]
                  for hl in range(2):
                      h = 2 * hh_ + hl
                      chunks = [(kc, (MK_NONSTRICT + kc - 4 * tt) if kc >= 4 * tt else None) for kc in range(0, 4 * tt + 4)]
                      softmax_tile(qa_[h][:], ("qa", bf, h), ksel, "ksel", vsel, "vsel", chunks, selmask=mneg_, selkey=("mneg", bf), bg=bg)
                      P.op("dve", lambda e: e.tensor_tensor(out=tmp64[:], in0=o_sb[:], in1=gl_[1 * 2 + hl][:], op=ALU.mult),
                           reads=["o_sb", ("gl", bf, 2 + hl)], writes=["tmp64"])
                      P.op("dve", lambda e: e.tensor_copy(ocmp[:, hl, :], tmp64[:]), reads=["tmp64"], writes=["ocmp"])
                      chunks = []
                      for kc in range(max(0, 4 * tt - 4), 4 * tt + 4):
                          r = kc - 4 * tt
                          chunks.append((kc, MK_WINA + r + 4 if r < 0 else MK_NONSTRICT + r))
                      softmax_tile(qa_[h][:], ("qa", bf, h), kwin, "kwin", vwin, "vwin", chunks, bg=bg)
                      P.op("dve", lambda e: e.tensor_tensor(out=tmp64[:], in0=o_sb[:], in1=gl_[2 * 2 + hl][:], op=ALU.mult),
                           reads=["o_sb", ("gl", bf, 4 + hl)], writes=["tmp64"])
                      P.op("dve", lambda e: e.tensor_tensor(out=tmp64[:], in0=tmp64[:], in1=ocmp[:, hl, :], op=ALU.add),
                           reads=["tmp64", "ocmp"], writes=["tmp64"])
                      P.op("dve", lambda e: e.tensor_copy(ocmp[:, hl, :], ya_[hl * 64:hl * 64 + 64, :]), reads=[("ya", bf)], writes=["ocmp"])
                      P.op("dve", lambda e: e.tensor_tensor(out=ya_[hl * 64:hl * 64 + 64, :], in0=tmp64[:], in1=ocmp[:, hl, :], op=ALU.add),
                           reads=["tmp64", "ocmp"], writes=[("ya", bf)])
                  P.dma("sp", zt_sb[:], fmv[FM_AZ * 128:(FM_AZ + 1) * 128, ts_], writes=["zt_sb"])
                  P.op("act", lambda e: e.activation(out=zs_sb[:], in_=zt_sb[:], func=AF.Silu), reads=["zt_sb"], writes=["zs_sb"])
                  P.op("dve", lambda e: e.tensor_tensor(out=yo[:], in0=ya_[:], in1=zs_sb[:], op=ALU.mult),
                       reads=[("ya", bf), "zs_sb"], writes=["yo"])
                  P.dma("pool", yzd[0:128, ts_], yo[:], reads=["yo"], writes=[("yzd", 0, tt)])

              for _ in prep(0):
                  pass
              for tt in range(NT):
                  bg = prep(tt + 1) if tt + 1 < NT else None
                  selwin(tt, bg)
                  if bg is not None:
                      for _ in bg:
                          pass
              P.barrier()
        if "C" in mixers:
          with ExitStack() as c:
            ntri_sb = sb(c, "ntri_sb", [128, 128], BF16)
            P.dma("sp", ntri_sb[:], ntri[:, :], writes=["ntri_sb"])
            qs = sb(c, "qs", [64, S], BF16)
            ks = sb(c, "ks", [64, S], BF16)
            vs = sb(c, "vs", [128, NB, 64], BF16)
            ee = [sb(c, "ee%d" % i, [128, TT], F32) for i in range(2)]
            sp_ = [sb(c, "sp%d" % i, [128, TT], F32) for i in range(2)]
            hi = [sb(c, "hi%d" % i, [128, TT], BF16) for i in range(3)]
            lo = [sb(c, "lo%d" % i, [128, TT], BF16) for i in range(3)]
            ww = [sb(c, "ww%d" % i, [64, TT], F32) for i in range(3)]
            aa = [sb(c, "aa%d" % i, [128, TT], BF16) for i in range(2)]
            tmpc = sb(c, "tmpc", [64, TT], F32)
            oacc = sb(c, "oacc", [64, TT], F32)
            zt_c = sb(c, "zt_c", [64, TT], BF16)
            zs_c = sb(c, "zs_c", [64, TT], F32)
            yo_c = sb(c, "yo_c", [64, TT], BF16)
            CARRY = 4
            for hl in range(2):
                P.dma("sp", qs[:], fmv[FM_CQ * 128 + hl * 64:FM_CQ * 128 + hl * 64 + 64, :], writes=["qs"])
                P.dma("sp", ks[:], fmv[FM_CK * 128 + hl * 64:FM_CK * 128 + hl * 64 + 64, :], writes=["ks"])
                P.dma("sp", vs[:], tmv[:, :, TM_CV0 + 64 * hl:TM_CV0 + 64 * hl + 64], writes=["vs"])
                recs = []
                for tt in range(NT):
                    kcs = list(range(4 * tt + 3, -1, -1))
                    for j, kc in enumerate(kcs):
                        recs.append(dict(tt=tt, kc=kc, first=(j == 0), last=(j == len(kcs) - 1), i=len(recs)))

                def s1(r):
                    i = r["i"]; zi = i % 4; tt = r["tt"]; kc = r["kc"]
                    diag = kc >= 4 * tt
                    P.op("pe", lambda e: e.matmul(banks[zi][:], ks[:, kc * 128:(kc + 1) * 128], qs[:, tt * TT:(tt + 1) * TT],
                                                  start=True, stop=not diag),
                         reads=["ks", "qs"], writes=[("bank", zi)])
                    if diag:
                        P.op("pe", lambda e: e.matmul(banks[zi][:], ident_b[:], masks_sb[:, MK_STRICT + kc - 4 * tt, :], start=False, stop=True),
                             reads=["ident_b", "masks_sb"], writes=[("bank", zi)])

                def s2(r):
                    i = r["i"]; zi = i % 4
                    P.op("act", lambda e: e.activation(out=ee[i % 2][:], in_=banks[zi][:], func=AF.Exp),
                         reads=[("bank", zi)], writes=[("ee", i % 2)])
                    P.op("act", lambda e: e.activation(out=sp_[i % 2][:], in_=ee[i % 2][:], func=AF.Ln, bias=1.0),
                         reads=[("ee", i % 2)], writes=[("sp", i % 2)])
                    P.op("dve", lambda e: e.tensor_copy(hi[i % 3][:], sp_[i % 2][:]), reads=[("sp", i % 2)], writes=[("hi", i % 3)])
                    P.op("dve" if i % 2 == 0 else "pool", lambda e: e.tensor_tensor(out=lo[i % 3][:], in0=sp_[i % 2][:], in1=hi[i % 3][:], op=ALU.subtract),
                         reads=[("sp", i % 2), ("hi", i % 3)], writes=[("lo", i % 3)])

                def s3(r):
                    i = r["i"]; zi = i % 4
                    if not r["first"]:
                        P.op("act", lambda e: e.activation(out=ww[i % 3][:], in_=banks[CARRY][0:64, :], func=AF.Exp, scale=-1.0),
                             reads=[("bank", CARRY)], writes=[("ww", i % 3)])
                    P.op("pe", lambda e: e.matmul(banks[zi][:], ntri_sb[:], hi[i % 3][:], start=False, stop=False),
                         reads=["ntri_sb", ("hi", i % 3)], writes=[("bank", zi)])
                    P.op("pe", lambda e: e.matmul(banks[zi][:], ntri_sb[:], lo[i % 3][:], start=False, stop=True),
                         reads=["ntri_sb", ("lo", i % 3)], writes=[("bank", zi)])
                    if not r["last"]:
                        P.op("pe", lambda e: e.matmul(banks[CARRY][0:64, :], ones_b[:, 0:64], hi[i % 3][:], start=r["first"], stop=False),
                             reads=["ones_b", ("hi", i % 3)], writes=[("bank", CARRY)])
                        P.op("pe", lambda e: e.matmul(banks[CARRY][0:64, :], ones_b[:, 0:64], lo[i % 3][:], start=False, stop=True),
                             reads=["ones_b", ("lo", i % 3)], writes=[("bank", CARRY)])

                def s4(r):
                    i = r["i"]; zi = i % 4; tt = r["tt"]; kc = r["kc"]
                    pb = 5 + i % 2
                    P.op("act", lambda e: e.activation(out=aa[i % 2][:], in_=banks[zi][:], func=AF.Exp),
                         reads=[("bank", zi)], writes=[("aa", i % 2)])
                    P.op("pe", lambda e: e.matmul(banks[pb][0:64, :], vs[:, kc, :], aa[i % 2][:], start=True, stop=True),
                         reads=["vs", ("aa", i % 2)], writes=[("bank", pb)])
                    if r["first"]:
                        P.op("dve", lambda e: e.tensor_copy(oacc[:], banks[pb][0:64, :]), reads=[("bank", pb)], writes=["oacc"])
                    else:
                        P.op("dve", lambda e: e.tensor_tensor(out=tmpc[:], in0=banks[pb][0:64, :], in1=ww[i % 3][:], op=ALU.mult),
                             reads=[("bank", pb), ("ww", i % 3)], writes=["tmpc"])
                        P.op("pool", lambda e: e.tensor_tensor(out=oacc[:], in0=oacc[:], in1=tmpc[:], op=ALU.add),
                             reads=["oacc", "tmpc"], writes=["oacc"])
                    if r["last"]:
                        ts_ = slice(tt * TT, (tt + 1) * TT)
                        r0 = FM_CZ * 128 + hl * 64
                        P.dma("sp", zt_c[:], fmv[r0:r0 + 64, ts_], writes=["zt_c"])
                        P.op("act", lambda e: e.activation(out=zs_c[:], in_=zt_c[:], func=AF.Silu), reads=["zt_c"], writes=["zs_c"])
                        P.op("dve", lambda e: e.tensor_tensor(out=yo_c[:], in0=oacc[:], in1=zs_c[:], op=ALU.mult),
                             reads=["oacc", "zs_c"], writes=["yo_c"])
                        P.dma("pool", yzd[256 + hl * 64:256 + hl * 64 + 64, ts_], yo_c[:], reads=["yo_c"], writes=[("yzd", 2, hl, tt)])

                n = len(recs)
                for step in range(n + 3):
                    for lag, fn in ((0, s1), (1, s2), (2, s3), (3, s4)):
                        j = step - lag
                        if 0 <= j < n:
                            fn(recs[j])
            P.barrier()

        if "D" in mixers:
          with ExitStack() as c:
            qd = sb(c, "qd", [64, S], BF16)
            kd = sb(c, "kd", [64, S], BF16)
            vd = sb(c, "vd", [128, NB, 64], BF16)
            kt = sb(c, "kt", [128, NB, 64], BF16)
            vz = sb(c, "vz", [128, NB, 64], BF16)
            dt_sb = sb(c, "dt_sb", [128, 2, 128], F32)
            ze_sb = sb(c, "ze_sb", [128, 2], F32)
            xi_sb = sb(c, "xi_sb", [64, 2, 128], F32)
            gc_sb = sb(c, "gc_sb", [64, 2], F32)
            P.dma("sp", dt_sb[:], dtab[:, :, :], writes=["dt_sb"])
            P.dma("sp", ze_sb[:], zeta[:, :], writes=["ze_sb"])
            P.dma("sp", xi_sb[:], xi_bc[:, :, :], writes=["xi_sb"])
            P.dma("sp", gc_sb[:], gchunk[:, :], writes=["gc_sb"])
            uall = sb(c, "uall", [64, NB, 64], F32)
            rall = sb(c, "rall", [64, NB, 64], F32)
            rbf = sb(c, "rbf", [64, NB, 64], BF16)
            sm = [sb(c, "sm%d" % i, [128, TT], BF16) for i in range(2)]
            od = sb(c, "od", [64, TT], F32)
            od2 = sb(c, "od2", [64, TT], F32)
            mean = sb(c, "mean", [64, TT], F32)
            var = sb(c, "var", [64, TT], F32)
            zt_d = sb(c, "zt_d", [64, TT], BF16)
            zs_d = sb(c, "zs_d", [64, TT], F32)
            yo_d = sb(c, "yo_d", [64, TT], BF16)
            for hl in range(2):
                P.dma("sp", qd[:], fmv[FM_DQ * 128 + hl * 64:FM_DQ * 128 + hl * 64 + 64, :], writes=["qd"])
                P.dma("sp", kd[:], fmv[FM_DK * 128 + hl * 64:FM_DK * 128 + hl * 64 + 64, :], writes=["kd"])
                P.dma("sp", vd[:], tmv[:, :, TM_DV0 + 64 * hl:TM_DV0 + 64 * hl + 64], writes=["vd"])
                P.dma("sp", kt[:], tmv[:, :, TM_DK0 + 64 * hl:TM_DK0 + 64 * hl + 64], writes=["kt"])
                P.op("dve", lambda e: e.tensor_scalar(out=vz[:], in0=vd[:], scalar1=ze_sb[:, hl:hl + 1], scalar2=None, op0=ALU.mult),
                     reads=["vd", "ze_sb"], writes=["vz"])
                for g in range(NB // 8):
                    bi = g % 2
                    for j in range(8):
                        n_ = g * 8 + j
                        P.op("pe", lambda e: e.matmul(banks[bi][0:64, j * 64:(j + 1) * 64], kt[:, n_, :], vz[:, n_, :],
                                                      start=(j == 0), stop=(j == 7)),
                             reads=["kt", "vz"], writes=[("bank", bi)])
                    P.op("act", lambda e: e.activation(out=uall[:, g * 8:(g + 1) * 8, :],
                                                       in_=banks[bi][0:64, :].rearrange("p (n e) -> p n e", e=64), func=AF.Copy),
                         reads=[("bank", bi)], writes=["uall"])
                P.op("dve", lambda e: e.memset(rall[:, 0, :], 0.0), writes=["rall"])
                for n_ in range(1, NB):
                    P.op("dve", lambda e: e.scalar_tensor_tensor(out=rall[:, n_, :], in0=rall[:, n_ - 1, :], scalar=gc_sb[:, hl:hl + 1],
                                                                 in1=uall[:, n_ - 1, :], op0=ALU.mult, op1=ALU.add),
                         reads=["rall", "uall", "gc_sb"], writes=["rall"])
                P.op("dve", lambda e: e.tensor_copy(rbf[:], rall[:]), reads=["rall"], writes=["rbf"])
                for tt in range(NT):
                    ts_ = slice(tt * TT, (tt + 1) * TT)
                    si = tt % 2
                    zi = tt % 2
                    for sub in range(4):
                        n_ = tt * 4 + sub
                        cs_ = slice(n_ * 128, (n_ + 1) * 128)
                        P.op("pe", lambda e: e.matmul(banks[zi][:, sub * 128:(sub + 1) * 128], kd[:, cs_], qd[:, cs_],
                                                      start=(sub == 0), stop=(sub == 3)),
                             reads=["kd", "qd"], writes=[("bank", zi)])
                    P.op("dve", lambda e: e.tensor_tensor(out=sm[si][:].rearrange("p (n i) -> p n i", i=128),
                                                          in0=banks[zi][:].rearrange("p (n i) -> p n i", i=128),
                                                          in1=_bc(dt_sb[:, hl, :], [128, 4, 128], 1), op=ALU.mult),
                         reads=[("bank", zi), "dt_sb"], writes=[("sm", si)])
                    for sub in range(4):
                        n_ = tt * 4 + sub
                        cs_ = slice(n_ * 128, (n_ + 1) * 128)
                        P.op("pe", lambda e: e.matmul(banks[2 + zi][0:64, sub * 128:(sub + 1) * 128], vd[:, n_, :], sm[si][:, sub * 128:(sub + 1) * 128],
                                                      start=(sub == 0), stop=(sub == 3)),
                             reads=["vd", ("sm", si)], writes=[("bank", 2 + zi)])
                    for sub in range(4):
                        n_ = tt * 4 + sub
                        cs_ = slice(n_ * 128, (n_ + 1) * 128)
                        P.op("pe", lambda e: e.matmul(banks[4 + zi][0:64, sub * 128:(sub + 1) * 128], rbf[:, n_, :], qd[:, cs_],
                                                      start=(sub == 0), stop=(sub == 3)),
                             reads=["rbf", "qd"], writes=[("bank", 4 + zi)])
                    P.op("dve", lambda e: e.tensor_tensor(out=od[:].rearrange("p (n i) -> p n i", i=128),
                                                          in0=banks[4 + zi][0:64, :].rearrange("p (n i) -> p n i", i=128),
                                                          in1=_bc(xi_sb[:, hl, :], [64, 4, 128], 1), op=ALU.mult),
                         reads=[("bank", 4 + zi), "xi_sb"], writes=["od"])
                    P.op("dve", lambda e: e.tensor_tensor(out=od[:], in0=od[:], in1=banks[2 + zi][0:64, :], op=ALU.add),
                         reads=["od", ("bank", 2 + zi)], writes=["od"])
                    P.op("act", lambda e: e.activation(out=od2[:], in_=od[:], func=AF.Square), reads=["od"], writes=["od2"])
                    P.op("pe", lambda e: e.matmul(banks[6][0:64, :], ones_f[0:64, 0:64], od[:], start=True, stop=True),
                         reads=["ones_f", "od"], writes=[("bank", 6)])
                    P.op("pe", lambda e: e.matmul(banks[7][0:64, :], ones_f[0:64, 0:64], od2[:], start=True, stop=True),
                         reads=["ones_f", "od2"], writes=[("bank", 7)])
                    P.op("act", lambda e: e.activation(out=mean[:], in_=banks[6][0:64, :], func=AF.Copy, scale=1.0 / 64),
                         reads=[("bank", 6)], writes=["mean"])
                    P.op("dve", lambda e: e.tensor_tensor(out=var[:], in0=mean[:], in1=mean[:], op=ALU.mult),
                         reads=["mean"], writes=["var"])
                    P.op("dve", lambda e: e.scalar_tensor_tensor(out=var[:], in0=banks[7][0:64, :], scalar=1.0 / 64, in1=var[:],
                                                                 op0=ALU.mult, op1=ALU.subtract),
                         reads=[("bank", 7), "var"], writes=["var"])
                    P.op("act", lambda e: e.activation(out=var[:], in_=var[:], func=AF.Sqrt, bias=LN_EPS), reads=["var"], writes=["var"])
                    P.op("dve", lambda e: e.reciprocal(out=var[:], in_=var[:]), reads=["var"], writes=["var"])
                    P.op("dve", lambda e: e.tensor_tensor(out=od[:], in0=od[:], in1=mean[:], op=ALU.subtract),
                         reads=["od", "mean"], writes=["od"])
                    P.op("dve", lambda e: e.tensor_tensor(out=od[:], in0=od[:], in1=var[:], op=ALU.mult),
                         reads=["od", "var"], writes=["od"])
                    r0 = FM_DZ * 128 + hl * 64
                    P.dma("sp", zt_d[:], fmv[r0:r0 + 64, ts_], writes=["zt_d"])
                    P.op("act", lambda e: e.activation(out=zs_d[:], in_=zt_d[:], func=AF.Silu), reads=["zt_d"], writes=["zs_d"])
                    P.op("dve", lambda e: e.tensor_tensor(out=yo_d[:], in0=od[:], in1=zs_d[:], op=ALU.mult),
                         reads=["od", "zs_d"], writes=["yo_d"])
                    P.dma("pool", yzd[384 + hl * 64:384 + hl * 64 + 64, ts_], yo_d[:], reads=["yo_d"], writes=[("yzd", 3, hl, tt)])
            P.barrier()
        stage2.close()
        if stop_after <= 2:
            P.finish()
            return nc

        with ExitStack() as c:
            wm = sb(c, "wm", [128, 4, 8, D], BF16)
            wo = sb(c, "wo", [128, 8, D], BF16)
            wbr = sb(c, "wbr", [128, 4, D], BF16)
            stg = [sb(c, "stg3_%d" % i, [128, D], F32) for i in range(2)]
            k_ = 0
            for i in range(4):
                for kc in range(8):
                    load_cast(c, stg, wm[:, i, kc, :], w_merge[i, kc * 128:(kc + 1) * 128, :], D, ("wm", i, kc), k_)
                    k_ += 1
                load_cast(c, stg, wbr[:, i, :], w_br[i, :, :], D, ("wbr", i), k_)
                k_ += 1
            for kc in range(8):
                load_cast(c, stg, wo[:, kc, :], w_out[kc * 128:(kc + 1) * 128, :], D, ("wo", kc), k_)
                k_ += 1
            hb3 = [sb(c, "hb3_%d" % i, [128, 8, TT], BF16) for i in range(2)]
            yz3 = [sb(c, "yz3_%d" % i, [128, 4, TT], BF16) for i in range(2)]
            sg = [sb(c, "sg%d" % i, [128, TT], F32) for i in range(2)]
            term = [sb(c, "term%d" % i, [128, TT], F32) for i in range(2)]
            mrg = sb(c, "mrg", [128, 8, TT], F32)
            mg = sb(c, "mg", [128, 8, TT], BF16)
            po = [sb(c, "po%d" % i, [128, TT], F32) for i in range(2)]
            hview = hT.rearrange("(k p) t -> p k t", p=128)
            yview = yzd.rearrange("(m p) t -> p m t", p=128)
            pview = partT.rearrange("(k p) t -> p k t", p=128)
            nb_ = 0
            ns_ = 0
            for tt in range(NT):
                i2 = tt % 2
                ts_ = slice(tt * TT, (tt + 1) * TT)
                P.dma("sp", hb3[i2][:], hview[:, :, ts_], writes=[("hb3", i2)])
                P.dma("sp", yz3[i2][:], yview[:, :, ts_], writes=[("yz3", i2)])
                for oc in range(8):
                    for i in range(4):
                        bg = nb_ % 6
                        nb_ += 1
                        for kc in range(8):
                            P.op("pe", lambda e: e.matmul(banks[bg][:], wm[:, i, kc, oc * 128:(oc + 1) * 128], hb3[i2][:, kc, :],
                                                          start=(kc == 0), stop=(kc == 7)),
                                 reads=[("wm", i, kc), ("hb3", i2)], writes=[("bank", bg)])
                        bb = 6 + ns_ % 2
                        P.op("pe", lambda e: e.matmul(banks[bb][:], wbr[:, i, oc * 128:(oc + 1) * 128], yz3[i2][:, i, :], start=True, stop=True),
                             reads=[("wbr", i), ("yz3", i2)], writes=[("bank", bb)])
                        si = ns_ % 2
                        ns_ += 1
                        P.op("act", lambda e: e.activation(out=sg[si][:], in_=banks[bg][:], func=AF.Sigmoid),
                             reads=[("bank", bg)], writes=[("sg", si)])
                        if i == 0:
                            P.op("dve", lambda e: e.tensor_tensor(out=mrg[:, oc, :], in0=banks[bb][:], in1=sg[si][:], op=ALU.mult),
                                 reads=[("bank", bb), ("sg", si)], writes=[("mrg", oc)])
                        else:
                            P.op("dve", lambda e: e.tensor_tensor(out=term[si][:], in0=banks[bb][:], in1=sg[si][:], op=ALU.mult),
                                 reads=[("bank", bb), ("sg", si)], writes=[("term", si)])
                            P.op("pool", lambda e: e.tensor_tensor(out=mrg[:, oc, :], in0=mrg[:, oc, :], in1=term[si][:], op=ALU.add),
                                 reads=[("mrg", oc), ("term", si)], writes=[("mrg", oc)])
                    P.op("pool", lambda e: e.tensor_copy(mg[:, oc, :], mrg[:, oc, :]), reads=[("mrg", oc)], writes=[("mg", oc)])
                for oc2 in range(8):
                    bg = nb_ % 6
                    nb_ += 1
                    for kc in range(8):
                        P.op("pe", lambda e: e.matmul(banks[bg][:], wo[:, kc, oc2 * 128:(oc2 + 1) * 128], mg[:, kc, :],
                                                      start=(kc == 0), stop=(kc == 7)),
                             reads=[("wo", kc), ("mg", kc)], writes=[("bank", bg)])
                    pi = oc2 % 2
                    P.op("act", lambda e: e.activation(out=po[pi][:], in_=banks[bg][:], func=AF.Copy, scale=mod[:, 16 + oc2:17 + oc2]),
                         reads=[("bank", bg), "mod"], writes=[("po", pi)])
                    P.dma("pool", pview[:, oc2, ts_], po[pi][:], reads=[("po", pi)], is_output=True)
            P.barrier()
        P.finish()
    return nc


def _bf(a):
    return np.asarray(a, dtype=np.float32).astype(ml_dtypes.bfloat16)


def make_consts():
    sl = np.arange(128)[:, None]
    tl = np.arange(TT)[None, :]
    m = np.zeros((NMASK, 128, TT), np.float32)

    def put(idx, ok):
        m[idx] = np.where(ok, 0.0, NEG)
    for r in range(4):
        put(MK_STRICT + r, sl + 128 * r < tl)
        put(MK_NONSTRICT + r, sl + 128 * r <= tl)
    for j, r in enumerate(range(-4, 0)):
        d = tl - sl - 128 * r
        put(MK_WINA + j, (d >= 0) & (d < 512))
    for j, r in enumerate(range(-1, 4)):
        d = tl - sl - 128 * r
        put(MK_WINB + j, (d >= 0) & (d < 128))
    for mm in range(8):
        put(MK_CMP + mm, tl + 512 * mm - 32 * sl - 31 >= 0)
    t = np.arange(S)
    slopes = 2.0 ** (-8.0 * (np.arange(4) + 1) / 4)
    qpos = np.zeros((4, 4, S), np.float32)
    for h in range(4):
        qpos[h, 0] = -slopes[h] * 64 * (t // 64)
        qpos[h, 1] = -slopes[h] * (t % 64)
        qpos[h, 2] = slopes[h] * 64
        qpos[h, 3] = slopes[h]
    kpos_tok = np.stack([np.ones(S), np.ones(S), t // 64, t % 64]).astype(np.float32)
    pc = 32 * np.arange(256) + 31
    kpos_cmp = np.stack([np.ones(256), np.ones(256), pc // 64, pc % 64]).astype(np.float32)
    ewide = 30000.0 * (np.arange(S)[None, :] // 64 == np.arange(128)[:, None]).astype(np.float32)
    ident = np.eye(128, dtype=np.float32)
    ntri = -(np.arange(128)[:, None] >= np.arange(128)[None, :]).astype(np.float32)
    pairsum = np.zeros((2, 128, 128), np.float32)
    for cc in range(2):
        pairsum[cc, np.arange(128), 64 * cc + np.arange(128) // 2] = 1.0
    u = np.arange(256)[None, :] - 128
    cur = (np.arange(128)[:, None] >= 64).astype(np.int64)
    forced = (u == cur) | (u == cur - 1)
    future = u > cur
    selkeep = (~(forced | future)).astype(np.float32)
    seladd = np.where(forced, 1e4, np.where(future, -1.0, 0.0)).astype(np.float32)
    return dict(masks=_bf(m), qpos_all=qpos, kpos_tok=_bf(kpos_tok), kpos_cmp=_bf(kpos_cmp), ewide=_bf(ewide),
                ident=_bf(ident), ntri=_bf(ntri), pairsum=_bf(pairsum), selkeep=selkeep, seladd=seladd)


def ret_consts(hh):
    out = {}
    i = np.arange(128)
    dt_ = np.zeros((128, 2, 128), np.float32)
    zt = np.zeros((128, 2), np.float32)
    xi = np.zeros((64, 2, 128), np.float32)
    gc = np.zeros((64, 2), np.float32)
    for hl in range(2):
        h = 2 * hh + hl
        log_g = np.log(np.float32(1.0) - np.float32(2.0 ** (-5.0 - h)))
        diff = (i[None, :] - i[:, None]).astype(np.float32)
        dt_[:, hl, :] = np.where(diff >= 0, np.exp(log_g * np.maximum(diff, 0.0)), 0.0)
        zt[:, hl] = np.exp(log_g * (127 - i))
        xi[:, hl, :] = np.exp(log_g * (i + 1))[None, :]
        gc[:, hl] = np.exp(log_g * 128)
    return dict(dtab=dt_, zeta=zt, xi_bc=xi, gchunk=gc)


def arrange_w_in(w, hh):
    o = np.cumsum([0, 256, 384, 12, 256, 256, 128, 128, 256, 256, 256, 256, 256, 256, 256, 256, 256])
    a_q, a_kv, a_g, a_z, b_q, b_k, b_v, b_z, c_q, c_k, c_v, c_z, d_q, d_k, d_v, d_z = [
        w[:, o[i]:o[i + 1]] for i in range(16)]
    own = slice(128 * hh, 128 * hh + 128)
    kvh = slice(64 * hh, 64 * hh + 64)
    k_cmp, v_cmp, k_sel, v_sel, k_win, v_win = [a_kv[:, 64 * i:64 * i + 64] for i in range(6)]
    gcols = []
    for j in range(3):
        g = np.concatenate([np.repeat(a_g[:, 3 * (2 * hh + hl) + j][:, None], 64, axis=1) for hl in range(2)], axis=1)
        gcols.append(g)
    zpad = np.zeros((D, 64), np.float32)
    oth = slice(128 * (1 - hh), 128 * (1 - hh) + 128)
    fmt = [a_q[:, own], a_q[:, oth], np.concatenate([k_cmp, v_cmp], 1), np.concatenate([k_sel, k_win], 1),
           gcols[0], gcols[1], gcols[2], a_z[:, own], b_q[:, own], np.concatenate([b_k[:, kvh], zpad], 1), b_z[:, own],
           c_q[:, own], c_k[:, own], c_z[:, own], d_q[:, own], d_k[:, own], d_z[:, own]]
    tmc = [v_sel, v_win, b_v[:, kvh], c_v[:, own], d_v[:, own], d_k[:, own]]
    return np.ascontiguousarray(np.concatenate(fmt + tmc, axis=1), dtype=np.float32)


def layer_inputs(l, b, hh, c, w_ada, b_ada, norm_g, w_in, cmp_pos, cmp_w1, cmp_w2, sink, w_merge, w_br, w_out, consts):
    d = dict(consts)
    qp = d.pop("qpos_all")
    order = [2 * hh, 2 * hh + 1, 2 * (1 - hh), 2 * (1 - hh) + 1]
    d["qpos"] = _bf(qp[order])
    d.update(ret_consts(hh))
    d["cT"] = np.ascontiguousarray(c[b].reshape(8, 128).T)
    d["w_ada"] = np.ascontiguousarray(w_ada[l])
    d["b_adaT"] = np.ascontiguousarray(b_ada[l].reshape(24, 128).T)
    d["gT"] = np.ascontiguousarray(norm_g[l].reshape(8, 128).T)
    d["w_in"] = arrange_w_in(w_in[l], hh)
    d["cmp_posT"] = np.ascontiguousarray(cmp_pos[l].transpose(2, 0, 1))
    d["cmp_w1"] = np.ascontiguousarray(cmp_w1[l].reshape(2, 32, 64, 64).transpose(2, 0, 1, 3))
    d["cmp_w2"] = np.ascontiguousarray(cmp_w2[l].transpose(1, 0, 2))
    d["sinkb"] = np.ascontiguousarray(np.repeat(sink[l][2 * hh:2 * hh + 2], 64)[:, None].astype(np.float32))
    d["w_merge"] = np.ascontiguousarray(w_merge[l])
    d["w_br"] = np.ascontiguousarray(w_br[l][:, 128 * hh:128 * hh + 128, :])
    d["w_out"] = np.ascontiguousarray(w_out[l])
    return d


_PROG_CACHE = {}


def _prog(first, last_only):
    key = (first, last_only)
    if key not in _PROG_CACHE:
        _PROG_CACHE[key] = build_program(first, last_only)
    return _PROG_CACHE[key]


def kernel(x, c, w_ada, b_ada, norm_g, w_in, cmp_pos, cmp_w1, cmp_w2, sink, w_merge, w_br, w_out, final_g):
    args = [np.asarray(a, dtype=np.float32) for a in
            (x, c, w_ada, b_ada, norm_g, w_in, cmp_pos, cmp_w1, cmp_w2, sink, w_merge, w_br, w_out, final_g)]
    x, c, w_ada, b_ada, norm_g, w_in, cmp_pos, cmp_w1, cmp_w2, sink, w_merge, w_br, w_out, final_g = args
    B = x.shape[0]
    depth = w_ada.shape[0]
    consts = make_consts()
    cores = [(b, hh) for b in range(B) for hh in range(2)]
    xT = [np.ascontiguousarray(x[b].T) for b in range(B)]
    zero = np.zeros((D, S), np.float32)
    parts = [zero] * (2 * B)
    for l in range(depth):
        in_maps = []
        for (b, hh) in cores:
            d = layer_inputs(l, b, hh, c, w_ada, b_ada, norm_g, w_in, cmp_pos, cmp_w1, cmp_w2, sink, w_merge, w_br, w_out, consts)
            d["xT"] = xT[b]
            d["paT"] = parts[2 * b]
            d["pbT"] = parts[2 * b + 1]
            in_maps.append(d)
        res = run_bass_kernel_spmd(_prog(False, False), in_maps, core_ids=list(range(len(cores))))
        xT = [np.asarray(res.results[2 * b]["xcT"]) for b in range(B)]
        parts = [np.asarray(r["partT"]) for r in res.results]
    in_maps = []
    fgT = np.ascontiguousarray(final_g.reshape(8, 128).T)
    for (b, hh) in cores:
        in_maps.append({"xT": xT[b], "paT": parts[2 * b], "pbT": parts[2 * b + 1], "fgT": fgT})
    res = run_bass_kernel_spmd(_prog(False, True), in_maps, core_ids=list(range(len(cores))))
    out = np.stack([np.asarray(res.results[2 * b]["outT"]).T for b in range(B)]).astype(np.float32)
    return np.ascontiguousarray(out)
```

```python
from contextlib import ExitStack
import numpy as np
import ml_dtypes
import concourse.bass as bass
import concourse.mybir as mybir
from concourse.bass_utils import run_bass_kernel_spmd

F32 = mybir.dt.float32
BF16 = mybir.dt.bfloat16
AF = mybir.ActivationFunctionType
ALU = mybir.AluOpType

D = 1024
S = 8192
NT = 16
TT = 512
NB = 64
NEG = -30000.0
RMS_EPS = 1e-6
LN_EPS = 1e-5

FM_AQ01, FM_AQ23, FM_KVC, FM_KSW, FM_G0, FM_G1, FM_G2, FM_AZ, FM_BQ, FM_BK, FM_BZ, \
    FM_CQ, FM_CK, FM_CZ, FM_DQ, FM_DK, FM_DZ = range(17)
NFM = 17
Q_TILES = (FM_AQ01, FM_AQ23, FM_BQ, FM_CQ, FM_DQ)
TM_VSEL, TM_VWIN, TM_BV, TM_CV0, TM_CV1, TM_DV0, TM_DV1, TM_DK0, TM_DK1 = [64 * i for i in range(9)]
NTM = 576
NCOL = NFM * 128 + NTM

MK_STRICT = 0
MK_NONSTRICT = 4
MK_WINA = 8
MK_WINB = 12
MK_CMP = 17
NMASK = 25


class Prog:
    def __init__(self, nc, ctx, n_dma_sems=24):
        self.nc = nc
        self.eng = {"pe": nc.tensor, "act": nc.scalar, "dve": nc.vector, "pool": nc.gpsimd, "sp": nc.sync}
        self.sems = []
        self.semid = {}
        for e in self.eng:
            self.semid[e] = len(self.sems)
            self.sems.append(ctx.enter_context(nc.semaphore("p_" + e)))
        self.cnt = {e: 0 for e in self.eng}
        self.dsem = []
        for i in range(n_dma_sems):
            self.dsem.append(len(self.sems))
            self.sems.append(ctx.enter_context(nc.semaphore("d%d" % i)))
        self.duse = [0] * n_dma_sems
        n_sw = n_dma_sems // 3
        self.dpool = {"pool": list(range(0, n_sw)), "sp": list(range(n_sw, n_dma_sems))}
        self.dpos = {"pool": 0, "sp": 0}
        self.known = {e: {} for e in self.eng}
        self.res = {}
        self.out_tokens = []
        self.ninstr = 0

    def _st(self, k):
        st = self.res.get(k)
        if st is None:
            st = [None, {}]
            self.res[k] = st
        return st

    def _deps(self, reads, writes):
        need = {}

        def add(tok):
            s, v = tok
            if need.get(s, 0) < v:
                need[s] = v
        for k in reads:
            st = self._st(k)
            if st[0] is not None:
                add(st[0])
        for k in writes:
            st = self._st(k)
            if st[0] is not None:
                add(st[0])
            for s, v in st[1].items():
                add((s, v))
        return need

    def _wait(self, e, need):
        kn = self.known[e]
        for s, v in need.items():
            if e == "pe" and s == self.semid["pe"]:
                continue
            if kn.get(s, 0) >= v:
                continue
            self.eng[e].wait_ge(self.sems[s], v)
            kn[s] = v
            self.ninstr += 1

    def _record(self, tok, reads, writes):
        for k in reads:
            st = self._st(k)
            if st[1].get(tok[0], 0) < tok[1]:
                st[1][tok[0]] = tok[1]
        for k in writes:
            st = self._st(k)
            st[0] = tok
            st[1] = {}

    def op(self, e, fn, reads=(), writes=()):
        self._wait(e, self._deps(reads, writes))
        ins = fn(self.eng[e])
        self.cnt[e] += 1
        tok = (self.semid[e], self.cnt[e])
        ins.then_inc(self.sems[tok[0]], 1)
        self._record(tok, reads, writes)
        self.ninstr += 1
        return tok

    def dma(self, q, out, in_, reads=(), writes=(), is_output=False):
        pl = self.dpool[q]
        k = pl[self.dpos[q] % len(pl)]
        self.dpos[q] += 1
        need = self._deps(reads, writes)
        if self.duse[k] > 0:
            s = self.dsem[k]
            if need.get(s, 0) < 16 * self.duse[k]:
                need[s] = 16 * self.duse[k]
        self._wait(q, need)
        ins = self.eng[q].dma_start(out=out, in_=in_)
        self.duse[k] += 1
        tok = (self.dsem[k], 16 * self.duse[k])
        ins.then_inc(self.sems[tok[0]], 16)
        self._record(tok, reads, writes)
        if is_output:
            self.out_tokens.append(tok)
        self.ninstr += 1
        return tok

    def barrier(self):
        need = {}
        for e in self.eng:
            if self.cnt[e] > 0:
                need[self.semid[e]] = self.cnt[e]
        for k, s in enumerate(self.dsem):
            if self.duse[k] > 0:
                need[s] = 16 * self.duse[k]
        for e in self.eng:
            n2 = {s: v for s, v in need.items() if s != self.semid[e] or e != "pe"}
            self._wait(e, n2)
        self.res = {}

    def finish(self):
        need = {}
        for s, v in self.out_tokens:
            if need.get(s, 0) < v:
                need[s] = v
        self._wait("sp", need)


def _bc(ap, shape, axis):
    return ap.unsqueeze(axis).broadcast_to(shape)


def build_program(first, last_only, dbg=None, stop_after=99, mixers="ABCD"):
    dbg = dbg or set()
    nc = bass.Bass("TRN2", target_bir_lowering=False)

    def din(name, shape, dt=F32):
        return nc.dram_tensor(name, list(shape), dt, kind="ExternalInput").ap()

    def dout(name, shape, dt=F32):
        return nc.dram_tensor(name, list(shape), dt, kind="ExternalOutput").ap()

    def dscr(name, shape, dt):
        kind = "ExternalOutput" if name in dbg else "Internal"
        return nc.dram_tensor(name, list(shape), dt, kind=kind).ap()

    xT = din("xT", [D, S])
    if not first:
        paT = din("paT", [D, S])
        pbT = din("pbT", [D, S])
    if last_only:
        fgT = din("fgT", [128, 8])
        outT = dout("outT", [D, S])
    else:
        cT = din("cT", [128, 8])
        w_ada = din("w_ada", [D, 3 * D])
        b_adaT = din("b_adaT", [128, 24])
        gT = din("gT", [128, 8])
        w_in = din("w_in", [D, NCOL])
        cmp_posT = din("cmp_posT", [64, 2, 32])
        cmp_w1 = din("cmp_w1", [64, 2, 32, 64])
        cmp_w2 = din("cmp_w2", [64, 2, 64])
        sinkb = din("sinkb", [128, 1])
        w_merge = din("w_merge", [4, D, D])
        w_br = din("w_br", [4, 128, D])
        w_out = din("w_out", [D, D])
        masks = din("masks", [NMASK, 128, TT], BF16)
        qpos = din("qpos", [4, 4, S], BF16)
        kpos_tok = din("kpos_tok", [4, S], BF16)
        kpos_cmp = din("kpos_cmp", [4, 256], BF16)
        ewide = din("ewide", [128, S], BF16)
        ident = din("ident", [128, 128], BF16)
        ntri = din("ntri", [128, 128], BF16)
        pairsum = din("pairsum", [2, 128, 128], BF16)
        selkeep = din("selkeep", [128, 256])
        seladd = din("seladd", [128, 256])
        dtab = din("dtab", [128, 2, 128])
        zeta = din("zeta", [128, 2])
        xi_bc = din("xi_bc", [64, 2, 128])
        gchunk = din("gchunk", [64, 2])
        partT = dout("partT", [D, S])
        if not first:
            xcT = dout("xcT", [D, S])
        hT = dscr("hT", [D, S], BF16)
        fm = dscr("fm", [NFM * 128, S], BF16)
        tm = dscr("tm", [S, NTM], BF16)
        yzd = dscr("yzd", [512, S], BF16)

    ctx = ExitStack()
    with ctx:
        P = Prog(nc, ctx)
        banks = [ctx.enter_context(nc.psum_tensor("bank%d" % i, [128, 512], F32)) for i in range(8)]

        def sb(c, name, shape, dt):
            return c.enter_context(nc.sbuf_tensor(name, list(shape), dt))

        ones_f = sb(ctx, "ones_f", [128, 128], F32)
        P.op("dve", lambda e: e.memset(ones_f[:], 1.0), writes=["ones_f"])
        xview = xT.rearrange("(k p) t -> p k t", p=128)

        if last_only:
            with ExitStack() as c:
                fg = sb(c, "fg", [128, 8], F32)
                P.dma("sp", fg[:], fgT[:, :], writes=["fg"])
                pav = paT.rearrange("(k p) t -> p k t", p=128)
                pbv = pbT.rearrange("(k p) t -> p k t", p=128)
                ov = outT.rearrange("(k p) t -> p k t", p=128)
                xt = [sb(c, "xt%d" % i, [128, 8, TT], F32) for i in range(2)]
                pt = [sb(c, "pt%d" % i, [128, 8, TT], F32) for i in range(2)]
                qt = [sb(c, "qt%d" % i, [128, 8, TT], F32) for i in range(2)]
                sq = sb(c, "sq", [128, 8, TT], F32)
                rt = sb(c, "rt", [128, TT], F32)
                rstd = sb(c, "rstd", [128, TT], F32)
                for tt in range(NT):
                    i = tt % 2
                    ts_ = slice(tt * TT, (tt + 1) * TT)
                    P.dma("sp", xt[i][:], xview[:, :, ts_], writes=[("xt", i)])
                    P.dma("sp", pt[i][:], pav[:, :, ts_], writes=[("pt", i)])
                    P.dma("sp", qt[i][:], pbv[:, :, ts_], writes=[("qt", i)])
                    P.op("dve", lambda e: e.tensor_tensor(out=xt[i][:], in0=xt[i][:], in1=pt[i][:], op=ALU.add),
                         reads=[("xt", i), ("pt", i)], writes=[("xt", i)])
                    P.op("dve", lambda e: e.tensor_tensor(out=xt[i][:], in0=xt[i][:], in1=qt[i][:], op=ALU.add),
                         reads=[("xt", i), ("qt", i)], writes=[("xt", i)])
                    P.op("act", lambda e: e.activation(out=sq[:], in_=xt[i][:], func=AF.Square),
                         reads=[("xt", i)], writes=["sq"])
                    bk = banks[tt % 2]
                    for kc in range(8):
                        P.op("pe", lambda e: e.matmul(bk[:], ones_f[:], sq[:, kc, :], start=(kc == 0), stop=(kc == 7)),
                             reads=["sq", "ones_f"], writes=[("bank", tt % 2)])
                    P.op("act", lambda e: e.activation(out=rt[:], in_=bk[:], func=AF.Sqrt, scale=1.0 / D, bias=RMS_EPS),
                         reads=[("bank", tt % 2)], writes=["rt"])
                    P.op("dve", lambda e: e.reciprocal(out=rstd[:], in_=rt[:]), reads=["rt"], writes=["rstd"])
                    P.op("dve", lambda e: e.tensor_tensor(out=xt[i][:], in0=xt[i][:],
                                                          in1=_bc(rstd[:], [128, 8, TT], 1), op=ALU.mult),
                         reads=[("xt", i), "rstd"], writes=[("xt", i)])
                    for kc in range(8):
                        P.op("act", lambda e: e.activation(out=xt[i][:, kc, :], in_=xt[i][:, kc, :], func=AF.Copy,
                                                           scale=fg[:, kc:kc + 1]),
                             reads=[("xt", i), "fg"], writes=[("xt", i)])
                    P.dma("pool", ov[:, :, ts_], xt[i][:], reads=[("xt", i)], is_output=True)
            P.finish()
            return nc


        ident_b = sb(ctx, "ident_b", [128, 128], BF16)
        P.dma("sp", ident_b[:], ident[:, :], writes=["ident_b"])
        ones_b = sb(ctx, "ones_b", [128, 128], BF16)
        P.op("dve", lambda e: e.memset(ones_b[:], 1.0), writes=["ones_b"])
        mod = sb(ctx, "mod", [128, 24], F32)
        gs = sb(ctx, "gs", [128, 8], F32)
        with ExitStack() as c:
            cs = sb(c, "cs", [128, 8], F32)
            csil = sb(c, "csil", [128, 8, 2], F32)
            bada = sb(c, "bada", [128, 24], F32)
            gt_ = sb(c, "gt_", [128, 8], F32)
            wa = [sb(c, "wa%d" % i, [128, 3 * D], F32) for i in range(2)]
            P.dma("sp", cs[:], cT[:, :], writes=["cs"])
            P.dma("sp", bada[:], b_adaT[:, :], writes=["bada"])
            P.dma("sp", gt_[:], gT[:, :], writes=["gt_"])
            for j in range(2):
                P.op("act", lambda e: e.activation(out=csil[:, :, j], in_=cs[:], func=AF.Silu),
                     reads=["cs"], writes=["csil"])
            for kc in range(8):
                i = kc % 2
                P.dma("sp", wa[i][:], w_ada[kc * 128:(kc + 1) * 128, :], writes=[("wa", i)])
                for oc in range(24):
                    P.op("pe", lambda e: e.matmul(banks[0][:, 2 * oc:2 * oc + 2], wa[i][:, oc * 128:(oc + 1) * 128],
                                                  csil[:, kc, :], start=(kc == 0 and oc == 0), stop=(kc == 7 and oc == 23)),
                         reads=[("wa", i), "csil"], writes=[("bank", 0)])
            P.op("dve", lambda e: e.tensor_tensor(out=mod[:], in0=banks[0][:, 0:48:2], in1=bada[:], op=ALU.add),
                 reads=[("bank", 0), "bada"], writes=["mod"])
            P.op("dve", lambda e: e.scalar_tensor_tensor(out=gs[:], in0=mod[:, 8:16], scalar=1.0, in1=gt_[:],
                                                         op0=ALU.add, op1=ALU.mult),
                 reads=["mod", "gt_"], writes=["gs"])
            P.barrier()

        def load_cast(c_stage, stg, dst_ap, src_ap, n, key_dst, idx):
            i = idx % 2
            P.dma("sp", stg[i][:, 0:n], src_ap, writes=[("stg", i)])
            eng = "dve" if idx % 2 == 0 else "pool"
            P.op(eng, lambda e: e.tensor_copy(dst_ap, stg[i][:, 0:n]), reads=[("stg", i)], writes=[key_dst])

        with ExitStack() as c:
            wb = sb(c, "wb", [128, 8, NCOL], BF16)
            stg = [sb(c, "stg%d" % i, [128, NCOL], F32) for i in range(2)]
            for kc in range(8):
                load_cast(c, stg, wb[:, kc, :], w_in[kc * 128:(kc + 1) * 128, :], NCOL, ("wb", kc), kc)
            xt = [sb(c, "xt%d" % i, [128, 8, TT], F32) for i in range(2)]
            if not first:
                pt = sb(c, "pt", [128, 8, TT], F32)
                ptb = sb(c, "ptb", [128, 8, TT], F32)
                pav = paT.rearrange("(k p) t -> p k t", p=128)
                pbv = pbT.rearrange("(k p) t -> p k t", p=128)
                xcv = xcT.rearrange("(k p) t -> p k t", p=128)
            sq = sb(c, "sq", [128, 8, TT], F32)
            rt = sb(c, "rt", [128, TT], F32)
            rstd = sb(c, "rstd", [128, TT], F32)
            hb = [sb(c, "hb%d" % i, [128, 8, TT], BF16) for i in range(2)]
            ev = [sb(c, "ev%d" % i, [128, TT], BF16) for i in range(4)]
            evt = [sb(c, "evt%d" % i, [128, NTM], BF16) for i in range(2)]
            hview = hT.rearrange("(k p) t -> p k t", p=128)
            ctr = {'nbank': 0, 'nev': 0}

            def s1_norm(tt):
                i = tt % 2
                ts_ = slice(tt * TT, (tt + 1) * TT)
                P.dma("sp", xt[i][:], xview[:, :, ts_], writes=[("xt", i)])
                if not first:
                    for pv, pbuf, pkey in ((pav, pt, "pt"), (pbv, ptb, "ptb")):
                        P.dma("sp", pbuf[:], pv[:, :, ts_], writes=[pkey])
                    for pv, pbuf, pkey in ((pav, pt, "pt"), (pbv, ptb, "ptb")):
                        P.op("dve", lambda e: e.tensor_tensor(out=xt[i][:], in0=xt[i][:], in1=pbuf[:], op=ALU.add),
                             reads=[("xt", i), pkey], writes=[("xt", i)])
                    P.dma("pool", xcv[:, :, ts_], xt[i][:], reads=[("xt", i)], is_output=True)
                P.op("act", lambda e: e.activation(out=sq[:], in_=xt[i][:], func=AF.Square),
                     reads=[("xt", i)], writes=["sq"])
                bi = ctr['nbank'] % 8
                ctr['nbank'] += 1
                for kc in range(8):
                    P.op("pe", lambda e: e.matmul(banks[bi][:], ones_f[:], sq[:, kc, :], start=(kc == 0), stop=(kc == 7)),
                         reads=["sq", "ones_f"], writes=[("bank", bi)])
                P.op("act", lambda e: e.activation(out=rt[:], in_=banks[bi][:], func=AF.Sqrt, scale=1.0 / D, bias=RMS_EPS),
                     reads=[("bank", bi)], writes=["rt"])
                P.op("dve", lambda e: e.reciprocal(out=rstd[:], in_=rt[:]), reads=["rt"], writes=["rstd"])
                P.op("dve", lambda e: e.tensor_tensor(out=sq[:], in0=xt[i][:], in1=_bc(rstd[:], [128, 8, TT], 1), op=ALU.mult),
                     reads=[("xt", i), "rstd", "sq"], writes=["sq"])
                for kc in range(8):
                    P.op("act", lambda e: e.activation(out=hb[i][:, kc, :], in_=sq[:, kc, :], func=AF.Identity,
                                                       scale=gs[:, kc:kc + 1], bias=mod[:, kc:kc + 1]),
                         reads=["sq", "gs", "mod"], writes=[("hb", i)])
                P.dma("pool", hview[:, :, ts_], hb[i][:], reads=[("hb", i)], writes=[("hT", tt)])

            def s1_proj(tt):
                i = tt % 2
                ts_ = slice(tt * TT, (tt + 1) * TT)
                for ct in range(NFM):
                    bi = ctr['nbank'] % 8
                    ctr['nbank'] += 1
                    for kc in range(8):
                        P.op("pe", lambda e: e.matmul(banks[bi][:], wb[:, kc, ct * 128:(ct + 1) * 128], hb[i][:, kc, :],
                                                      start=(kc == 0), stop=(kc == 7)),
                             reads=[("wb", kc), ("hb", i)], writes=[("bank", bi)])
                    ei = ctr['nev'] % 4
                    ctr['nev'] += 1
                    if ct in Q_TILES:
                        P.op("act", lambda e: e.activation(out=ev[ei][:], in_=banks[bi][:], func=AF.Copy, scale=0.125),
                             reads=[("bank", bi)], writes=[("ev", ei)])
                    elif ct % 2 == 0:
                        P.op("act", lambda e: e.activation(out=ev[ei][:], in_=banks[bi][:], func=AF.Copy),
                             reads=[("bank", bi)], writes=[("ev", ei)])
                    else:
                        P.op("dve", lambda e: e.tensor_copy(ev[ei][:], banks[bi][:]),
                             reads=[("bank", bi)], writes=[("ev", ei)])
                    P.dma("pool", fm[ct * 128:(ct + 1) * 128, ts_], ev[ei][:], reads=[("ev", ei)], writes=[("fm", ct, tt)])

            def s1_proj_tm(tt):
                i = tt % 2
                ts_ = slice(tt * TT, (tt + 1) * TT)
                for sub in range(4):
                    ei = (tt * 4 + sub) % 2
                    for g, (c0, c1) in enumerate(((0, 320), (320, 576))):
                        bi = ctr['nbank'] % 8
                        ctr['nbank'] += 1
                        for kc in range(8):
                            P.op("pe", lambda e: e.matmul(banks[bi][:, 0:c1 - c0], hb[i][:, kc, sub * 128:(sub + 1) * 128],
                                                          wb[:, kc, NFM * 128 + c0:NFM * 128 + c1],
                                                          start=(kc == 0), stop=(kc == 7)),
                                 reads=[("wb", kc), ("hb", i)], writes=[("bank", bi)])
                        P.op("dve" if g == 0 else "act",
                             (lambda e: e.tensor_copy(evt[ei][:, c0:c1], banks[bi][:, 0:c1 - c0])) if g == 0 else
                             (lambda e: e.activation(out=evt[ei][:, c0:c1], in_=banks[bi][:, 0:c1 - c0], func=AF.Copy)),
                             reads=[("bank", bi)], writes=[("evt", ei)])
                    r0 = tt * TT + sub * 128
                    P.dma("pool", tm[r0:r0 + 128, :], evt[ei][:], reads=[("evt", ei)], writes=[("tm", tt)])

            s1_norm(0)
            for tt in range(NT):
                s1_proj(tt)
                if tt + 1 < NT:
                    s1_norm(tt + 1)
                s1_proj_tm(tt)
            P.barrier()

        if stop_after <= 1:
            P.finish()
            return nc
        hh_ = 0
        qpos_own = [qpos[0], qpos[1]]
        fmv = fm
        tmv = tm.rearrange("(n p) c -> p n c", p=128)
        stage2 = ExitStack()
        masks_sb = sb(stage2, "masks_sb", [128, NMASK, TT], BF16)
        P.dma("sp", masks_sb[:], masks.rearrange("m p t -> p m t"), writes=["masks_sb"])
        zrot = [0]
        arot = [0]
        a_sb = [sb(stage2, "a_sb%d" % i, [128, TT], BF16) for i in range(3)]
        o_sb = sb(stage2, "o_sb", [64, TT], F32)
        dn_sb = sb(stage2, "dn_sb", [64, TT], F32)
        ACC = 3

        def softmax_tile(q_ap, qkey, ka, kakey, vo, vokey, chunks, selmask=None, sink_ap=None, selkey=None, bg=None):
            n = len(chunks)
            slots = []

            def qk(idx):
                kc, mk = chunks[idx]
                zi = zrot[0] % 3
                zrot[0] += 1
                ai = arot[0] % 3
                arot[0] += 1
                slots.append((zi, ai))
                mms = [(ka[0:68, kc * 128:(kc + 1) * 128], q_ap, [kakey, qkey])]
                if selmask is not None:
                    mms.append((ewide_sb[:, kc * 128:(kc + 1) * 128], selmask[:], ["ewide_sb", selkey]))
                if mk is not None:
                    mms.append((ident_b[:], masks_sb[:, mk, :], ["ident_b", "masks_sb"]))
                for j, (l_, r_, rd) in enumerate(mms):
                    P.op("pe", lambda e: e.matmul(banks[zi][:], l_, r_, start=(j == 0), stop=(j == len(mms) - 1)),
                         reads=rd, writes=[("bank", zi)])

            def ex(idx):
                zi, ai = slots[idx]
                P.op("act", lambda e: e.activation(out=a_sb[ai][:], in_=banks[zi][:], func=AF.Exp),
                     reads=[("bank", zi)], writes=[("a_sb", ai)])

            def av(idx):
                kc, mk = chunks[idx]
                zi, ai = slots[idx]
                P.op("pe", lambda e: e.matmul(banks[ACC][:], vo[:, kc, :], a_sb[ai][:], start=(idx == 0), stop=(idx == n - 1)),
                     reads=[vokey, ("a_sb", ai)], writes=[("bank", ACC)])

            for step in range(n + 2):
                if step < n:
                    qk(step)
                if 0 <= step - 1 < n:
                    ex(step - 1)
                if 0 <= step - 2 < n:
                    av(step - 2)
                if bg is not None:
                    next(bg, None)
            if sink_ap is not None:
                P.op("dve", lambda e: e.tensor_scalar(out=dn_sb[:], in0=banks[ACC][64:128, :], scalar1=sink_ap, scalar2=1e-30,
                                                      op0=ALU.add, op1=ALU.max),
                     reads=[("bank", ACC), "esink"], writes=["dn_sb"])
            else:
                P.op("dve", lambda e: e.tensor_scalar_max(out=dn_sb[:], in0=banks[ACC][64:128, :], scalar1=1e-30),
                     reads=[("bank", ACC)], writes=["dn_sb"])
            P.op("dve", lambda e: e.reciprocal(out=dn_sb[:], in_=dn_sb[:]), reads=["dn_sb"], writes=["dn_sb"])
            P.op("dve", lambda e: e.tensor_tensor(out=o_sb[:], in0=banks[ACC][0:64, :], in1=dn_sb[:], op=ALU.mult),
                 reads=[("bank", ACC), "dn_sb"], writes=["o_sb"])

        def build_vo(c, name, col):
            vo = sb(c, name, [128, NB, 128], BF16)
            P.dma("sp", vo[:, :, 0:64], tmv[:, :, col:col + 64], writes=[name])
            P.op("pool", lambda e: e.memset(vo[:, :, 64:128], 1.0), writes=[name])
            return vo

        def build_ka(c, name, ct, row0, kp):
            ka = sb(c, name, [68, S], BF16)
            P.dma("sp", ka[0:64, :], fmv[ct * 128 + row0:ct * 128 + row0 + 64, :], writes=[name])
            P.dma("sp", ka[64:68, :], kp, writes=[name])
            return ka

        if "B" in mixers or "A" in mixers:
          with ExitStack() as c:
            qa = [sb(c, "qa%d" % h, [68, TT], BF16) for h in range(4)]
            gl = [sb(c, "gl%d" % i, [64, TT], F32) for i in range(6)]
            zt_sb = sb(c, "zt_sb", [128, TT], BF16)
            zs_sb = sb(c, "zs_sb", [128, TT], F32)
            ya = sb(c, "ya", [128, TT], F32)
            yo = sb(c, "yo", [128, TT], BF16)
            tmp64 = sb(c, "tmp64", [64, TT], F32)
            if "B" in mixers:
                cB = ExitStack()
                kb = build_ka(cB, "kb", FM_BK, 0, kpos_tok[:, :])
                vb = build_vo(cB, "vb", TM_BV)
                esink = sb(cB, "esink", [128, 1], F32)
                P.dma("sp", esink[:], sinkb[:, :], writes=["esink"])
                P.op("act", lambda e: e.activation(out=esink[:], in_=esink[:], func=AF.Exp), reads=["esink"], writes=["esink"])
                es2 = sb(cB, "es2", [64, 2], F32)
                P.op("dve", lambda e: e.tensor_copy(es2[:, 0:1], esink[0:64, :]), reads=["esink"], writes=["esink2"])
                P.op("dve", lambda e: e.tensor_copy(es2[:, 1:2], esink[64:128, :]), reads=["esink"], writes=["esink2"])
                for tt in range(NT):
                    ts_ = slice(tt * TT, (tt + 1) * TT)
                    P.dma("sp", zt_sb[:], fmv[FM_BZ * 128:(FM_BZ + 1) * 128, ts_], writes=["zt_sb"])
                    P.op("act", lambda e: e.activation(out=zs_sb[:], in_=zt_sb[:], func=AF.Silu), reads=["zt_sb"], writes=["zs_sb"])
                    for hl in range(2):
                        h = 2 * hh_ + hl
                        P.dma("sp", qa[hl][0:64, :], fmv[FM_BQ * 128 + hl * 64:FM_BQ * 128 + hl * 64 + 64, ts_], writes=[("qa", hl)])
                        P.dma("sp", qa[hl][64:68, :], qpos_own[hl][:, ts_], writes=[("qa", hl)])
                        chunks = [(kc, MK_WINB + (kc - 4 * tt + 1)) for kc in range(max(0, 4 * tt - 1), 4 * tt + 4)]
                        softmax_tile(qa[hl][:], ("qa", hl), kb, "kb", vb, "vb", chunks, sink_ap=es2[:, hl:hl + 1])
                        P.op("dve", lambda e: e.tensor_copy(ya[hl * 64:hl * 64 + 64, :], o_sb[:]),
                             reads=["o_sb"], writes=["ya"])
                    P.op("dve", lambda e: e.tensor_tensor(out=yo[:], in0=ya[:], in1=zs_sb[:], op=ALU.mult),
                         reads=["ya", "zs_sb"], writes=["yo"])
                    P.dma("pool", yzd[128:256, ts_], yo[:], reads=["yo"], writes=[("yzd", 1, tt)])
                P.barrier()
                cB.close()
            if "A" in mixers:
             with ExitStack() as c2:
              ksel = build_ka(c2, "ksel", FM_KSW, 0, kpos_tok[:, :])
              kwin = build_ka(c2, "kwin", FM_KSW, 64, kpos_tok[:, :])
              vsel = build_vo(c2, "vsel", TM_VSEL)
              vwin = build_vo(c2, "vwin", TM_VWIN)
              ewide_sb = sb(c2, "ewide_sb", [128, S], BF16)
              P.dma("sp", ewide_sb[:], ewide[:, :], writes=["ewide_sb"])
              keep_sb = sb(c2, "keep_sb", [128, 256], F32)
              add_sb = sb(c2, "add_sb", [128, 256], F32)
              P.dma("sp", keep_sb[:], selkeep[:, :], writes=["keep_sb"])
              P.dma("sp", add_sb[:], seladd[:, :], writes=["add_sb"])
              ps_sb = sb(c2, "ps_sb", [128, 2, 128], BF16)
              P.dma("sp", ps_sb[:], pairsum.rearrange("c p b -> p c b"), writes=["ps_sb"])
              kca = sb(c2, "kca", [68, 256], BF16)
              vcs = sb(c2, "vcs", [128, 2, 64], BF16)
              P.dma("sp", kca[64:68, :], kpos_cmp[:, :], writes=["kca"])
              with ExitStack() as c3:
                  kv = sb(c3, "kv", [128, S], BF16)
                  P.dma("sp", kv[:], fmv[FM_KVC * 128:(FM_KVC + 1) * 128, :], writes=["kv"])
                  posf = sb(c3, "posf", [128, 32], F32)
                  P.dma("sp", posf[0:64, :], cmp_posT[:, 0, :], writes=["posf"])
                  P.dma("sp", posf[64:128, :], cmp_posT[:, 1, :], writes=["posf"])
                  kvp = sb(c3, "kvp", [128, 256, 32], BF16)
                  P.op("dve", lambda e: e.tensor_tensor(out=kvp[:], in0=kv[:].rearrange("p (c j) -> p c j", j=32),
                                                        in1=_bc(posf[:], [128, 256, 32], 1), op=ALU.add),
                       reads=["kv", "posf"], writes=["kvp"])
                  w1f = sb(c3, "w1f", [128, 32, 64], F32)
                  w1b = sb(c3, "w1b", [128, 32, 64], BF16)
                  P.dma("sp", w1f[0:64], cmp_w1[:, 0], writes=["w1f"])
                  P.dma("sp", w1f[64:128], cmp_w1[:, 1], writes=["w1f"])
                  P.op("dve", lambda e: e.tensor_copy(w1b[:], w1f[:]), reads=["w1f"], writes=["w1b"])
                  w2f = sb(c3, "w2f", [128, 64], F32)
                  w2b = sb(c3, "w2b", [128, 64], BF16)
                  P.dma("sp", w2f[0:64], cmp_w2[:, 0], writes=["w2f"])
                  P.dma("sp", w2f[64:128], cmp_w2[:, 1], writes=["w2f"])
                  P.op("dve", lambda e: e.tensor_copy(w2b[:], w2f[:]), reads=["w2f"], writes=["w2b"])
                  hk = sb(c3, "hk", [128, 256], BF16)
                  for jj, p0 in enumerate((0, 64)):
                      for j in range(32):
                          P.op("pe", lambda e: e.matmul(banks[jj][p0:p0 + 64, 0:256], w1b[p0:p0 + 64, j, :], kvp[p0:p0 + 64, :, j],
                                                        start=(j == 0), stop=(j == 31)),
                               reads=["w1b", "kvp"], writes=[("bank", jj)])
                      P.op("act", lambda e: e.activation(out=hk[p0:p0 + 64, :], in_=banks[jj][p0:p0 + 64, 0:256], func=AF.Silu),
                           reads=[("bank", jj)], writes=["hk"])
                  P.op("pe", lambda e: e.matmul(banks[2][0:64, 0:256], w2b[0:64, :], hk[0:64, :], start=True, stop=True),
                       reads=["w2b", "hk"], writes=[("bank", 2)])
                  P.op("dve", lambda e: e.tensor_copy(kca[0:64, :], banks[2][0:64, 0:256]), reads=[("bank", 2)], writes=["kca"])
                  for cc in range(2):
                      P.op("pe", lambda e: e.matmul(banks[4 + cc][:, 0:64], hk[64:128, cc * 128:(cc + 1) * 128], w2b[64:128, :],
                                                    start=True, stop=True),
                           reads=["w2b", "hk"], writes=[("bank", 4 + cc)])
                      P.op("dve", lambda e: e.tensor_copy(vcs[:, cc, :], banks[4 + cc][:, 0:64]), reads=[("bank", 4 + cc)], writes=["vcs"])
                  P.barrier()
              e_sb = [sb(c2, "e_sb%d" % i, [128, TT], BF16) for i in range(8)]
              rec = sb(c2, "rec", [128, TT], F32)
              adj = sb(c2, "adj", [128, 128], F32)
              adj2 = sb(c2, "adj2", [128, 128], F32)
              m8a = sb(c2, "m8a", [128, 8], F32)
              m8b = sb(c2, "m8b", [128, 8], F32)
              mt = sb(c2, "mt", [128, 128], BF16)
              ocmp = sb(c2, "ocmp", [64, 2, TT], F32)
              bank4b = banks[4][:].bitcast(BF16)
              qa2 = [qa, [sb(c2, "qab%d" % h, [68, TT], BF16) for h in range(4)]]
              gl2 = [gl, [sb(c2, "glb%d" % i, [64, TT], F32) for i in range(6)]]
              ya2 = [ya, sb(c2, "yab", [128, TT], F32)]
              mneg2 = [sb(c2, "mneg%d" % i, [128, TT], BF16) for i in range(2)]
              gt_sb = sb(c2, "gt_sb", [64, TT], BF16)
              DEN = 7

              def prep(tt):
                  bf = tt % 2
                  ts_ = slice(tt * TT, (tt + 1) * TT)
                  qa_, gl_, ya_, mneg_ = qa2[bf], gl2[bf], ya2[bf], mneg2[bf]
                  for h in range(4):
                      ct = FM_AQ01 if h < 2 else FM_AQ23
                      r0 = ct * 128 + (h % 2) * 64
                      P.dma("sp", qa_[h][0:64, :], fmv[r0:r0 + 64, ts_], writes=[("qa", bf, h)])
                      P.dma("sp", qa_[h][64:68, :], qpos[h, :, ts_], writes=[("qa", bf, h)])
                  yield
                  for j in range(3):
                      for hl in range(2):
                          r0 = (FM_G0 + j) * 128 + hl * 64
                          gi = j * 2 + hl
                          P.dma("sp", gt_sb[:], fmv[r0:r0 + 64, ts_], writes=["gt_sb"])
                          P.op("act", lambda e: e.activation(out=gl_[gi][:], in_=gt_sb[:], func=AF.Sigmoid),
                               reads=["gt_sb"], writes=[("gl", bf, gi)])
                      yield
                  ccs = [0] if tt < 8 else [0, 1]
                  for h in range(4):
                      for cc in ccs:
                          zi = zrot[0] % 3
                          zrot[0] += 1
                          m = tt - 8 * cc
                          P.op("pe", lambda e: e.matmul(banks[zi][:], kca[0:68, cc * 128:(cc + 1) * 128], qa_[h][:],
                                                        start=True, stop=(m >= 8)),
                               reads=["kca", ("qa", bf, h)], writes=[("bank", zi)])
                          if m < 8:
                              P.op("pe", lambda e: e.matmul(banks[zi][:], ident_b[:], masks_sb[:, MK_CMP + m, :], start=False, stop=True),
                                   reads=["ident_b", "masks_sb"], writes=[("bank", zi)])
                          P.op("act", lambda e: e.activation(out=e_sb[h * 2 + cc][:], in_=banks[zi][:], func=AF.Exp),
                               reads=[("bank", zi)], writes=[("e_sb", h * 2 + cc)])
                          yield
                      for cc in ccs:
                          P.op("pe", lambda e: e.matmul(banks[DEN][:], ones_b[:], e_sb[h * 2 + cc][:], start=(cc == 0), stop=(cc == ccs[-1])),
                               reads=["ones_b", ("e_sb", h * 2 + cc)], writes=[("bank", DEN)])
                      P.op("dve", lambda e: e.tensor_scalar_max(out=rec[:], in0=banks[DEN][:], scalar1=1e-30),
                           reads=[("bank", DEN)], writes=["rec"])
                      P.op("dve", lambda e: e.reciprocal(out=rec[:], in_=rec[:]), reads=["rec"], writes=["rec"])
                      yield
                      for cc in ccs:
                          P.op("dve" if cc == 0 else "pool",
                               lambda e: e.tensor_tensor(out=e_sb[h * 2 + cc][:], in0=e_sb[h * 2 + cc][:], in1=rec[:], op=ALU.mult),
                               reads=[("e_sb", h * 2 + cc), "rec"], writes=[("e_sb", h * 2 + cc)])
                      if h // 2 == hh_:
                          hl = h % 2
                          for cc in ccs:
                              P.op("pe", lambda e: e.matmul(banks[5][0:64, :], vcs[:, cc, :], e_sb[h * 2 + cc][:],
                                                            start=(cc == 0), stop=(cc == ccs[-1])),
                                   reads=["vcs", ("e_sb", h * 2 + cc)], writes=[("bank", 5)])
                          P.op("dve", lambda e: e.tensor_tensor(out=ya_[hl * 64:hl * 64 + 64, :], in0=banks[5][0:64, :], in1=gl_[0 * 2 + hl][:],
                                                                op=ALU.mult),
                               reads=[("bank", 5), ("gl", bf, hl)], writes=[("ya", bf)])
                      yield
                  for sub in range(4):
                      i_blk = tt * 4 + sub
                      first_mm = True
                      nmm = 4 * len(ccs)
                      k_ = 0
                      for h in range(4):
                          for cc in ccs:
                              k_ += 1
                              P.op("pe", lambda e: e.matmul(banks[6][:, 0:128], e_sb[h * 2 + cc][:, sub * 128:(sub + 1) * 128], ps_sb[:, cc, :],
                                                            start=first_mm, stop=(k_ == nmm)),
                                   reads=[("e_sb", h * 2 + cc), "ps_sb"], writes=[("bank", 6)])
                              first_mm = False
                          if h == 1:
                              yield
                      o0 = 128 - 2 * i_blk
                      P.op("dve", lambda e: e.tensor_tensor(out=adj[:], in0=banks[6][:, 0:128], in1=keep_sb[:, o0:o0 + 128], op=ALU.mult),
                           reads=[("bank", 6), "keep_sb"], writes=["adj"])
                      P.op("dve", lambda e: e.tensor_tensor(out=adj[:], in0=adj[:], in1=add_sb[:, o0:o0 + 128], op=ALU.add),
                           reads=["adj", "add_sb"], writes=["adj"])
                      P.op("dve", lambda e: e.memset(adj[:, 0:1], 1e4), reads=["adj"], writes=["adj"])
                      yield
                      P.op("dve", lambda e: e.max(out=m8a[:], in_=adj[:]), reads=["adj"], writes=["m8a"])
                      P.op("dve", lambda e: e.match_replace(out=adj2[:], in_to_replace=m8a[:], in_values=adj[:], imm_value=-1e9),
                           reads=["adj", "m8a"], writes=["adj2"])
                      yield
                      P.op("dve", lambda e: e.max(out=m8b[:], in_=adj2[:]), reads=["adj2"], writes=["m8b"])
                      P.op("dve", lambda e: e.tensor_scalar(out=mt[:], in0=adj[:], scalar1=m8b[:, 7:8], scalar2=-1.0,
                                                            op0=ALU.is_ge, op1=ALU.add),
                           reads=["adj", "m8b"], writes=["mt"])
                      yield
                      P.op("pe", lambda e: e.transpose(bank4b[:, 0:128], mt[:], ident_b[:]),
                           reads=["mt", "ident_b"], writes=[("bank", 4)])
                      P.op("act", lambda e: e.activation(out=mneg_[:, sub * 128:(sub + 1) * 128], in_=bank4b[:, 0:128], func=AF.Copy),
                           reads=[("bank", 4)], writes=[("mneg", bf)])
                      yield

              def selwin(tt, bg):
                  bf = tt % 2
                  ts_ = slice(tt * TT, (tt + 1) * TT)
                  qa_, gl_, ya_, mneg_ = qa2[bf], gl2[bf], ya2[bf], mneg2[bf]
                  for hl in range(2):
                      h = 2 * hh_ + hl
                      chunks = [(kc, (MK_NONSTRICT + kc - 4 * tt) if kc >= 4 * tt else None) for kc in range(0, 4 * tt + 4)]
                      softmax_tile(qa_[h][:], ("qa", bf, h), ksel, "ksel", vsel, "vsel", chunks, selmask=mneg_, selkey=("mneg", bf), bg=bg)
                      P.op("dve", lambda e: e.tensor_tensor(out=tmp64[:], in0=o_sb[:], in1=gl_[1 * 2 + hl][:], op=ALU.mult),
                           reads=["o_sb", ("gl", bf, 2 + hl)], writes=["tmp64"])
                      P.op("dve", lambda e: e.tensor_copy(ocmp[:, hl, :], tmp64[:]), reads=["tmp64"], writes=["ocmp"])
                      chunks = []
                      for kc in range(max(0, 4 * tt - 4), 4 * tt + 4):
                          r = kc - 4 * tt
                          chunks.append((kc, MK_WINA + r + 4 if r < 0 else MK_NONSTRICT + r))
                      softmax_tile(qa_[h][:], ("qa", bf, h), kwin, "kwin", vwin, "vwin", chunks, bg=bg)
                      P.op("dve", lambda e: e.tensor_tensor(out=tmp64[:], in0=o_sb[:], in1=gl_[2 * 2 + hl][:], op=ALU.mult),
                           reads=["o_sb", ("gl", bf, 4 + hl)], writes=["tmp64"])
                      P.op("dve", lambda e: e.tensor_tensor(out=tmp64[:], in0=tmp64[:], in1=ocmp[:, hl, :], op=ALU.add),
                           reads=["tmp64", "ocmp"], writes=["tmp64"])
                      P.op("dve", lambda e: e.tensor_copy(ocmp[:, hl, :], ya_[hl * 64:hl * 64 + 64, :]), reads=[("ya", bf)], writes=["ocmp"])
                      P.op("dve", lambda e: e.tensor_tensor(out=ya_[hl * 64:hl * 64 + 64, :], in0=tmp64[:], in1=ocmp[:, hl, :], op=ALU.add),
                           reads=["tmp64", "ocmp"], writes=[("ya", bf)])
                  P.dma("sp", zt_sb[:], fmv[FM_AZ * 128:(FM_AZ + 1) * 128, ts_], writes=["zt_sb"])
                  P.op("act", lambda e: e.activation(out=zs_sb[:], in_=zt_sb[:], func=AF.Silu), reads=["zt_sb"], writes=["zs_sb"])
                  P.op("dve", lambda e: e.tensor_tensor(out=yo[:], in0=ya_[:], in1=zs_sb[:], op=ALU.mult),
                       reads=[("ya", bf), "zs_sb"], writes=["yo"])
                  P.dma("pool", yzd[0:128, ts_], yo[:], reads=["yo"], writes=[("yzd", 0, tt)])

              for _ in prep(0):
                  pass
              for tt in range(NT):
                  bg = prep(tt + 1) if tt + 1 < NT else None
                  selwin(tt, bg)
                  if bg is not None:
                      for _ in bg:
                          pass
              P.barrier()
        if "C" in mixers:
          with ExitStack() as c:
            ntri_sb = sb(c, "ntri_sb", [128, 128], BF16)
            P.dma("sp", ntri_sb[:], ntri[:, :], writes=["ntri_sb"])
            qs = sb(c, "qs", [64, S], BF16)
            ks = sb(c, "ks", [64, S], BF16)
            vs = sb(c, "vs", [128, NB, 64], BF16)
            ee = [sb(c, "ee%d" % i, [128, TT], F32) for i in range(2)]
            sp_ = [sb(c, "sp%d" % i, [128, TT], F32) for i in range(2)]
            hi = [sb(c, "hi%d" % i, [128, TT], BF16) for i in range(3)]
            lo = [sb(c, "lo%d" % i, [128, TT], BF16) for i in range(3)]
            ww = [sb(c, "ww%d" % i, [64, TT], F32) for i in range(3)]
            aa = [sb(c, "aa%d" % i, [128, TT], BF16) for i in range(2)]
            tmpc = sb(c, "tmpc", [64, TT], F32)
            oacc = sb(c, "oacc", [64, TT], F32)
            zt_c = sb(c, "zt_c", [64, TT], BF16)
            zs_c = sb(c, "zs_c", [64, TT], F32)
            yo_c = sb(c, "yo_c", [64, TT], BF16)
            CARRY = 4
            for hl in range(2):
                P.dma("sp", qs[:], fmv[FM_CQ * 128 + hl * 64:FM_CQ * 128 + hl * 64 + 64, :], writes=["qs"])
                P.dma("sp", ks[:], fmv[FM_CK * 128 + hl * 64:FM_CK * 128 + hl * 64 + 64, :], writes=["ks"])
                P.dma("sp", vs[:], tmv[:, :, TM_CV0 + 64 * hl:TM_CV0 + 64 * hl + 64], writes=["vs"])
                recs = []
                for tt in range(NT):
                    kcs = list(range(4 * tt + 3, -1, -1))
                    for j, kc in enumerate(kcs):
                        recs.append(dict(tt=tt, kc=kc, first=(j == 0), last=(j == len(kcs) - 1), i=len(recs)))

                def s1(r):
                    i = r["i"]; zi = i % 4; tt = r["tt"]; kc = r["kc"]
                    diag = kc >= 4 * tt
                    P.op("pe", lambda e: e.matmul(banks[zi][:], ks[:, kc * 128:(kc + 1) * 128], qs[:, tt * TT:(tt + 1) * TT],
                                                  start=True, stop=not diag),
                         reads=["ks", "qs"], writes=[("bank", zi)])
                    if diag:
                        P.op("pe", lambda e: e.matmul(banks[zi][:], ident_b[:], masks_sb[:, MK_STRICT + kc - 4 * tt, :], start=False, stop=True),
                             reads=["ident_b", "masks_sb"], writes=[("bank", zi)])

                def s2(r):
                    i = r["i"]; zi = i % 4
                    P.op("act", lambda e: e.activation(out=ee[i % 2][:], in_=banks[zi][:], func=AF.Exp),
                         reads=[("bank", zi)], writes=[("ee", i % 2)])
                    P.op("act", lambda e: e.activation(out=sp_[i % 2][:], in_=ee[i % 2][:], func=AF.Ln, bias=1.0),
                         reads=[("ee", i % 2)], writes=[("sp", i % 2)])
                    P.op("dve", lambda e: e.tensor_copy(hi[i % 3][:], sp_[i % 2][:]), reads=[("sp", i % 2)], writes=[("hi", i % 3)])
                    P.op("dve" if i % 2 == 0 else "pool", lambda e: e.tensor_tensor(out=lo[i % 3][:], in0=sp_[i % 2][:], in1=hi[i % 3][:], op=ALU.subtract),
                         reads=[("sp", i % 2), ("hi", i % 3)], writes=[("lo", i % 3)])

                def s3(r):
                    i = r["i"]; zi = i % 4
                    if not r["first"]:
                        P.op("act", lambda e: e.activation(out=ww[i % 3][:], in_=banks[CARRY][0:64, :], func=AF.Exp, scale=-1.0),
                             reads=[("bank", CARRY)], writes=[("ww", i % 3)])
                    P.op("pe", lambda e: e.matmul(banks[zi][:], ntri_sb[:], hi[i % 3][:], start=False, stop=False),
                         reads=["ntri_sb", ("hi", i % 3)], writes=[("bank", zi)])
                    P.op("pe", lambda e: e.matmul(banks[zi][:], ntri_sb[:], lo[i % 3][:], start=False, stop=True),
                         reads=["ntri_sb", ("lo", i % 3)], writes=[("bank", zi)])
                    if not r["last"]:
                        P.op("pe", lambda e: e.matmul(banks[CARRY][0:64, :], ones_b[:, 0:64], hi[i % 3][:], start=r["first"], stop=False),
                             reads=["ones_b", ("hi", i % 3)], writes=[("bank", CARRY)])
                        P.op("pe", lambda e: e.matmul(banks[CARRY][0:64, :], ones_b[:, 0:64], lo[i % 3][:], start=False, stop=True),
                             reads=["ones_b", ("lo", i % 3)], writes=[("bank", CARRY)])

                def s4(r):
                    i = r["i"]; zi = i % 4; tt = r["tt"]; kc = r["kc"]
                    pb = 5 + i % 2
                    P.op("act", lambda e: e.activation(out=aa[i % 2][:], in_=banks[zi][:], func=AF.Exp),
                         reads=[("bank", zi)], writes=[("aa", i % 2)])
                    P.op("pe", lambda e: e.matmul(banks[pb][0:64, :], vs[:, kc, :], aa[i % 2][:], start=True, stop=True),
                         reads=["vs", ("aa", i % 2)], writes=[("bank", pb)])
                    if r["first"]:
                        P.op("dve", lambda e: e.tensor_copy(oacc[:], banks[pb][0:64, :]), reads=[("bank", pb)], writes=["oacc"])
                    else:
                        P.op("dve", lambda e: e.tensor_tensor(out=tmpc[:], in0=banks[pb][0:64, :], in1=ww[i % 3][:], op=ALU.mult),
                             reads=[("bank", pb), ("ww", i % 3)], writes=["tmpc"])
                        P.op("pool", lambda e: e.tensor_tensor(out=oacc[:], in0=oacc[:], in1=tmpc[:], op=ALU.add),
                             reads=["oacc", "tmpc"], writes=["oacc"])
                    if r["last"]:
                        ts_ = slice(tt * TT, (tt + 1) * TT)
                        r0 = FM_CZ * 128 + hl * 64
                        P.dma("sp", zt_c[:], fmv[r0:r0 + 64, ts_], writes=["zt_c"])
                        P.op("act", lambda e: e.activation(out=zs_c[:], in_=zt_c[:], func=AF.Silu), reads=["zt_c"], writes=["zs_c"])
                        P.op("dve", lambda e: e.tensor_tensor(out=yo_c[:], in0=oacc[:], in1=zs_c[:], op=ALU.mult),
                             reads=["oacc", "zs_c"], writes=["yo_c"])
                        P.dma("pool", yzd[256 + hl * 64:256 + hl * 64 + 64, ts_], yo_c[:], reads=["yo_c"], writes=[("yzd", 2, hl, tt)])

                n = len(recs)
                for step in range(n + 3):
                    for lag, fn in ((0, s1), (1, s2), (2, s3), (3, s4)):
                        j = step - lag
                        if 0 <= j < n:
                            fn(recs[j])
            P.barrier()

        if "D" in mixers:
          with ExitStack() as c:
            qd = sb(c, "qd", [64, S], BF16)
            kd = sb(c, "kd", [64, S], BF16)
            vd = sb(c, "vd", [128, NB, 64], BF16)
            kt = sb(c, "kt", [128, NB, 64], BF16)
            vz = sb(c, "vz", [128, NB, 64], BF16)
            dt_sb = sb(c, "dt_sb", [128, 2, 128], F32)
            ze_sb = sb(c, "ze_sb", [128, 2], F32)
            xi_sb = sb(c, "xi_sb", [64, 2, 128], F32)
            gc_sb = sb(c, "gc_sb", [64, 2], F32)
            P.dma("sp", dt_sb[:], dtab[:, :, :], writes=["dt_sb"])
            P.dma("sp", ze_sb[:], zeta[:, :], writes=["ze_sb"])
            P.dma("sp", xi_sb[:], xi_bc[:, :, :], writes=["xi_sb"])
            P.dma("sp", gc_sb[:], gchunk[:, :], writes=["gc_sb"])
            uall = sb(c, "uall", [64, NB, 64], F32)
            rall = sb(c, "rall", [64, NB, 64], F32)
            rbf = sb(c, "rbf", [64, NB, 64], BF16)
            sm = [sb(c, "sm%d" % i, [128, TT], BF16) for i in range(2)]
            od = sb(c, "od", [64, TT], F32)
            od2 = sb(c, "od2", [64, TT], F32)
            mean = sb(c, "mean", [64, TT], F32)
            var = sb(c, "var", [64, TT], F32)
            zt_d = sb(c, "zt_d", [64, TT], BF16)
            zs_d = sb(c, "zs_d", [64, TT], F32)
            yo_d = sb(c, "yo_d", [64, TT], BF16)
            for hl in range(2):
                P.dma("sp", qd[:], fmv[FM_DQ * 128 + hl * 64:FM_DQ * 128 + hl * 64 + 64, :], writes=["qd"])
                P.dma("sp", kd[:], fmv[FM_DK * 128 + hl * 64:FM_DK * 128 + hl * 64 + 64, :], writes=["kd"])
                P.dma("sp", vd[:], tmv[:, :, TM_DV0 + 64 * hl:TM_DV0 + 64 * hl + 64], writes=["vd"])
                P.dma("sp", kt[:], tmv[:, :, TM_DK0 + 64 * hl:TM_DK0 + 64 * hl + 64], writes=["kt"])
                P.op("dve", lambda e: e.tensor_scalar(out=vz[:], in0=vd[:], scalar1=ze_sb[:, hl:hl + 1], scalar2=None, op0=ALU.mult),
                     reads=["vd", "ze_sb"], writes=["vz"])
                for g in range(NB // 8):
                    bi = g % 2
                    for j in range(8):
                        n_ = g * 8 + j
                        P.op("pe", lambda e: e.matmul(banks[bi][0:64, j * 64:(j + 1) * 64], kt[:, n_, :], vz[:, n_, :],
                                                      start=(j == 0), stop=(j == 7)),
                             reads=["kt", "vz"], writes=[("bank", bi)])
                    P.op("act", lambda e: e.activation(out=uall[:, g * 8:(g + 1) * 8, :],
                                                       in_=banks[bi][0:64, :].rearrange("p (n e) -> p n e", e=64), func=AF.Copy),
                         reads=[("bank", bi)], writes=["uall"])
                P.op("dve", lambda e: e.memset(rall[:, 0, :], 0.0), writes=["rall"])
                for n_ in range(1, NB):
                    P.op("dve", lambda e: e.scalar_tensor_tensor(out=rall[:, n_, :], in0=rall[:, n_ - 1, :], scalar=gc_sb[:, hl:hl + 1],
                                                                 in1=uall[:, n_ - 1, :], op0=ALU.mult, op1=ALU.add),
                         reads=["rall", "uall", "gc_sb"], writes=["rall"])
                P.op("dve", lambda e: e.tensor_copy(rbf[:], rall[:]), reads=["rall"], writes=["rbf"])
                for tt in range(NT):
                    ts_ = slice(tt * TT, (tt + 1) * TT)
                    si = tt % 2
                    zi = tt % 2
                    for sub in range(4):
                        n_ = tt * 4 + sub
                        cs_ = slice(n_ * 128, (n_ + 1) * 128)
                        P.op("pe", lambda e: e.matmul(banks[zi][:, sub * 128:(sub + 1) * 128], kd[:, cs_], qd[:, cs_],
                                                      start=(sub == 0), stop=(sub == 3)),
                             reads=["kd", "qd"], writes=[("bank", zi)])
                    P.op("dve", lambda e: e.tensor_tensor(out=sm[si][:].rearrange("p (n i) -> p n i", i=128),
                                                          in0=banks[zi][:].rearrange("p (n i) -> p n i", i=128),
                                                          in1=_bc(dt_sb[:, hl, :], [128, 4, 128], 1), op=ALU.mult),
                         reads=[("bank", zi), "dt_sb"], writes=[("sm", si)])
                    for sub in range(4):
                        n_ = tt * 4 + sub
                        cs_ = slice(n_ * 128, (n_ + 1) * 128)
                        P.op("pe", lambda e: e.matmul(banks[2 + zi][0:64, sub * 128:(sub + 1) * 128], vd[:, n_, :], sm[si][:, sub * 128:(sub + 1) * 128],
                                                      start=(sub == 0), stop=(sub == 3)),
                             reads=["vd", ("sm", si)], writes=[("bank", 2 + zi)])
                    for sub in range(4):
                        n_ = tt * 4 + sub
                        cs_ = slice(n_ * 128, (n_ + 1) * 128)
                        P.op("pe", lambda e: e.matmul(banks[4 + zi][0:64, sub * 128:(sub + 1) * 128], rbf[:, n_, :], qd[:, cs_],
                                                      start=(sub == 0), stop=(sub == 3)),
                             reads=["rbf", "qd"], writes=[("bank", 4 + zi)])
                    P.op("dve", lambda e: e.tensor_tensor(out=od[:].rearrange("p (n i) -> p n i", i=128),
                                                          in0=banks[4 + zi][0:64, :].rearrange("p (n i) -> p n i", i=128),
                                                          in1=_bc(xi_sb[:, hl, :], [64, 4, 128], 1), op=ALU.mult),
                         reads=[("bank", 4 + zi), "xi_sb"], writes=["od"])
                    P.op("dve", lambda e: e.tensor_tensor(out=od[:], in0=od[:], in1=banks[2 + zi][0:64, :], op=ALU.add),
                         reads=["od", ("bank", 2 + zi)], writes=["od"])
                    P.op("act", lambda e: e.activation(out=od2[:], in_=od[:], func=AF.Square), reads=["od"], writes=["od2"])
                    P.op("pe", lambda e: e.matmul(banks[6][0:64, :], ones_f[0:64, 0:64], od[:], start=True, stop=True),
                         reads=["ones_f", "od"], writes=[("bank", 6)])
                    P.op("pe", lambda e: e.matmul(banks[7][0:64, :], ones_f[0:64, 0:64], od2[:], start=True, stop=True),
                         reads=["ones_f", "od2"], writes=[("bank", 7)])
                    P.op("act", lambda e: e.activation(out=mean[:], in_=banks[6][0:64, :], func=AF.Copy, scale=1.0 / 64),
                         reads=[("bank", 6)], writes=["mean"])
                    P.op("dve", lambda e: e.tensor_tensor(out=var[:], in0=mean[:], in1=mean[:], op=ALU.mult),
                         reads=["mean"], writes=["var"])
                    P.op("dve", lambda e: e.scalar_tensor_tensor(out=var[:], in0=banks[7][0:64, :], scalar=1.0 / 64, in1=var[:],
                                                                 op0=ALU.mult, op1=ALU.subtract),
                         reads=[("bank", 7), "var"], writes=["var"])
                    P.op("act", lambda e: e.activation(out=var[:], in_=var[:], func=AF.Sqrt, bias=LN_EPS), reads=["var"], writes=["var"])
                    P.op("dve", lambda e: e.reciprocal(out=var[:], in_=var[:]), reads=["var"], writes=["var"])
                    P.op("dve", lambda e: e.tensor_tensor(out=od[:], in0=od[:], in1=mean[:], op=ALU.subtract),
                         reads=["od", "mean"], writes=["od"])
                    P.op("dve", lambda e: e.tensor_tensor(out=od[:], in0=od[:], in1=var[:], op=ALU.mult),
                         reads=["od", "var"], writes=["od"])
                    r0 = FM_DZ * 128 + hl * 64
                    P.dma("sp", zt_d[:], fmv[r0:r0 + 64, ts_], writes=["zt_d"])
                    P.op("act", lambda e: e.activation(out=zs_d[:], in_=zt_d[:], func=AF.Silu), reads=["zt_d"], writes=["zs_d"])
                    P.op("dve", lambda e: e.tensor_tensor(out=yo_d[:], in0=od[:], in1=zs_d[:], op=ALU.mult),
                         reads=["od", "zs_d"], writes=["yo_d"])
                    P.dma("pool", yzd[384 + hl * 64:384 + hl * 64 + 64, ts_], yo_d[:], reads=["yo_d"], writes=[("yzd", 3, hl, tt)])
            P.barrier()
        stage2.close()
        if stop_after <= 2:
            P.finish()
            return nc

        with ExitStack() as c:
            wm = sb(c, "wm", [128, 4, 8, D], BF16)
            wo = sb(c, "wo", [128, 8, D], BF16)
            wbr = sb(c, "wbr", [128, 4, D], BF16)
            stg = [sb(c, "stg3_%d" % i, [128, D], F32) for i in range(2)]
            k_ = 0
            for i in range(4):
                for kc in range(8):
                    load_cast(c, stg, wm[:, i, kc, :], w_merge[i, kc * 128:(kc + 1) * 128, :], D, ("wm", i, kc), k_)
                    k_ += 1
                load_cast(c, stg, wbr[:, i, :], w_br[i, :, :], D, ("wbr", i), k_)
                k_ += 1
            for kc in range(8):
                load_cast(c, stg, wo[:, kc, :], w_out[kc * 128:(kc + 1) * 128, :], D, ("wo", kc), k_)
                k_ += 1
            hb3 = [sb(c, "hb3_%d" % i, [128, 8, TT], BF16) for i in range(2)]
            yz3 = [sb(c, "yz3_%d" % i, [128, 4, TT], BF16) for i in range(2)]
            sg = [sb(c, "sg%d" % i, [128, TT], F32) for i in range(2)]
            term = [sb(c, "term%d" % i, [128, TT], F32) for i in range(2)]
            mrg = sb(c, "mrg", [128, 8, TT], F32)
            mg = sb(c, "mg", [128, 8, TT], BF16)
            po = [sb(c, "po%d" % i, [128, TT], F32) for i in range(2)]
            hview = hT.rearrange("(k p) t -> p k t", p=128)
            yview = yzd.rearrange("(m p) t -> p m t", p=128)
            pview = partT.rearrange("(k p) t -> p k t", p=128)
            nb_ = 0
            ns_ = 0
            for tt in range(NT):
                i2 = tt % 2
                ts_ = slice(tt * TT, (tt + 1) * TT)
                P.dma("sp", hb3[i2][:], hview[:, :, ts_], writes=[("hb3", i2)])
                P.dma("sp", yz3[i2][:], yview[:, :, ts_], writes=[("yz3", i2)])
                for oc in range(8):
                    for i in range(4):
                        bg = nb_ % 6
                        nb_ += 1
                        for kc in range(8):
                            P.op("pe", lambda e: e.matmul(banks[bg][:], wm[:, i, kc, oc * 128:(oc + 1) * 128], hb3[i2][:, kc, :],
                                                          start=(kc == 0), stop=(kc == 7)),
                                 reads=[("wm", i, kc), ("hb3", i2)], writes=[("bank", bg)])
                        bb = 6 + ns_ % 2
                        P.op("pe", lambda e: e.matmul(banks[bb][:], wbr[:, i, oc * 128:(oc + 1) * 128], yz3[i2][:, i, :], start=True, stop=True),
                             reads=[("wbr", i), ("yz3", i2)], writes=[("bank", bb)])
                        si = ns_ % 2
                        ns_ += 1
                        P.op("act", lambda e: e.activation(out=sg[si][:], in_=banks[bg][:], func=AF.Sigmoid),
                             reads=[("bank", bg)], writes=[("sg", si)])
                        if i == 0:
                            P.op("dve", lambda e: e.tensor_tensor(out=mrg[:, oc, :], in0=banks[bb][:], in1=sg[si][:], op=ALU.mult),
                                 reads=[("bank", bb), ("sg", si)], writes=[("mrg", oc)])
                        else:
                            P.op("dve", lambda e: e.tensor_tensor(out=term[si][:], in0=banks[bb][:], in1=sg[si][:], op=ALU.mult),
                                 reads=[("bank", bb), ("sg", si)], writes=[("term", si)])
                            P.op("pool", lambda e: e.tensor_tensor(out=mrg[:, oc, :], in0=mrg[:, oc, :], in1=term[si][:], op=ALU.add),
                                 reads=[("mrg", oc), ("term", si)], writes=[("mrg", oc)])
                    P.op("pool", lambda e: e.tensor_copy(mg[:, oc, :], mrg[:, oc, :]), reads=[("mrg", oc)], writes=[("mg", oc)])
                for oc2 in range(8):
                    bg = nb_ % 6
                    nb_ += 1
                    for kc in range(8):
                        P.op("pe", lambda e: e.matmul(banks[bg][:], wo[:, kc, oc2 * 128:(oc2 + 1) * 128], mg[:, kc, :],
                                                      start=(kc == 0), stop=(kc == 7)),
                             reads=[("wo", kc), ("mg", kc)], writes=[("bank", bg)])
                    pi = oc2 % 2
                    P.op("act", lambda e: e.activation(out=po[pi][:], in_=banks[bg][:], func=AF.Copy, scale=mod[:, 16 + oc2:17 + oc2]),
                         reads=[("bank", bg), "mod"], writes=[("po", pi)])
                    P.dma("pool", pview[:, oc2, ts_], po[pi][:], reads=[("po", pi)], is_output=True)
            P.barrier()
        P.finish()
    return nc


def _bf(a):
    return np.asarray(a, dtype=np.float32).astype(ml_dtypes.bfloat16)


def make_consts():
    sl = np.arange(128)[:, None]
    tl = np.arange(TT)[None, :]
    m = np.zeros((NMASK, 128, TT), np.float32)

    def put(idx, ok):
        m[idx] = np.where(ok, 0.0, NEG)
    for r in range(4):
        put(MK_STRICT + r, sl + 128 * r < tl)
        put(MK_NONSTRICT + r, sl + 128 * r <= tl)
    for j, r in enumerate(range(-4, 0)):
        d = tl - sl - 128 * r
        put(MK_WINA + j, (d >= 0) & (d < 512))
    for j, r in enumerate(range(-1, 4)):
        d = tl - sl - 128 * r
        put(MK_WINB + j, (d >= 0) & (d < 128))
    for mm in range(8):
        put(MK_CMP + mm, tl + 512 * mm - 32 * sl - 31 >= 0)
    t = np.arange(S)
    slopes = 2.0 ** (-8.0 * (np.arange(4) + 1) / 4)
    qpos = np.zeros((4, 4, S), np.float32)
    for h in range(4):
        qpos[h, 0] = -slopes[h] * 64 * (t // 64)
        qpos[h, 1] = -slopes[h] * (t % 64)
        qpos[h, 2] = slopes[h] * 64
        qpos[h, 3] = slopes[h]
    kpos_tok = np.stack([np.ones(S), np.ones(S), t // 64, t % 64]).astype(np.float32)
    pc = 32 * np.arange(256) + 31
    kpos_cmp = np.stack([np.ones(256), np.ones(256), pc // 64, pc % 64]).astype(np.float32)
    ewide = 30000.0 * (np.arange(S)[None, :] // 64 == np.arange(128)[:, None]).astype(np.float32)
    ident = np.eye(128, dtype=np.float32)
    ntri = -(np.arange(128)[:, None] >= np.arange(128)[None, :]).astype(np.float32)
    pairsum = np.zeros((2, 128, 128), np.float32)
    for cc in range(2):
        pairsum[cc, np.arange(128), 64 * cc + np.arange(128) // 2] = 1.0
    u = np.arange(256)[None, :] - 128
    cur = (np.arange(128)[:, None] >= 64).astype(np.int64)
    forced = (u == cur) | (u == cur - 1)
    future = u > cur
    selkeep = (~(forced | future)).astype(np.float32)
    seladd = np.where(forced, 1e4, np.where(future, -1.0, 0.0)).astype(np.float32)
    return dict(masks=_bf(m), qpos_all=qpos, kpos_tok=_bf(kpos_tok), kpos_cmp=_bf(kpos_cmp), ewide=_bf(ewide),
                ident=_bf(ident), ntri=_bf(ntri), pairsum=_bf(pairsum), selkeep=selkeep, seladd=seladd)


def ret_consts(hh):
    out = {}
    i = np.arange(128)
    dt_ = np.zeros((128, 2, 128), np.float32)
    zt = np.zeros((128, 2), np.float32)
    xi = np.zeros((64, 2, 128), np.float32)
    gc = np.zeros((64, 2), np.float32)
    for hl in range(2):
        h = 2 * hh + hl
        log_g = np.log(np.float32(1.0) - np.float32(2.0 ** (-5.0 - h)))
        diff = (i[None, :] - i[:, None]).astype(np.float32)
        dt_[:, hl, :] = np.where(diff >= 0, np.exp(log_g * np.maximum(diff, 0.0)), 0.0)
        zt[:, hl] = np.exp(log_g * (127 - i))
        xi[:, hl, :] = np.exp(log_g * (i + 1))[None, :]
        gc[:, hl] = np.exp(log_g * 128)
    return dict(dtab=dt_, zeta=zt, xi_bc=xi, gchunk=gc)


def arrange_w_in(w, hh):
    o = np.cumsum([0, 256, 384, 12, 256, 256, 128, 128, 256, 256, 256, 256, 256, 256, 256, 256, 256])
    a_q, a_kv, a_g, a_z, b_q, b_k, b_v, b_z, c_q, c_k, c_v, c_z, d_q, d_k, d_v, d_z = [
        w[:, o[i]:o[i + 1]] for i in range(16)]
    own = slice(128 * hh, 128 * hh + 128)
    kvh = slice(64 * hh, 64 * hh + 64)
    k_cmp, v_cmp, k_sel, v_sel, k_win, v_win = [a_kv[:, 64 * i:64 * i + 64] for i in range(6)]
    gcols = []
    for j in range(3):
        g = np.concatenate([np.repeat(a_g[:, 3 * (2 * hh + hl) + j][:, None], 64, axis=1) for hl in range(2)], axis=1)
        gcols.append(g)
    zpad = np.zeros((D, 64), np.float32)
    oth = slice(128 * (1 - hh), 128 * (1 - hh) + 128)
    fmt = [a_q[:, own], a_q[:, oth], np.concatenate([k_cmp, v_cmp], 1), np.concatenate([k_sel, k_win], 1),
           gcols[0], gcols[1], gcols[2], a_z[:, own], b_q[:, own], np.concatenate([b_k[:, kvh], zpad], 1), b_z[:, own],
           c_q[:, own], c_k[:, own], c_z[:, own], d_q[:, own], d_k[:, own], d_z[:, own]]
    tmc = [v_sel, v_win, b_v[:, kvh], c_v[:, own], d_v[:, own], d_k[:, own]]
    return np.ascontiguousarray(np.concatenate(fmt + tmc, axis=1), dtype=np.float32)


def layer_inputs(l, b, hh, c, w_ada, b_ada, norm_g, w_in, cmp_pos, cmp_w1, cmp_w2, sink, w_merge, w_br, w_out, consts):
    d = dict(consts)
    qp = d.pop("qpos_all")
    order = [2 * hh, 2 * hh + 1, 2 * (1 - hh), 2 * (1 - hh) + 1]
    d["qpos"] = _bf(qp[order])
    d.update(ret_consts(hh))
    d["cT"] = np.ascontiguousarray(c[b].reshape(8, 128).T)
    d["w_ada"] = np.ascontiguousarray(w_ada[l])
    d["b_adaT"] = np.ascontiguousarray(b_ada[l].reshape(24, 128).T)
    d["gT"] = np.ascontiguousarray(norm_g[l].reshape(8, 128).T)
    d["w_in"] = arrange_w_in(w_in[l], hh)
    d["cmp_posT"] = np.ascontiguousarray(cmp_pos[l].transpose(2, 0, 1))
    d["cmp_w1"] = np.ascontiguousarray(cmp_w1[l].reshape(2, 32, 64, 64).transpose(2, 0, 1, 3))
    d["cmp_w2"] = np.ascontiguousarray(cmp_w2[l].transpose(1, 0, 2))
    d["sinkb"] = np.ascontiguousarray(np.repeat(sink[l][2 * hh:2 * hh + 2], 64)[:, None].astype(np.float32))
    d["w_merge"] = np.ascontiguousarray(w_merge[l])
    d["w_br"] = np.ascontiguousarray(w_br[l][:, 128 * hh:128 * hh + 128, :])
    d["w_out"] = np.ascontiguousarray(w_out[l])
    return d


_PROG_CACHE = {}


def _prog(first, last_only):
    key = (first, last_only)
    if key not in _PROG_CACHE:
        _PROG_CACHE[key] = build_program(first, last_only)
    return _PROG_CACHE[key]


def kernel(x, c, w_ada, b_ada, norm_g, w_in, cmp_pos, cmp_w1, cmp_w2, sink, w_merge, w_br, w_out, final_g):
    args = [np.asarray(a, dtype=np.float32) for a in
            (x, c, w_ada, b_ada, norm_g, w_in, cmp_pos, cmp_w1, cmp_w2, sink, w_merge, w_br, w_out, final_g)]
    x, c, w_ada, b_ada, norm_g, w_in, cmp_pos, cmp_w1, cmp_w2, sink, w_merge, w_br, w_out, final_g = args
    B = x.shape[0]
    depth = w_ada.shape[0]
    consts = make_consts()
    cores = [(b, hh) for b in range(B) for hh in range(2)]
    xT = [np.ascontiguousarray(x[b].T) for b in range(B)]
    zero = np.zeros((D, S), np.float32)
    parts = [zero] * (2 * B)
    for l in range(depth):
        in_maps = []
        for (b, hh) in cores:
            d = layer_inputs(l, b, hh, c, w_ada, b_ada, norm_g, w_in, cmp_pos, cmp_w1, cmp_w2, sink, w_merge, w_br, w_out, consts)
            d["xT"] = xT[b]
            d["paT"] = parts[2 * b]
            d["pbT"] = parts[2 * b + 1]
            in_maps.append(d)
        res = run_bass_kernel_spmd(_prog(False, False), in_maps, core_ids=list(range(len(cores))))
        xT = [np.asarray(res.results[2 * b]["xcT"]) for b in range(B)]
        parts = [np.asarray(r["partT"]) for r in res.results]
    in_maps = []
    fgT = np.ascontiguousarray(final_g.reshape(8, 128).T)
    for (b, hh) in cores:
        in_maps.append({"xT": xT[b], "paT": parts[2 * b], "pbT": parts[2 * b + 1], "fgT": fgT})
    res = run_bass_kernel_spmd(_prog(False, True), in_maps, core_ids=list(range(len(cores))))
    out = np.stack([np.asarray(res.results[2 * b]["outT"]).T for b in range(B)]).astype(np.float32)
    return np.ascontiguousarray(out)
```

```python
from contextlib import ExitStack
import numpy as np
import ml_dtypes
import concourse.bass as bass
import concourse.mybir as mybir
from concourse.bass_utils import run_bass_kernel_spmd

F32 = mybir.dt.float32
BF16 = mybir.dt.bfloat16
AF = mybir.ActivationFunctionType
ALU = mybir.AluOpType

D = 1024
S = 8192
NT = 16
TT = 512
NB = 64
NEG = -30000.0
RMS_EPS = 1e-6
LN_EPS = 1e-5

FM_AQ01, FM_AQ23, FM_KVC, FM_KSW, FM_G0, FM_G1, FM_G2, FM_AZ, FM_BQ, FM_BK, FM_BZ, \
    FM_CQ, FM_CK, FM_CZ, FM_DQ, FM_DK, FM_DZ = range(17)
NFM = 17
Q_TILES = (FM_AQ01, FM_AQ23, FM_BQ, FM_CQ, FM_DQ)
TM_VSEL, TM_VWIN, TM_BV, TM_CV0, TM_CV1, TM_DV0, TM_DV1, TM_DK0, TM_DK1 = [64 * i for i in range(9)]
NTM = 576
NCOL = NFM * 128 + NTM

MK_STRICT = 0
MK_NONSTRICT = 4
MK_WINA = 8
MK_WINB = 12
MK_CMP = 17
NMASK = 25


class Prog:
    def __init__(self, nc, ctx, n_dma_sems=24):
        self.nc = nc
        self.eng = {"pe": nc.tensor, "act": nc.scalar, "dve": nc.vector, "pool": nc.gpsimd, "sp": nc.sync}
        self.sems = []
        self.semid = {}
        for e in self.eng:
            self.semid[e] = len(self.sems)
            self.sems.append(ctx.enter_context(nc.semaphore("p_" + e)))
        self.cnt = {e: 0 for e in self.eng}
        self.dsem = []
        for i in range(n_dma_sems):
            self.dsem.append(len(self.sems))
            self.sems.append(ctx.enter_context(nc.semaphore("d%d" % i)))
        self.duse = [0] * n_dma_sems
        n_sw = n_dma_sems // 3
        self.dpool = {"pool": list(range(0, n_sw)), "sp": list(range(n_sw, n_dma_sems))}
        self.dpos = {"pool": 0, "sp": 0}
        self.known = {e: {} for e in self.eng}
        self.res = {}
        self.out_tokens = []
        self.ninstr = 0

    def _st(self, k):
        st = self.res.get(k)
        if st is None:
            st = [None, {}]
            self.res[k] = st
        return st

    def _deps(self, reads, writes):
        need = {}

        def add(tok):
            s, v = tok
            if need.get(s, 0) < v:
                need[s] = v
        for k in reads:
            st = self._st(k)
            if st[0] is not None:
                add(st[0])
        for k in writes:
            st = self._st(k)
            if st[0] is not None:
                add(st[0])
            for s, v in st[1].items():
                add((s, v))
        return need

    def _wait(self, e, need):
        kn = self.known[e]
        for s, v in need.items():
            if e == "pe" and s == self.semid["pe"]:
                continue
            if kn.get(s, 0) >= v:
                continue
            self.eng[e].wait_ge(self.sems[s], v)
            kn[s] = v
            self.ninstr += 1

    def _record(self, tok, reads, writes):
        for k in reads:
            st = self._st(k)
            if st[1].get(tok[0], 0) < tok[1]:
                st[1][tok[0]] = tok[1]
        for k in writes:
            st = self._st(k)
            st[0] = tok
            st[1] = {}

    def op(self, e, fn, reads=(), writes=()):
        self._wait(e, self._deps(reads, writes))
        ins = fn(self.eng[e])
        self.cnt[e] += 1
        tok = (self.semid[e], self.cnt[e])
        ins.then_inc(self.sems[tok[0]], 1)
        self._record(tok, reads, writes)
        self.ninstr += 1
        return tok

    def dma(self, q, out, in_, reads=(), writes=(), is_output=False):
        pl = self.dpool[q]
        k = pl[self.dpos[q] % len(pl)]
        self.dpos[q] += 1
        need = self._deps(reads, writes)
        if self.duse[k] > 0:
            s = self.dsem[k]
            if need.get(s, 0) < 16 * self.duse[k]:
                need[s] = 16 * self.duse[k]
        self._wait(q, need)
        ins = self.eng[q].dma_start(out=out, in_=in_)
        self.duse[k] += 1
        tok = (self.dsem[k], 16 * self.duse[k])
        ins.then_inc(self.sems[tok[0]], 16)
        self._record(tok, reads, writes)
        if is_output:
            self.out_tokens.append(tok)
        self.ninstr += 1
        return tok

    def barrier(self):
        need = {}
        for e in self.eng:
            if self.cnt[e] > 0:
                need[self.semid[e]] = self.cnt[e]
        for k, s in enumerate(self.dsem):
            if self.duse[k] > 0:
                need[s] = 16 * self.duse[k]
        for e in self.eng:
            n2 = {s: v for s, v in need.items() if s != self.semid[e] or e != "pe"}
            self._wait(e, n2)
        self.res = {}

    def finish(self):
        need = {}
        for s, v in self.out_tokens:
            if need.get(s, 0) < v:
                need[s] = v
        self._wait("sp", need)


def _bc(ap, shape, axis):
    return ap.unsqueeze(axis).broadcast_to(shape)


def build_program(first, last_only, dbg=None, stop_after=99, mixers="ABCD"):
    dbg = dbg or set()
    nc = bass.Bass("TRN2", target_bir_lowering=False)

    def din(name, shape, dt=F32):
        return nc.dram_tensor(name, list(shape), dt, kind="ExternalInput").ap()

    def dout(name, shape, dt=F32):
        return nc.dram_tensor(name, list(shape), dt, kind="ExternalOutput").ap()

    def dscr(name, shape, dt):
        kind = "ExternalOutput" if name in dbg else "Internal"
        return nc.dram_tensor(name, list(shape), dt, kind=kind).ap()

    xT = din("xT", [D, S])
    if not first:
        paT = din("paT", [D, S])
        pbT = din("pbT", [D, S])
    if last_only:
        fgT = din("fgT", [128, 8])
        outT = dout("outT", [D, S])
    else:
        cT = din("cT", [128, 8])
        w_ada = din("w_ada", [D, 3 * D])
        b_adaT = din("b_adaT", [128, 24])
        gT = din("gT", [128, 8])
        w_in = din("w_in", [D, NCOL])
        cmp_posT = din("cmp_posT", [64, 2, 32])
        cmp_w1 = din("cmp_w1", [64, 2, 32, 64])
        cmp_w2 = din("cmp_w2", [64, 2, 64])
        sinkb = din("sinkb", [128, 1])
        w_merge = din("w_merge", [4, D, D])
        w_br = din("w_br", [4, 128, D])
        w_out = din("w_out", [D, D])
        masks = din("masks", [NMASK, 128, TT], BF16)
        qpos = din("qpos", [4, 4, S], BF16)
        kpos_tok = din("kpos_tok", [4, S], BF16)
        kpos_cmp = din("kpos_cmp", [4, 256], BF16)
        ewide = din("ewide", [128, S], BF16)
        ident = din("ident", [128, 128], BF16)
        ntri = din("ntri", [128, 128], BF16)
        pairsum = din("pairsum", [2, 128, 128], BF16)
        selkeep = din("selkeep", [128, 256])
        seladd = din("seladd", [128, 256])
        dtab = din("dtab", [128, 2, 128])
        zeta = din("zeta", [128, 2])
        xi_bc = din("xi_bc", [64, 2, 128])
        gchunk = din("gchunk", [64, 2])
        partT = dout("partT", [D, S])
        if not first:
            xcT = dout("xcT", [D, S])
        hT = dscr("hT", [D, S], BF16)
        fm = dscr("fm", [NFM * 128, S], BF16)
        tm = dscr("tm", [S, NTM], BF16)
        yzd = dscr("yzd", [512, S], BF16)

    ctx = ExitStack()
    with ctx:
        P = Prog(nc, ctx)
        banks = [ctx.enter_context(nc.psum_tensor("bank%d" % i, [128, 512], F32)) for i in range(8)]

        def sb(c, name, shape, dt):
            return c.enter_context(nc.sbuf_tensor(name, list(shape), dt))

        ones_f = sb(ctx, "ones_f", [128, 128], F32)
        P.op("dve", lambda e: e.memset(ones_f[:], 1.0), writes=["ones_f"])
        xview = xT.rearrange("(k p) t -> p k t", p=128)

        if last_only:
            with ExitStack() as c:
                fg = sb(c, "fg", [128, 8], F32)
                P.dma("sp", fg[:], fgT[:, :], writes=["fg"])
                pav = paT.rearrange("(k p) t -> p k t", p=128)
                pbv = pbT.rearrange("(k p) t -> p k t", p=128)
                ov = outT.rearrange("(k p) t -> p k t", p=128)
                xt = [sb(c, "xt%d" % i, [128, 8, TT], F32) for i in range(2)]
                pt = [sb(c, "pt%d" % i, [128, 8, TT], F32) for i in range(2)]
                qt = [sb(c, "qt%d" % i, [128, 8, TT], F32) for i in range(2)]
                sq = sb(c, "sq", [128, 8, TT], F32)
                rt = sb(c, "rt", [128, TT], F32)
                rstd = sb(c, "rstd", [128, TT], F32)
                for tt in range(NT):
                    i = tt % 2
                    ts_ = slice(tt * TT, (tt + 1) * TT)
                    P.dma("sp", xt[i][:], xview[:, :, ts_], writes=[("xt", i)])
                    P.dma("sp", pt[i][:], pav[:, :, ts_], writes=[("pt", i)])
                    P.dma("sp", qt[i][:], pbv[:, :, ts_], writes=[("qt", i)])
                    P.op("dve", lambda e: e.tensor_tensor(out=xt[i][:], in0=xt[i][:], in1=pt[i][:], op=ALU.add),
                         reads=[("xt", i), ("pt", i)], writes=[("xt", i)])
                    P.op("dve", lambda e: e.tensor_tensor(out=xt[i][:], in0=xt[i][:], in1=qt[i][:], op=ALU.add),
                         reads=[("xt", i), ("qt", i)], writes=[("xt", i)])
                    P.op("act", lambda e: e.activation(out=sq[:], in_=xt[i][:], func=AF.Square),
                         reads=[("xt", i)], writes=["sq"])
                    bk = banks[tt % 2]
                    for kc in range(8):
                        P.op("pe", lambda e: e.matmul(bk[:], ones_f[:], sq[:, kc, :], start=(kc == 0), stop=(kc == 7)),
                             reads=["sq", "ones_f"], writes=[("bank", tt % 2)])
                    P.op("act", lambda e: e.activation(out=rt[:], in_=bk[:], func=AF.Sqrt, scale=1.0 / D, bias=RMS_EPS),
                         reads=[("bank", tt % 2)], writes=["rt"])
                    P.op("dve", lambda e: e.reciprocal(out=rstd[:], in_=rt[:]), reads=["rt"], writes=["rstd"])
                    P.op("dve", lambda e: e.tensor_tensor(out=xt[i][:], in0=xt[i][:],
                                                          in1=_bc(rstd[:], [128, 8, TT], 1), op=ALU.mult),
                         reads=[("xt", i), "rstd"], writes=[("xt", i)])
                    for kc in range(8):
                        P.op("act", lambda e: e.activation(out=xt[i][:, kc, :], in_=xt[i][:, kc, :], func=AF.Copy,
                                                           scale=fg[:, kc:kc + 1]),
                             reads=[("xt", i), "fg"], writes=[("xt", i)])
                    P.dma("pool", ov[:, :, ts_], xt[i][:], reads=[("xt", i)], is_output=True)
            P.finish()
            return nc


        ident_b = sb(ctx, "ident_b", [128, 128], BF16)
        P.dma("sp", ident_b[:], ident[:, :], writes=["ident_b"])
        ones_b = sb(ctx, "ones_b", [128, 128], BF16)
        P.op("dve", lambda e: e.memset(ones_b[:], 1.0), writes=["ones_b"])
        mod = sb(ctx, "mod", [128, 24], F32)
        gs = sb(ctx, "gs", [128, 8], F32)
        with ExitStack() as c:
            cs = sb(c, "cs", [128, 8], F32)
            csil = sb(c, "csil", [128, 8, 2], F32)
            bada = sb(c, "bada", [128, 24], F32)
            gt_ = sb(c, "gt_", [128, 8], F32)
            wa = [sb(c, "wa%d" % i, [128, 3 * D], F32) for i in range(2)]
            P.dma("sp", cs[:], cT[:, :], writes=["cs"])
            P.dma("sp", bada[:], b_adaT[:, :], writes=["bada"])
            P.dma("sp", gt_[:], gT[:, :], writes=["gt_"])
            for j in range(2):
                P.op("act", lambda e: e.activation(out=csil[:, :, j], in_=cs[:], func=AF.Silu),
                     reads=["cs"], writes=["csil"])
            for kc in range(8):
                i = kc % 2
                P.dma("sp", wa[i][:], w_ada[kc * 128:(kc + 1) * 128, :], writes=[("wa", i)])
                for oc in range(24):
                    P.op("pe", lambda e: e.matmul(banks[0][:, 2 * oc:2 * oc + 2], wa[i][:, oc * 128:(oc + 1) * 128],
                                                  csil[:, kc, :], start=(kc == 0 and oc == 0), stop=(kc == 7 and oc == 23)),
                         reads=[("wa", i), "csil"], writes=[("bank", 0)])
            P.op("dve", lambda e: e.tensor_tensor(out=mod[:], in0=banks[0][:, 0:48:2], in1=bada[:], op=ALU.add),
                 reads=[("bank", 0), "bada"], writes=["mod"])
            P.op("dve", lambda e: e.scalar_tensor_tensor(out=gs[:], in0=mod[:, 8:16], scalar=1.0, in1=gt_[:],
                                                         op0=ALU.add, op1=ALU.mult),
                 reads=["mod", "gt_"], writes=["gs"])
            P.barrier()

        def load_cast(c_stage, stg, dst_ap, src_ap, n, key_dst, idx):
            i = idx % 2
            P.dma("sp", stg[i][:, 0:n], src_ap, writes=[("stg", i)])
            eng = "dve" if idx % 2 == 0 else "pool"
            P.op(eng, lambda e: e.tensor_copy(dst_ap, stg[i][:, 0:n]), reads=[("stg", i)], writes=[key_dst])

        with ExitStack() as c:
            wb = sb(c, "wb", [128, 8, NCOL], BF16)
            stg = [sb(c, "stg%d" % i, [128, NCOL], F32) for i in range(2)]
            for kc in range(8):
                load_cast(c, stg, wb[:, kc, :], w_in[kc * 128:(kc + 1) * 128, :], NCOL, ("wb", kc), kc)
            xt = [sb(c, "xt%d" % i, [128, 8, TT], F32) for i in range(2)]
            if not first:
                pt = sb(c, "pt", [128, 8, TT], F32)
                ptb = sb(c, "ptb", [128, 8, TT], F32)
                pav = paT.rearrange("(k p) t -> p k t", p=128)
                pbv = pbT.rearrange("(k p) t -> p k t", p=128)
                xcv = xcT.rearrange("(k p) t -> p k t", p=128)
            sq = sb(c, "sq", [128, 8, TT], F32)
            rt = sb(c, "rt", [128, TT], F32)
            rstd = sb(c, "rstd", [128, TT], F32)
            hb = [sb(c, "hb%d" % i, [128, 8, TT], BF16) for i in range(2)]
            ev = [sb(c, "ev%d" % i, [128, TT], BF16) for i in range(4)]
            evt = [sb(c, "evt%d" % i, [128, NTM], BF16) for i in range(2)]
            hview = hT.rearrange("(k p) t -> p k t", p=128)
            ctr = {'nbank': 0, 'nev': 0}

            def s1_norm(tt):
                i = tt % 2
                ts_ = slice(tt * TT, (tt + 1) * TT)
                P.dma("sp", xt[i][:], xview[:, :, ts_], writes=[("xt", i)])
                if not first:
                    for pv, pbuf, pkey in ((pav, pt, "pt"), (pbv, ptb, "ptb")):
                        P.dma("sp", pbuf[:], pv[:, :, ts_], writes=[pkey])
                    for pv, pbuf, pkey in ((pav, pt, "pt"), (pbv, ptb, "ptb")):
                        P.op("dve", lambda e: e.tensor_tensor(out=xt[i][:], in0=xt[i][:], in1=pbuf[:], op=ALU.add),
                             reads=[("xt", i), pkey], writes=[("xt", i)])
                    P.dma("pool", xcv[:, :, ts_], xt[i][:], reads=[("xt", i)], is_output=True)
                P.op("act", lambda e: e.activation(out=sq[:], in_=xt[i][:], func=AF.Square),
                     reads=[("xt", i)], writes=["sq"])
                bi = ctr['nbank'] % 8
                ctr['nbank'] += 1
                for kc in range(8):
                    P.op("pe", lambda e: e.matmul(banks[bi][:], ones_f[:], sq[:, kc, :], start=(kc == 0), stop=(kc == 7)),
                         reads=["sq", "ones_f"], writes=[("bank", bi)])
                P.op("act", lambda e: e.activation(out=rt[:], in_=banks[bi][:], func=AF.Sqrt, scale=1.0 / D, bias=RMS_EPS),
                     reads=[("bank", bi)], writes=["rt"])
                P.op("dve", lambda e: e.reciprocal(out=rstd[:], in_=rt[:]), reads=["rt"], writes=["rstd"])
                P.op("dve", lambda e: e.tensor_tensor(out=sq[:], in0=xt[i][:], in1=_bc(rstd[:], [128, 8, TT], 1), op=ALU.mult),
                     reads=[("xt", i), "rstd", "sq"], writes=["sq"])
                for kc in range(8):
                    P.op("act", lambda e: e.activation(out=hb[i][:, kc, :], in_=sq[:, kc, :], func=AF.Identity,
                                                       scale=gs[:, kc:kc + 1], bias=mod[:, kc:kc + 1]),
                         reads=["sq", "gs", "mod"], writes=[("hb", i)])
                P.dma("pool", hview[:, :, ts_], hb[i][:], reads=[("hb", i)], writes=[("hT", tt)])

            def s1_proj(tt):
                i = tt % 2
                ts_ = slice(tt * TT, (tt + 1) * TT)
                for ct in range(NFM):
                    bi = ctr['nbank'] % 8
                    ctr['nbank'] += 1
                    for kc in range(8):
                        P.op("pe", lambda e: e.matmul(banks[bi][:], wb[:, kc, ct * 128:(ct + 1) * 128], hb[i][:, kc, :],
                                                      start=(kc == 0), stop=(kc == 7)),
                             reads=[("wb", kc), ("hb", i)], writes=[("bank", bi)])
                    ei = ctr['nev'] % 4
                    ctr['nev'] += 1
                    if ct in Q_TILES:
                        P.op("act", lambda e: e.activation(out=ev[ei][:], in_=banks[bi][:], func=AF.Copy, scale=0.125),
                             reads=[("bank", bi)], writes=[("ev", ei)])
                    elif ct % 2 == 0:
                        P.op("act", lambda e: e.activation(out=ev[ei][:], in_=banks[bi][:], func=AF.Copy),
                             reads=[("bank", bi)], writes=[("ev", ei)])
                    else:
                        P.op("dve", lambda e: e.tensor_copy(ev[ei][:], banks[bi][:]),
                             reads=[("bank", bi)], writes=[("ev", ei)])
                    P.dma("pool", fm[ct * 128:(ct + 1) * 128, ts_], ev[ei][:], reads=[("ev", ei)], writes=[("fm", ct, tt)])

            def s1_proj_tm(tt):
                i = tt % 2
                ts_ = slice(tt * TT, (tt + 1) * TT)
                for sub in range(4):
                    ei = (tt * 4 + sub) % 2
                    for g, (c0, c1) in enumerate(((0, 320), (320, 576))):
                        bi = ctr['nbank'] % 8
                        ctr['nbank'] += 1
                        for kc in range(8):
                            P.op("pe", lambda e: e.matmul(banks[bi][:, 0:c1 - c0], hb[i][:, kc, sub * 128:(sub + 1) * 128],
                                                          wb[:, kc, NFM * 128 + c0:NFM * 128 + c1],
                                                          start=(kc == 0), stop=(kc == 7)),
                                 reads=[("wb", kc), ("hb", i)], writes=[("bank", bi)])
                        P.op("dve" if g == 0 else "act",
                             (lambda e: e.tensor_copy(evt[ei][:, c0:c1], banks[bi][:, 0:c1 - c0])) if g == 0 else
                             (lambda e: e.activation(out=evt[ei][:, c0:c1], in_=banks[bi][:, 0:c1 - c0], func=AF.Copy)),
                             reads=[("bank", bi)], writes=[("evt", ei)])
                    r0 = tt * TT + sub * 128
                    P.dma("pool", tm[r0:r0 + 128, :], evt[ei][:], reads=[("evt", ei)], writes=[("tm", tt)])

            s1_norm(0)
            for tt in range(NT):
                s1_proj(tt)
                if tt + 1 < NT:
                    s1_norm(tt + 1)
                s1_proj_tm(tt)
            P.barrier()

        if stop_after <= 1:
            P.finish()
            return nc
        hh_ = 0
        qpos_own = [qpos[0], qpos[1]]
        fmv = fm
        tmv = tm.rearrange("(n p) c -> p n c", p=128)
        stage2 = ExitStack()
        masks_sb = sb(stage2, "masks_sb", [128, NMASK, TT], BF16)
        P.dma("sp", masks_sb[:], masks.rearrange("m p t -> p m t"), writes=["masks_sb"])
        zrot = [0]
        arot = [0]
        a_sb = [sb(stage2, "a_sb%d" % i, [128, TT], BF16) for i in range(3)]
        o_sb = sb(stage2, "o_sb", [64, TT], F32)
        dn_sb = sb(stage2, "dn_sb", [64, TT], F32)
        ACC = 3

        def softmax_tile(q_ap, qkey, ka, kakey, vo, vokey, chunks, selmask=None, sink_ap=None, selkey=None, bg=None):
            n = len(chunks)
            slots = []

            def qk(idx):
                kc, mk = chunks[idx]
                zi = zrot[0] % 3
                zrot[0] += 1
                ai = arot[0] % 3
                arot[0] += 1
                slots.append((zi, ai))
                mms = [(ka[0:68, kc * 128:(kc + 1) * 128], q_ap, [kakey, qkey])]
                if selmask is not None:
                    mms.append((ewide_sb[:, kc * 128:(kc + 1) * 128], selmask[:], ["ewide_sb", selkey]))
                if mk is not None:
                    mms.append((ident_b[:], masks_sb[:, mk, :], ["ident_b", "masks_sb"]))
                for j, (l_, r_, rd) in enumerate(mms):
                    P.op("pe", lambda e: e.matmul(banks[zi][:], l_, r_, start=(j == 0), stop=(j == len(mms) - 1)),
                         reads=rd, writes=[("bank", zi)])

            def ex(idx):
                zi, ai = slots[idx]
                P.op("act", lambda e: e.activation(out=a_sb[ai][:], in_=banks[zi][:], func=AF.Exp),
                     reads=[("bank", zi)], writes=[("a_sb", ai)])

            def av(idx):
                kc, mk = chunks[idx]
                zi, ai = slots[idx]
                P.op("pe", lambda e: e.matmul(banks[ACC][:], vo[:, kc, :], a_sb[ai][:], start=(idx == 0), stop=(idx == n - 1)),
                     reads=[vokey, ("a_sb", ai)], writes=[("bank", ACC)])

            for step in range(n + 2):
                if step < n:
                    qk(step)
                if 0 <= step - 1 < n:
                    ex(step - 1)
                if 0 <= step - 2 < n:
                    av(step - 2)
                if bg is not None:
                    next(bg, None)
            if sink_ap is not None:
                P.op("dve", lambda e: e.tensor_scalar(out=dn_sb[:], in0=banks[ACC][64:128, :], scalar1=sink_ap, scalar2=1e-30,
                                                      op0=ALU.add, op1=ALU.max),
                     reads=[("bank", ACC), "esink"], writes=["dn_sb"])
            else:
                P.op("dve", lambda e: e.tensor_scalar_max(out=dn_sb[:], in0=banks[ACC][64:128, :], scalar1=1e-30),
                     reads=[("bank", ACC)], writes=["dn_sb"])
            P.op("dve", lambda e: e.reciprocal(out=dn_sb[:], in_=dn_sb[:]), reads=["dn_sb"], writes=["dn_sb"])
            P.op("dve", lambda e: e.tensor_tensor(out=o_sb[:], in0=banks[ACC][0:64, :], in1=dn_sb[:], op=ALU.mult),
                 reads=[("bank", ACC), "dn_sb"], writes=["o_sb"])

        def build_vo(c, name, col):
            vo = sb(c, name, [128, NB, 128], BF16)
            P.dma("sp", vo[:, :, 0:64], tmv[:, :, col:col + 64], writes=[name])
            P.op("pool", lambda e: e.memset(vo[:, :, 64:128], 1.0), writes=[name])
            return vo

        def build_ka(c, name, ct, row0, kp):
            ka = sb(c, name, [68, S], BF16)
            P.dma("sp", ka[0:64, :], fmv[ct * 128 + row0:ct * 128 + row0 + 64, :], writes=[name])
            P.dma("sp", ka[64:68, :], kp, writes=[name])
            return ka

        if "B" in mixers or "A" in mixers:
          with ExitStack() as c:
            qa = [sb(c, "qa%d" % h, [68, TT], BF16) for h in range(4)]
            gl = [sb(c, "gl%d" % i, [64, TT], F32) for i in range(6)]
            zt_sb = sb(c, "zt_sb", [128, TT], BF16)
            zs_sb = sb(c, "zs_sb", [128, TT], F32)
            ya = sb(c, "ya", [128, TT], F32)
            yo = sb(c, "yo", [128, TT], BF16)
            tmp64 = sb(c, "tmp64", [64, TT], F32)
            if "B" in mixers:
                cB = ExitStack()
                kb = build_ka(cB, "kb", FM_BK, 0, kpos_tok[:, :])
                vb = build_vo(cB, "vb", TM_BV)
                esink = sb(cB, "esink", [128, 1], F32)
                P.dma("sp", esink[:], sinkb[:, :], writes=["esink"])
                P.op("act", lambda e: e.activation(out=esink[:], in_=esink[:], func=AF.Exp), reads=["esink"], writes=["esink"])
                es2 = sb(cB, "es2", [64, 2], F32)
                P.op("dve", lambda e: e.tensor_copy(es2[:, 0:1], esink[0:64, :]), reads=["esink"], writes=["esink2"])
                P.op("dve", lambda e: e.tensor_copy(es2[:, 1:2], esink[64:128, :]), reads=["esink"], writes=["esink2"])
                for tt in range(NT):
                    ts_ = slice(tt * TT, (tt + 1) * TT)
                    P.dma("sp", zt_sb[:], fmv[FM_BZ * 128:(FM_BZ + 1) * 128, ts_], writes=["zt_sb"])
                    P.op("act", lambda e: e.activation(out=zs_sb[:], in_=zt_sb[:], func=AF.Silu), reads=["zt_sb"], writes=["zs_sb"])
                    for hl in range(2):
                        h = 2 * hh_ + hl
                        P.dma("sp", qa[hl][0:64, :], fmv[FM_BQ * 128 + hl * 64:FM_BQ * 128 + hl * 64 + 64, ts_], writes=[("qa", hl)])
                        P.dma("sp", qa[hl][64:68, :], qpos_own[hl][:, ts_], writes=[("qa", hl)])
                        chunks = [(kc, MK_WINB + (kc - 4 * tt + 1)) for kc in range(max(0, 4 * tt - 1), 4 * tt + 4)]
                        softmax_tile(qa[hl][:], ("qa", hl), kb, "kb", vb, "vb", chunks, sink_ap=es2[:, hl:hl + 1])
                        P.op("dve", lambda e: e.tensor_copy(ya[hl * 64:hl * 64 + 64, :], o_sb[:]),
                             reads=["o_sb"], writes=["ya"])
                    P.op("dve", lambda e: e.tensor_tensor(out=yo[:], in0=ya[:], in1=zs_sb[:], op=ALU.mult),
                         reads=["ya", "zs_sb"], writes=["yo"])
                    P.dma("pool", yzd[128:256, ts_], yo[:], reads=["yo"], writes=[("yzd", 1, tt)])
                P.barrier()
                cB.close()
            if "A" in mixers:
             with ExitStack() as c2:
              ksel = build_ka(c2, "ksel", FM_KSW, 0, kpos_tok[:, :])
              kwin = build_ka(c2, "kwin", FM_KSW, 64, kpos_tok[:, :])
              vsel = build_vo(c2, "vsel", TM_VSEL)
              vwin = build_vo(c2, "vwin", TM_VWIN)
              ewide_sb = sb(c2, "ewide_sb", [128, S], BF16)
              P.dma("sp", ewide_sb[:], ewide[:, :], writes=["ewide_sb"])
              keep_sb = sb(c2, "keep_sb", [128, 256], F32)
              add_sb = sb(c2, "add_sb", [128, 256], F32)
              P.dma("sp", keep_sb[:], selkeep[:, :], writes=["keep_sb"])
              P.dma("sp", add_sb[:], seladd[:, :], writes=["add_sb"])
              ps_sb = sb(c2, "ps_sb", [128, 2, 128], BF16)
              P.dma("sp", ps_sb[:], pairsum.rearrange("c p b -> p c b"), writes=["ps_sb"])
              kca = sb(c2, "kca", [68, 256], BF16)
              vcs = sb(c2, "vcs", [128, 2, 64], BF16)
              P.dma("sp", kca[64:68, :], kpos_cmp[:, :], writes=["kca"])
              with ExitStack() as c3:
                  kv = sb(c3, "kv", [128, S], BF16)
                  P.dma("sp", kv[:], fmv[FM_KVC * 128:(FM_KVC + 1) * 128, :], writes=["kv"])
                  posf = sb(c3, "posf", [128, 32], F32)
                  P.dma("sp", posf[0:64, :], cmp_posT[:, 0, :], writes=["posf"])
                  P.dma("sp", posf[64:128, :], cmp_posT[:, 1, :], writes=["posf"])
                  kvp = sb(c3, "kvp", [128, 256, 32], BF16)
                  P.op("dve", lambda e: e.tensor_tensor(out=kvp[:], in0=kv[:].rearrange("p (c j) -> p c j", j=32),
                                                        in1=_bc(posf[:], [128, 256, 32], 1), op=ALU.add),
                       reads=["kv", "posf"], writes=["kvp"])
                  w1f = sb(c3, "w1f", [128, 32, 64], F32)
                  w1b = sb(c3, "w1b", [128, 32, 64], BF16)
                  P.dma("sp", w1f[0:64], cmp_w1[:, 0], writes=["w1f"])
                  P.dma("sp", w1f[64:128], cmp_w1[:, 1], writes=["w1f"])
                  P.op("dve", lambda e: e.tensor_copy(w1b[:], w1f[:]), reads=["w1f"], writes=["w1b"])
                  w2f = sb(c3, "w2f", [128, 64], F32)
                  w2b = sb(c3, "w2b", [128, 64], BF16)
                  P.dma("sp", w2f[0:64], cmp_w2[:, 0], writes=["w2f"])
                  P.dma("sp", w2f[64:128], cmp_w2[:, 1], writes=["w2f"])
                  P.op("dve", lambda e: e.tensor_copy(w2b[:], w2f[:]), reads=["w2f"], writes=["w2b"])
                  hk = sb(c3, "hk", [128, 256], BF16)
                  for jj, p0 in enumerate((0, 64)):
                      for j in range(32):
                          P.op("pe", lambda e: e.matmul(banks[jj][p0:p0 + 64, 0:256], w1b[p0:p0 + 64, j, :], kvp[p0:p0 + 64, :, j],
                                                        start=(j == 0), stop=(j == 31)),
                               reads=["w1b", "kvp"], writes=[("bank", jj)])
                      P.op("act", lambda e: e.activation(out=hk[p0:p0 + 64, :], in_=banks[jj][p0:p0 + 64, 0:256], func=AF.Silu),
                           reads=[("bank", jj)], writes=["hk"])
                  P.op("pe", lambda e: e.matmul(banks[2][0:64, 0:256], w2b[0:64, :], hk[0:64, :], start=True, stop=True),
                       reads=["w2b", "hk"], writes=[("bank", 2)])
                  P.op("dve", lambda e: e.tensor_copy(kca[0:64, :], banks[2][0:64, 0:256]), reads=[("bank", 2)], writes=["kca"])
                  for cc in range(2):
                      P.op("pe", lambda e: e.matmul(banks[4 + cc][:, 0:64], hk[64:128, cc * 128:(cc + 1) * 128], w2b[64:128, :],
                                                    start=True, stop=True),
                           reads=["w2b", "hk"], writes=[("bank", 4 + cc)])
                      P.op("dve", lambda e: e.tensor_copy(vcs[:, cc, :], banks[4 + cc][:, 0:64]), reads=[("bank", 4 + cc)], writes=["vcs"])
                  P.barrier()
              e_sb = [sb(c2, "e_sb%d" % i, [128, TT], BF16) for i in range(8)]
              rec = sb(c2, "rec", [128, TT], F32)
              adj = sb(c2, "adj", [128, 128], F32)
              adj2 = sb(c2, "adj2", [128, 128], F32)
              m8a = sb(c2, "m8a", [128, 8], F32)
              m8b = sb(c2, "m8b", [128, 8], F32)
              mt = sb(c2, "mt", [128, 128], BF16)
              ocmp = sb(c2, "ocmp", [64, 2, TT], F32)
              bank4b = banks[4][:].bitcast(BF16)
              qa2 = [qa, [sb(c2, "qab%d" % h, [68, TT], BF16) for h in range(4)]]
              gl2 = [gl, [sb(c2, "glb%d" % i, [64, TT], F32) for i in range(6)]]
              ya2 = [ya, sb(c2, "yab", [128, TT], F32)]
              mneg2 = [sb(c2, "mneg%d" % i, [128, TT], BF16) for i in range(2)]
              gt_sb = sb(c2, "gt_sb", [64, TT], BF16)
              DEN = 7

              def prep(tt):
                  bf = tt % 2
                  ts_ = slice(tt * TT, (tt + 1) * TT)
                  qa_, gl_, ya_, mneg_ = qa2[bf], gl2[bf], ya2[bf], mneg2[bf]
                  for h in range(4):
                      ct = FM_AQ01 if h < 2 else FM_AQ23
                      r0 = ct * 128 + (h % 2) * 64
                      P.dma("sp", qa_[h][0:64, :], fmv[r0:r0 + 64, ts_], writes=[("qa", bf, h)])
                      P.dma("sp", qa_[h][64:68, :], qpos[h, :, ts_], writes=[("qa", bf, h)])
                  yield
                  for j in range(3):
                      for hl in range(2):
                          r0 = (FM_G0 + j) * 128 + hl * 64
                          gi = j * 2 + hl
                          P.dma("sp", gt_sb[:], fmv[r0:r0 + 64, ts_], writes=["gt_sb"])
                          P.op("act", lambda e: e.activation(out=gl_[gi][:], in_=gt_sb[:], func=AF.Sigmoid),
                               reads=["gt_sb"], writes=[("gl", bf, gi)])
                      yield
                  ccs = [0] if tt < 8 else [0, 1]
                  for h in range(4):
                      for cc in ccs:
                          zi = zrot[0] % 3
                          zrot[0] += 1
                          m = tt - 8 * cc
                          P.op("pe", lambda e: e.matmul(banks[zi][:], kca[0:68, cc * 128:(cc + 1) * 128], qa_[h][:],
                                                        start=True, stop=(m >= 8)),
                               reads=["kca", ("qa", bf, h)], writes=[("bank", zi)])
                          if m < 8:
                              P.op("pe", lambda e: e.matmul(banks[zi][:], ident_b[:], masks_sb[:, MK_CMP + m, :], start=False, stop=True),
                                   reads=["ident_b", "masks_sb"], writes=[("bank", zi)])
                          P.op("act", lambda e: e.activation(out=e_sb[h * 2 + cc][:], in_=banks[zi][:], func=AF.Exp),
                               reads=[("bank", zi)], writes=[("e_sb", h * 2 + cc)])
                          yield
                      for cc in ccs:
                          P.op("pe", lambda e: e.matmul(banks[DEN][:], ones_b[:], e_sb[h * 2 + cc][:], start=(cc == 0), stop=(cc == ccs[-1])),
                               reads=["ones_b", ("e_sb", h * 2 + cc)], writes=[("bank", DEN)])
                      P.op("dve", lambda e: e.tensor_scalar_max(out=rec[:], in0=banks[DEN][:], scalar1=1e-30),
                           reads=[("bank", DEN)], writes=["rec"])
                      P.op("dve", lambda e: e.reciprocal(out=rec[:], in_=rec[:]), reads=["rec"], writes=["rec"])
                      yield
                      for cc in ccs:
                          P.op("dve" if cc == 0 else "pool",
                               lambda e: e.tensor_tensor(out=e_sb[h * 2 + cc][:], in0=e_sb[h * 2 + cc][:], in1=rec[:], op=ALU.mult),
                               reads=[("e_sb", h * 2 + cc), "rec"], writes=[("e_sb", h * 2 + cc)])
                      if h // 2 == hh_:
                          hl = h % 2
                          for cc in ccs:
                              P.op("pe", lambda e: e.matmul(banks[5][0:64, :], vcs[:, cc, :], e_sb[h * 2 + cc][:],
                                                            start=(cc == 0), stop=(cc == ccs[-1])),
                                   reads=["vcs", ("e_sb", h * 2 + cc)], writes=[("bank", 5)])
                          P.op("dve", lambda e: e.tensor_tensor(out=ya_[hl * 64:hl * 64 + 64, :], in0=banks[5][0:64, :], in1=gl_[0 * 2 + hl][:],
                                                                op=ALU.mult),
                               reads=[("bank", 5), ("gl", bf, hl)], writes=[("ya", bf)])
                      yield
                  for sub in range(4):
                      i_blk = tt * 4 + sub
                      first_mm = True
                      nmm = 4 * len(ccs)
                      k_ = 0
                      for h in range(4):
                          for cc in ccs:
                              k_ += 1
                              P.op("pe", lambda e: e.matmul(banks[6][:, 0:128], e_sb[h * 2 + cc][:, sub * 128:(sub + 1) * 128], ps_sb[:, cc, :],
                                                            start=first_mm, stop=(k_ == nmm)),
                                   reads=[("e_sb", h * 2 + cc), "ps_sb"], writes=[("bank", 6)])
                              first_mm = False
                          if h == 1:
                              yield
                      o0 = 128 - 2 * i_blk
                      P.op("dve", lambda e: e.tensor_tensor(out=adj[:], in0=banks[6][:, 0:128], in1=keep_sb[:, o0:o0 + 128], op=ALU.mult),
                           reads=[("bank", 6), "keep_sb"], writes=["adj"])
                      P.op("dve", lambda e: e.tensor_tensor(out=adj[:], in0=adj[:], in1=add_sb[:, o0:o0 + 128], op=ALU.add),
                           reads=["adj", "add_sb"], writes=["adj"])
                      P.op("dve", lambda e: e.memset(adj[:, 0:1], 1e4), reads=["adj"], writes=["adj"])
                      yield
                      P.op("dve", lambda e: e.max(out=m8a[:], in_=adj[:]), reads=["adj"], writes=["m8a"])
                      P.op("dve", lambda e: e.match_replace(out=adj2[:], in_to_replace=m8a[:], in_values=adj[:], imm_value=-1e9),
                           reads=["adj", "m8a"], writes=["adj2"])
                      yield
                      P.op("dve", lambda e: e.max(out=m8b[:], in_=adj2[:]), reads=["adj2"], writes=["m8b"])
                      P.op("dve", lambda e: e.tensor_scalar(out=mt[:], in0=adj[:], scalar1=m8b[:, 7:8], scalar2=-1.0,
                                                            op0=ALU.is_ge, op1=ALU.add),
                           reads=["adj", "m8b"], writes=["mt"])
                      yield
                      P.op("pe", lambda e: e.transpose(bank4b[:, 0:128], mt[:], ident_b[:]),
                           reads=["mt", "ident_b"], writes=[("bank", 4)])
                      P.op("act", lambda e: e.activation(out=mneg_[:, sub * 128:(sub + 1) * 128], in_=bank4b[:, 0:128], func=AF.Copy),
                           reads=[("bank", 4)], writes=[("mneg", bf)])
                      yield

              def selwin(tt, bg):
                  bf = tt % 2
                  ts_ = slice(tt * TT, (tt + 1) * TT)
                  qa_, gl_, ya_, mneg_ = qa2[bf], gl2[bf], ya2[bf], mneg2[bf]
                  for hl in range(2):
                      h = 2 * hh_ + hl
                      chunks = [(kc, (MK_NONSTRICT + kc - 4 * tt) if kc >= 4 * tt else None) for kc in range(0, 4 * tt + 4)]
                      softmax_tile(qa_[h][:], ("qa", bf, h), ksel, "ksel", vsel, "vsel", chunks, selmask=mneg_, selkey=("mneg", bf), bg=bg)
                      P.op("dve", lambda e: e.tensor_tensor(out=tmp64[:], in0=o_sb[:], in1=gl_[1 * 2 + hl][:], op=ALU.mult),
                           reads=["o_sb", ("gl", bf, 2 + hl)], writes=["tmp64"])
                      P.op("dve", lambda e: e.tensor_copy(ocmp[:, hl, :], tmp64[:]), reads=["tmp64"], writes=["ocmp"])
                      chunks = []
                      for kc in range(max(0, 4 * tt - 4), 4 * tt + 4):
                          r = kc - 4 * tt
                          chunks.append((kc, MK_WINA + r + 4 if r < 0 else MK_NONSTRICT + r))
                      softmax_tile(qa_[h][:], ("qa", bf, h), kwin, "kwin", vwin, "vwin", chunks, bg=bg)
                      P.op("dve", lambda e: e.tensor_tensor(out=tmp64[:], in0=o_sb[:], in1=gl_[2 * 2 + hl][:], op=ALU.mult),
                           reads=["o_sb", ("gl", bf, 4 + hl)], writes=["tmp64"])
                      P.op("dve", lambda e: e.tensor_tensor(out=tmp64[:], in0=tmp64[:], in1=ocmp[:, hl, :], op=ALU.add),
                           reads=["tmp64", "ocmp"], writes=["tmp64"])
                      P.op("dve", lambda e: e.tensor_copy(ocmp[:, hl, :], ya_[hl * 64:hl * 64 + 64, :]), reads=[("ya", bf)], writes=["ocmp"])
                      P.op("dve", lambda e: e.tensor_tensor(out=ya_[hl * 64:hl * 64 + 64, :], in0=tmp64[:], in1=ocmp[:, hl, :], op=ALU.add),
                           reads=["tmp64", "ocmp"], writes=[("ya", bf)])
                  P.dma("sp", zt_sb[:], fmv[FM_AZ * 128:(FM_AZ + 1) * 128, ts_], writes=["zt_sb"])
                  P.op("act", lambda e: e.activation(out=zs_sb[:], in_=zt_sb[:], func=AF.Silu), reads=["zt_sb"], writes=["zs_sb"])
                  P.op("dve", lambda e: e.tensor_tensor(out=yo[:], in0=ya_[:], in1=zs_sb[:], op=ALU.mult),
                       reads=[("ya", bf), "zs_sb"], writes=["yo"])
                  P.dma("pool", yzd[0:128, ts_], yo[:], reads=["yo"], writes=[("yzd", 0, tt)])

              for _ in prep(0):
                  pass
              for tt in range(NT):
                  bg = prep(tt + 1) if tt + 1 < NT else None
                  selwin(tt, bg)
                  if bg is not None:
                      for _ in bg:
                          pass
              P.barrier()
        if "C" in mixers:
          with ExitStack() as c:
            ntri_sb = sb(c, "ntri_sb", [128, 128], BF16)
            P.dma("sp", ntri_sb[:], ntri[:, :], writes=["ntri_sb"])
            qs = sb(c, "qs", [64, S], BF16)
            ks = sb(c, "ks", [64, S], BF16)
            vs = sb(c, "vs", [128, NB, 64], BF16)
            ee = [sb(c, "ee%d" % i, [128, TT], F32) for i in range(2)]
            sp_ = [sb(c, "sp%d" % i, [128, TT], F32) for i in range(2)]
            hi = [sb(c, "hi%d" % i, [128, TT], BF16) for i in range(3)]
            lo = [sb(c, "lo%d" % i, [128, TT], BF16) for i in range(3)]
            ww = [sb(c, "ww%d" % i, [64, TT], F32) for i in range(3)]
            aa = [sb(c, "aa%d" % i, [128, TT], BF16) for i in range(2)]
            tmpc = sb(c, "tmpc", [64, TT], F32)
            oacc = sb(c, "oacc", [64, TT], F32)
            zt_c = sb(c, "zt_c", [64, TT], BF16)
            zs_c = sb(c, "zs_c", [64, TT], F32)
            yo_c = sb(c, "yo_c", [64, TT], BF16)
            CARRY = 4
            for hl in range(2):
                P.dma("sp", qs[:], fmv[FM_CQ * 128 + hl * 64:FM_CQ * 128 + hl * 64 + 64, :], writes=["qs"])
                P.dma("sp", ks[:], fmv[FM_CK * 128 + hl * 64:FM_CK * 128 + hl * 64 + 64, :], writes=["ks"])
                P.dma("sp", vs[:], tmv[:, :, TM_CV0 + 64 * hl:TM_CV0 + 64 * hl + 64], writes=["vs"])
                recs = []
                for tt in range(NT):
                    kcs = list(range(4 * tt + 3, -1, -1))
                    for j, kc in enumerate(kcs):
                        recs.append(dict(tt=tt, kc=kc, first=(j == 0), last=(j == len(kcs) - 1), i=len(recs)))

                def s1(r):
                    i = r["i"]; zi = i % 4; tt = r["tt"]; kc = r["kc"]
                    diag = kc >= 4 * tt
                    P.op("pe", lambda e: e.matmul(banks[zi][:], ks[:, kc * 128:(kc + 1) * 128], qs[:, tt * TT:(tt + 1) * TT],
                                                  start=True, stop=not diag),
                         reads=["ks", "qs"], writes=[("bank", zi)])
                    if diag:
                        P.op("pe", lambda e: e.matmul(banks[zi][:], ident_b[:], masks_sb[:, MK_STRICT + kc - 4 * tt, :], start=False, stop=True),
                             reads=["ident_b", "masks_sb"], writes=[("bank", zi)])

                def s2(r):
                    i = r["i"]; zi = i % 4
                    P.op("act", lambda e: e.activation(out=ee[i % 2][:], in_=banks[zi][:], func=AF.Exp),
                         reads=[("bank", zi)], writes=[("ee", i % 2)])
                    P.op("act", lambda e: e.activation(out=sp_[i % 2][:], in_=ee[i % 2][:], func=AF.Ln, bias=1.0),
                         reads=[("ee", i % 2)], writes=[("sp", i % 2)])
                    P.op("dve", lambda e: e.tensor_copy(hi[i % 3][:], sp_[i % 2][:]), reads=[("sp", i % 2)], writes=[("hi", i % 3)])
                    P.op("dve" if i % 2 == 0 else "pool", lambda e: e.tensor_tensor(out=lo[i % 3][:], in0=sp_[i % 2][:], in1=hi[i % 3][:], op=ALU.subtract),
                         reads=[("sp", i % 2), ("hi", i % 3)], writes=[("lo", i % 3)])

                def s3(r):
                    i = r["i"]; zi = i % 4
                    if not r["first"]:
                        P.op("act", lambda e: e.activation(out=ww[i % 3][:], in_=banks[CARRY][0:64, :], func=AF.Exp, scale=-1.0),
                             reads=[("bank", CARRY)], writes=[("ww", i % 3)])
                    P.op("pe", lambda e: e.matmul(banks[zi][:], ntri_sb[:], hi[i % 3][:], start=False, stop=False, skip_group_check=True),
                         reads=["ntri_sb", ("hi", i % 3)], writes=[("bank", zi)])
                    P.op("pe", lambda e: e.matmul(banks[zi][:], ntri_sb[:], lo[i % 3][:], start=False, stop=True, skip_group_check=True),
                         reads=["ntri_sb", ("lo", i % 3)], writes=[("bank", zi)])
                    if not r["last"]:
                        P.op("pe", lambda e: e.matmul(banks[CARRY][0:64, :], ones_b[:, 0:64], hi[i % 3][:], start=r["first"], stop=False, skip_group_check=True),
                             reads=["ones_b", ("hi", i % 3)], writes=[("bank", CARRY)])
                        P.op("pe", lambda e: e.matmul(banks[CARRY][0:64, :], ones_b[:, 0:64], lo[i % 3][:], start=False, stop=True, skip_group_check=True),
                             reads=["ones_b", ("lo", i % 3)], writes=[("bank", CARRY)])

                def s4(r):
                    i = r["i"]; zi = i % 4; tt = r["tt"]; kc = r["kc"]
                    pb = 5 + i % 2
                    P.op("act", lambda e: e.activation(out=aa[i % 2][:], in_=banks[zi][:], func=AF.Exp),
                         reads=[("bank", zi)], writes=[("aa", i % 2)])
                    P.op("pe", lambda e: e.matmul(banks[pb][0:64, :], vs[:, kc, :], aa[i % 2][:], start=True, stop=True),
                         reads=["vs", ("aa", i % 2)], writes=[("bank", pb)])
                    if r["first"]:
                        P.op("dve", lambda e: e.tensor_copy(oacc[:], banks[pb][0:64, :]), reads=[("bank", pb)], writes=["oacc"])
                    else:
                        P.op("dve", lambda e: e.tensor_tensor(out=tmpc[:], in0=banks[pb][0:64, :], in1=ww[i % 3][:], op=ALU.mult),
                             reads=[("bank", pb), ("ww", i % 3)], writes=["tmpc"])
                        P.op("pool", lambda e: e.tensor_tensor(out=oacc[:], in0=oacc[:], in1=tmpc[:], op=ALU.add),
                             reads=["oacc", "tmpc"], writes=["oacc"])
                    if r["last"]:
                        ts_ = slice(tt * TT, (tt + 1) * TT)
                        r0 = FM_CZ * 128 + hl * 64
                        P.dma("sp", zt_c[:], fmv[r0:r0 + 64, ts_], writes=["zt_c"])
                        P.op("act", lambda e: e.activation(out=zs_c[:], in_=zt_c[:], func=AF.Silu), reads=["zt_c"], writes=["zs_c"])
                        P.op("dve", lambda e: e.tensor_tensor(out=yo_c[:], in0=oacc[:], in1=zs_c[:], op=ALU.mult),
                             reads=["oacc", "zs_c"], writes=["yo_c"])
                        P.dma("pool", yzd[256 + hl * 64:256 + hl * 64 + 64, ts_], yo_c[:], reads=["yo_c"], writes=[("yzd", 2, hl, tt)])

                n = len(recs)
                for step in range(n + 3):
                    for lag, fn in ((0, s1), (1, s2), (2, s3), (3, s4)):
                        j = step - lag
                        if 0 <= j < n:
                            fn(recs[j])
            P.barrier()

        if "D" in mixers:
          with ExitStack() as c:
            qd = sb(c, "qd", [64, S], BF16)
            kd = sb(c, "kd", [64, S], BF16)
            vd = sb(c, "vd", [128, NB, 64], BF16)
            kt = sb(c, "kt", [128, NB, 64], BF16)
            vz = sb(c, "vz", [128, NB, 64], BF16)
            dt_sb = sb(c, "dt_sb", [128, 2, 128], F32)
            ze_sb = sb(c, "ze_sb", [128, 2], F32)
            xi_sb = sb(c, "xi_sb", [64, 2, 128], F32)
            gc_sb = sb(c, "gc_sb", [64, 2], F32)
            P.dma("sp", dt_sb[:], dtab[:, :, :], writes=["dt_sb"])
            P.dma("sp", ze_sb[:], zeta[:, :], writes=["ze_sb"])
            P.dma("sp", xi_sb[:], xi_bc[:, :, :], writes=["xi_sb"])
            P.dma("sp", gc_sb[:], gchunk[:, :], writes=["gc_sb"])
            uall = sb(c, "uall", [64, NB, 64], F32)
            rall = sb(c, "rall", [64, NB, 64], F32)
            rbf = sb(c, "rbf", [64, NB, 64], BF16)
            sm = [sb(c, "sm%d" % i, [128, TT], BF16) for i in range(2)]
            od = sb(c, "od", [64, TT], F32)
            od2 = sb(c, "od2", [64, TT], F32)
            mean = sb(c, "mean", [64, TT], F32)
            var = sb(c, "var", [64, TT], F32)
            zt_d = sb(c, "zt_d", [64, TT], BF16)
            zs_d = sb(c, "zs_d", [64, TT], F32)
            yo_d = sb(c, "yo_d", [64, TT], BF16)
            for hl in range(2):
                P.dma("sp", qd[:], fmv[FM_DQ * 128 + hl * 64:FM_DQ * 128 + hl * 64 + 64, :], writes=["qd"])
                P.dma("sp", kd[:], fmv[FM_DK * 128 + hl * 64:FM_DK * 128 + hl * 64 + 64, :], writes=["kd"])
                P.dma("sp", vd[:], tmv[:, :, TM_DV0 + 64 * hl:TM_DV0 + 64 * hl + 64], writes=["vd"])
                P.dma("sp", kt[:], tmv[:, :, TM_DK0 + 64 * hl:TM_DK0 + 64 * hl + 64], writes=["kt"])
                P.op("dve", lambda e: e.tensor_scalar(out=vz[:], in0=vd[:], scalar1=ze_sb[:, hl:hl + 1], scalar2=None, op0=ALU.mult),
                     reads=["vd", "ze_sb"], writes=["vz"])
                for g in range(NB // 8):
                    bi = g % 2
                    for j in range(8):
                        n_ = g * 8 + j
                        P.op("pe", lambda e: e.matmul(banks[bi][0:64, j * 64:(j + 1) * 64], kt[:, n_, :], vz[:, n_, :],
                                                      start=(j == 0), stop=(j == 7)),
                             reads=["kt", "vz"], writes=[("bank", bi)])
                    P.op("act", lambda e: e.activation(out=uall[:, g * 8:(g + 1) * 8, :],
                                                       in_=banks[bi][0:64, :].rearrange("p (n e) -> p n e", e=64), func=AF.Copy),
                         reads=[("bank", bi)], writes=["uall"])
                P.op("dve", lambda e: e.memset(rall[:, 0, :], 0.0), writes=["rall"])
                for n_ in range(1, NB):
                    P.op("dve", lambda e: e.scalar_tensor_tensor(out=rall[:, n_, :], in0=rall[:, n_ - 1, :], scalar=gc_sb[:, hl:hl + 1],
                                                                 in1=uall[:, n_ - 1, :], op0=ALU.mult, op1=ALU.add),
                         reads=["rall", "uall", "gc_sb"], writes=["rall"])
                P.op("dve", lambda e: e.tensor_copy(rbf[:], rall[:]), reads=["rall"], writes=["rbf"])
                for tt in range(NT):
                    ts_ = slice(tt * TT, (tt + 1) * TT)
                    si = tt % 2
                    zi = tt % 2
                    for sub in range(4):
                        n_ = tt * 4 + sub
                        cs_ = slice(n_ * 128, (n_ + 1) * 128)
                        P.op("pe", lambda e: e.matmul(banks[zi][:, sub * 128:(sub + 1) * 128], kd[:, cs_], qd[:, cs_],
                                                      start=(sub == 0), stop=(sub == 3)),
                             reads=["kd", "qd"], writes=[("bank", zi)])
                    P.op("dve", lambda e: e.tensor_tensor(out=sm[si][:].rearrange("p (n i) -> p n i", i=128),
                                                          in0=banks[zi][:].rearrange("p (n i) -> p n i", i=128),
                                                          in1=_bc(dt_sb[:, hl, :], [128, 4, 128], 1), op=ALU.mult),
                         reads=[("bank", zi), "dt_sb"], writes=[("sm", si)])
                    for sub in range(4):
                        n_ = tt * 4 + sub
                        cs_ = slice(n_ * 128, (n_ + 1) * 128)
                        P.op("pe", lambda e: e.matmul(banks[2 + zi][0:64, sub * 128:(sub + 1) * 128], vd[:, n_, :], sm[si][:, sub * 128:(sub + 1) * 128],
                                                      start=(sub == 0), stop=(sub == 3)),
                             reads=["vd", ("sm", si)], writes=[("bank", 2 + zi)])
                    for sub in range(4):
                        n_ = tt * 4 + sub
                        cs_ = slice(n_ * 128, (n_ + 1) * 128)
                        P.op("pe", lambda e: e.matmul(banks[4 + zi][0:64, sub * 128:(sub + 1) * 128], rbf[:, n_, :], qd[:, cs_],
                                                      start=(sub == 0), stop=(sub == 3)),
                             reads=["rbf", "qd"], writes=[("bank", 4 + zi)])
                    P.op("dve", lambda e: e.tensor_tensor(out=od[:].rearrange("p (n i) -> p n i", i=128),
                                                          in0=banks[4 + zi][0:64, :].rearrange("p (n i) -> p n i", i=128),
                                                          in1=_bc(xi_sb[:, hl, :], [64, 4, 128], 1), op=ALU.mult),
                         reads=[("bank", 4 + zi), "xi_sb"], writes=["od"])
                    P.op("dve", lambda e: e.tensor_tensor(out=od[:], in0=od[:], in1=banks[2 + zi][0:64, :], op=ALU.add),
                         reads=["od", ("bank", 2 + zi)], writes=["od"])
                    P.op("act", lambda e: e.activation(out=od2[:], in_=od[:], func=AF.Square), reads=["od"], writes=["od2"])
                    P.op("pe", lambda e: e.matmul(banks[6][0:64, :], ones_f[0:64, 0:64], od[:], start=True, stop=True),
                         reads=["ones_f", "od"], writes=[("bank", 6)])
                    P.op("pe", lambda e: e.matmul(banks[7][0:64, :], ones_f[0:64, 0:64], od2[:], start=True, stop=True),
                         reads=["ones_f", "od2"], writes=[("bank", 7)])
                    P.op("act", lambda e: e.activation(out=mean[:], in_=banks[6][0:64, :], func=AF.Copy, scale=1.0 / 64),
                         reads=[("bank", 6)], writes=["mean"])
                    P.op("dve", lambda e: e.tensor_tensor(out=var[:], in0=mean[:], in1=mean[:], op=ALU.mult),
                         reads=["mean"], writes=["var"])
                    P.op("dve", lambda e: e.scalar_tensor_tensor(out=var[:], in0=banks[7][0:64, :], scalar=1.0 / 64, in1=var[:],
                                                                 op0=ALU.mult, op1=ALU.subtract),
                         reads=[("bank", 7), "var"], writes=["var"])
                    P.op("act", lambda e: e.activation(out=var[:], in_=var[:], func=AF.Sqrt, bias=LN_EPS), reads=["var"], writes=["var"])
                    P.op("dve", lambda e: e.reciprocal(out=var[:], in_=var[:]), reads=["var"], writes=["var"])
                    P.op("dve", lambda e: e.tensor_tensor(out=od[:], in0=od[:], in1=mean[:], op=ALU.subtract),
                         reads=["od", "mean"], writes=["od"])
                    P.op("dve", lambda e: e.tensor_tensor(out=od[:], in0=od[:], in1=var[:], op=ALU.mult),
                         reads=["od", "var"], writes=["od"])
                    r0 = FM_DZ * 128 + hl * 64
                    P.dma("sp", zt_d[:], fmv[r0:r0 + 64, ts_], writes=["zt_d"])
                    P.op("act", lambda e: e.activation(out=zs_d[:], in_=zt_d[:], func=AF.Silu), reads=["zt_d"], writes=["zs_d"])
                    P.op("dve", lambda e: e.tensor_tensor(out=yo_d[:], in0=od[:], in1=zs_d[:], op=ALU.mult),
                         reads=["od", "zs_d"], writes=["yo_d"])
                    P.dma("pool", yzd[384 + hl * 64:384 + hl * 64 + 64, ts_], yo_d[:], reads=["yo_d"], writes=[("yzd", 3, hl, tt)])
            P.barrier()
        stage2.close()
        if stop_after <= 2:
            P.finish()
            return nc

        with ExitStack() as c:
            wm = sb(c, "wm", [128, 4, 8, D], BF16)
            wo = sb(c, "wo", [128, 8, D], BF16)
            wbr = sb(c, "wbr", [128, 4, D], BF16)
            stg = [sb(c, "stg3_%d" % i, [128, D], F32) for i in range(2)]
            k_ = 0
            for i in range(4):
                for kc in range(8):
                    load_cast(c, stg, wm[:, i, kc, :], w_merge[i, kc * 128:(kc + 1) * 128, :], D, ("wm", i, kc), k_)
                    k_ += 1
                load_cast(c, stg, wbr[:, i, :], w_br[i, :, :], D, ("wbr", i), k_)
                k_ += 1
            for kc in range(8):
                load_cast(c, stg, wo[:, kc, :], w_out[kc * 128:(kc + 1) * 128, :], D, ("wo", kc), k_)
                k_ += 1
            hb3 = [sb(c, "hb3_%d" % i, [128, 8, TT], BF16) for i in range(2)]
            yz3 = [sb(c, "yz3_%d" % i, [128, 4, TT], BF16) for i in range(2)]
            sg = [sb(c, "sg%d" % i, [128, TT], F32) for i in range(2)]
            term = [sb(c, "term%d" % i, [128, TT], F32) for i in range(2)]
            mrg = sb(c, "mrg", [128, 8, TT], F32)
            mg = sb(c, "mg", [128, 8, TT], BF16)
            po = [sb(c, "po%d" % i, [128, TT], F32) for i in range(2)]
            hview = hT.rearrange("(k p) t -> p k t", p=128)
            yview = yzd.rearrange("(m p) t -> p m t", p=128)
            pview = partT.rearrange("(k p) t -> p k t", p=128)
            nb_ = 0
            ns_ = 0
            for tt in range(NT):
                i2 = tt % 2
                ts_ = slice(tt * TT, (tt + 1) * TT)
                P.dma("sp", hb3[i2][:], hview[:, :, ts_], writes=[("hb3", i2)])
                P.dma("sp", yz3[i2][:], yview[:, :, ts_], writes=[("yz3", i2)])
                for oc in range(8):
                    for i in range(4):
                        bg = nb_ % 6
                        nb_ += 1
                        for kc in range(8):
                            P.op("pe", lambda e: e.matmul(banks[bg][:], wm[:, i, kc, oc * 128:(oc + 1) * 128], hb3[i2][:, kc, :],
                                                          start=(kc == 0), stop=(kc == 7)),
                                 reads=[("wm", i, kc), ("hb3", i2)], writes=[("bank", bg)])
                        bb = 6 + ns_ % 2
                        P.op("pe", lambda e: e.matmul(banks[bb][:], wbr[:, i, oc * 128:(oc + 1) * 128], yz3[i2][:, i, :], start=True, stop=True),
                             reads=[("wbr", i), ("yz3", i2)], writes=[("bank", bb)])
                        si = ns_ % 2
                        ns_ += 1
                        P.op("act", lambda e: e.activation(out=sg[si][:], in_=banks[bg][:], func=AF.Sigmoid),
                             reads=[("bank", bg)], writes=[("sg", si)])
                        if i == 0:
                            P.op("dve", lambda e: e.tensor_tensor(out=mrg[:, oc, :], in0=banks[bb][:], in1=sg[si][:], op=ALU.mult),
                                 reads=[("bank", bb), ("sg", si)], writes=[("mrg", oc)])
                        else:
                            P.op("dve", lambda e: e.tensor_tensor(out=term[si][:], in0=banks[bb][:], in1=sg[si][:], op=ALU.mult),
                                 reads=[("bank", bb), ("sg", si)], writes=[("term", si)])
                            P.op("pool", lambda e: e.tensor_tensor(out=mrg[:, oc, :], in0=mrg[:, oc, :], in1=term[si][:], op=ALU.add),
                                 reads=[("mrg", oc), ("term", si)], writes=[("mrg", oc)])
                    P.op("pool", lambda e: e.tensor_copy(mg[:, oc, :], mrg[:, oc, :]), reads=[("mrg", oc)], writes=[("mg", oc)])
                for oc2 in range(8):
                    bg = nb_ % 6
                    nb_ += 1
                    for kc in range(8):
                        P.op("pe", lambda e: e.matmul(banks[bg][:], wo[:, kc, oc2 * 128:(oc2 + 1) * 128], mg[:, kc, :],
                                                      start=(kc == 0), stop=(kc == 7)),
                             reads=[("wo", kc), ("mg", kc)], writes=[("bank", bg)])
                    pi = oc2 % 2
                    P.op("act", lambda e: e.activation(out=po[pi][:], in_=banks[bg][:], func=AF.Copy, scale=mod[:, 16 + oc2:17 + oc2]),
                         reads=[("bank", bg), "mod"], writes=[("po", pi)])
                    P.dma("pool", pview[:, oc2, ts_], po[pi][:], reads=[("po", pi)], is_output=True)
            P.barrier()
        P.finish()
    return nc


def _bf(a):
    return np.asarray(a, dtype=np.float32).astype(ml_dtypes.bfloat16)


def make_consts():
    sl = np.arange(128)[:, None]
    tl = np.arange(TT)[None, :]
    m = np.zeros((NMASK, 128, TT), np.float32)

    def put(idx, ok):
        m[idx] = np.where(ok, 0.0, NEG)
    for r in range(4):
        put(MK_STRICT + r, sl + 128 * r < tl)
        put(MK_NONSTRICT + r, sl + 128 * r <= tl)
    for j, r in enumerate(range(-4, 0)):
        d = tl - sl - 128 * r
        put(MK_WINA + j, (d >= 0) & (d < 512))
    for j, r in enumerate(range(-1, 4)):
        d = tl - sl - 128 * r
        put(MK_WINB + j, (d >= 0) & (d < 128))
    for mm in range(8):
        put(MK_CMP + mm, tl + 512 * mm - 32 * sl - 31 >= 0)
    t = np.arange(S)
    slopes = 2.0 ** (-8.0 * (np.arange(4) + 1) / 4)
    qpos = np.zeros((4, 4, S), np.float32)
    for h in range(4):
        qpos[h, 0] = -slopes[h] * 64 * (t // 64)
        qpos[h, 1] = -slopes[h] * (t % 64)
        qpos[h, 2] = slopes[h] * 64
        qpos[h, 3] = slopes[h]
    kpos_tok = np.stack([np.ones(S), np.ones(S), t // 64, t % 64]).astype(np.float32)
    pc = 32 * np.arange(256) + 31
    kpos_cmp = np.stack([np.ones(256), np.ones(256), pc // 64, pc % 64]).astype(np.float32)
    ewide = 30000.0 * (np.arange(S)[None, :] // 64 == np.arange(128)[:, None]).astype(np.float32)
    ident = np.eye(128, dtype=np.float32)
    ntri = -(np.arange(128)[:, None] >= np.arange(128)[None, :]).astype(np.float32)
    pairsum = np.zeros((2, 128, 128), np.float32)
    for cc in range(2):
        pairsum[cc, np.arange(128), 64 * cc + np.arange(128) // 2] = 1.0
    u = np.arange(256)[None, :] - 128
    cur = (np.arange(128)[:, None] >= 64).astype(np.int64)
    forced = (u == cur) | (u == cur - 1)
    future = u > cur
    selkeep = (~(forced | future)).astype(np.float32)
    seladd = np.where(forced, 1e4, np.where(future, -1.0, 0.0)).astype(np.float32)
    return dict(masks=_bf(m), qpos_all=qpos, kpos_tok=_bf(kpos_tok), kpos_cmp=_bf(kpos_cmp), ewide=_bf(ewide),
                ident=_bf(ident), ntri=_bf(ntri), pairsum=_bf(pairsum), selkeep=selkeep, seladd=seladd)


def ret_consts(hh):
    out = {}
    i = np.arange(128)
    dt_ = np.zeros((128, 2, 128), np.float32)
    zt = np.zeros((128, 2), np.float32)
    xi = np.zeros((64, 2, 128), np.float32)
    gc = np.zeros((64, 2), np.float32)
    for hl in range(2):
        h = 2 * hh + hl
        log_g = np.log(np.float32(1.0) - np.float32(2.0 ** (-5.0 - h)))
        diff = (i[None, :] - i[:, None]).astype(np.float32)
        dt_[:, hl, :] = np.where(diff >= 0, np.exp(log_g * np.maximum(diff, 0.0)), 0.0)
        zt[:, hl] = np.exp(log_g * (127 - i))
        xi[:, hl, :] = np.exp(log_g * (i + 1))[None, :]
        gc[:, hl] = np.exp(log_g * 128)
    return dict(dtab=dt_, zeta=zt, xi_bc=xi, gchunk=gc)


def arrange_w_in(w, hh):
    o = np.cumsum([0, 256, 384, 12, 256, 256, 128, 128, 256, 256, 256, 256, 256, 256, 256, 256, 256])
    a_q, a_kv, a_g, a_z, b_q, b_k, b_v, b_z, c_q, c_k, c_v, c_z, d_q, d_k, d_v, d_z = [
        w[:, o[i]:o[i + 1]] for i in range(16)]
    own = slice(128 * hh, 128 * hh + 128)
    kvh = slice(64 * hh, 64 * hh + 64)
    k_cmp, v_cmp, k_sel, v_sel, k_win, v_win = [a_kv[:, 64 * i:64 * i + 64] for i in range(6)]
    gcols = []
    for j in range(3):
        g = np.concatenate([np.repeat(a_g[:, 3 * (2 * hh + hl) + j][:, None], 64, axis=1) for hl in range(2)], axis=1)
        gcols.append(g)
    zpad = np.zeros((D, 64), np.float32)
    oth = slice(128 * (1 - hh), 128 * (1 - hh) + 128)
    fmt = [a_q[:, own], a_q[:, oth], np.concatenate([k_cmp, v_cmp], 1), np.concatenate([k_sel, k_win], 1),
           gcols[0], gcols[1], gcols[2], a_z[:, own], b_q[:, own], np.concatenate([b_k[:, kvh], zpad], 1), b_z[:, own],
           c_q[:, own], c_k[:, own], c_z[:, own], d_q[:, own], d_k[:, own], d_z[:, own]]
    tmc = [v_sel, v_win, b_v[:, kvh], c_v[:, own], d_v[:, own], d_k[:, own]]
    return np.ascontiguousarray(np.concatenate(fmt + tmc, axis=1), dtype=np.float32)


def layer_inputs(l, b, hh, c, w_ada, b_ada, norm_g, w_in, cmp_pos, cmp_w1, cmp_w2, sink, w_merge, w_br, w_out, consts):
    d = dict(consts)
    qp = d.pop("qpos_all")
    order = [2 * hh, 2 * hh + 1, 2 * (1 - hh), 2 * (1 - hh) + 1]
    d["qpos"] = _bf(qp[order])
    d.update(ret_consts(hh))
    d["cT"] = np.ascontiguousarray(c[b].reshape(8, 128).T)
    d["w_ada"] = np.ascontiguousarray(w_ada[l])
    d["b_adaT"] = np.ascontiguousarray(b_ada[l].reshape(24, 128).T)
    d["gT"] = np.ascontiguousarray(norm_g[l].reshape(8, 128).T)
    d["w_in"] = arrange_w_in(w_in[l], hh)
    d["cmp_posT"] = np.ascontiguousarray(cmp_pos[l].transpose(2, 0, 1))
    d["cmp_w1"] = np.ascontiguousarray(cmp_w1[l].reshape(2, 32, 64, 64).transpose(2, 0, 1, 3))
    d["cmp_w2"] = np.ascontiguousarray(cmp_w2[l].transpose(1, 0, 2))
    d["sinkb"] = np.ascontiguousarray(np.repeat(sink[l][2 * hh:2 * hh + 2], 64)[:, None].astype(np.float32))
    d["w_merge"] = np.ascontiguousarray(w_merge[l])
    d["w_br"] = np.ascontiguousarray(w_br[l][:, 128 * hh:128 * hh + 128, :])
    d["w_out"] = np.ascontiguousarray(w_out[l])
    return d


_PROG_CACHE = {}


def _prog(first, last_only):
    key = (first, last_only)
    if key not in _PROG_CACHE:
        _PROG_CACHE[key] = build_program(first, last_only)
    return _PROG_CACHE[key]


def kernel(x, c, w_ada, b_ada, norm_g, w_in, cmp_pos, cmp_w1, cmp_w2, sink, w_merge, w_br, w_out, final_g):
    args = [np.asarray(a, dtype=np.float32) for a in
            (x, c, w_ada, b_ada, norm_g, w_in, cmp_pos, cmp_w1, cmp_w2, sink, w_merge, w_br, w_out, final_g)]
    x, c, w_ada, b_ada, norm_g, w_in, cmp_pos, cmp_w1, cmp_w2, sink, w_merge, w_br, w_out, final_g = args
    B = x.shape[0]
    depth = w_ada.shape[0]
    consts = make_consts()
    cores = [(b, hh) for b in range(B) for hh in range(2)]
    xT = [np.ascontiguousarray(x[b].T) for b in range(B)]
    zero = np.zeros((D, S), np.float32)
    parts = [zero] * (2 * B)
    for l in range(depth):
        in_maps = []
        for (b, hh) in cores:
            d = layer_inputs(l, b, hh, c, w_ada, b_ada, norm_g, w_in, cmp_pos, cmp_w1, cmp_w2, sink, w_merge, w_br, w_out, consts)
            d["xT"] = xT[b]
            d["paT"] = parts[2 * b]
            d["pbT"] = parts[2 * b + 1]
            in_maps.append(d)
        res = run_bass_kernel_spmd(_prog(False, False), in_maps, core_ids=list(range(len(cores))))
        xT = [np.asarray(res.results[2 * b]["xcT"]) for b in range(B)]
        parts = [np.asarray(r["partT"]) for r in res.results]
    in_maps = []
    fgT = np.ascontiguousarray(final_g.reshape(8, 128).T)
    for (b, hh) in cores:
        in_maps.append({"xT": xT[b], "paT": parts[2 * b], "pbT": parts[2 * b + 1], "fgT": fgT})
    res = run_bass_kernel_spmd(_prog(False, True), in_maps, core_ids=list(range(len(cores))))
    out = np.stack([np.asarray(res.results[2 * b]["outT"]).T for b in range(B)]).astype(np.float32)
    return np.ascontiguousarray(out)
```
